# Optimizing a Trainium2 kernel written in Bass

```python
import math
import jax, jax.numpy as jnp
from jax import lax
import numpy as np

D_MODEL = 1024
BATCH = 4
SEQ = 8192
DEPTH = 4

N_MIXERS = 4
N_HEADS = 8
HEAD_DIM = D_MODEL // N_HEADS
ROPE_THETA = 10000.0
Q_BLOCK = 128
HG_CHUNK = 64
DA_HALF = HEAD_DIM // 2
NSA_GROUPS = 2
NSA_HPG = N_HEADS // NSA_GROUPS
CMP_BLOCK = 32
CMP_STRIDE = 16
SLC_BLOCK = 64
SLC_TOPK = 16
WINDOW = 512
NSA_Q_BLOCK = 64
D_FF = 2816
N_EXPERTS = 8
TOP_K = 2
D_FF_EXPERT = 3584
MOE_BLOCK = 128
LN_EPS = 1e-5
NEG = -1e30
FORCE = 1e9
DEEPNORM_ALPHA = (2 * DEPTH) ** 0.25
DEEPNORM_BETA = (8 * DEPTH) ** -0.25

kernel_name = 'hybrid_hgrn2_diff_nsa_stickbreak_moe'


def layer_norm(x, g, b):
    xf = x.astype(jnp.float32)
    mu = jnp.mean(xf, axis=-1, keepdims=True)
    xc = xf - mu
    var = jnp.mean(xc * xc, axis=-1, keepdims=True)
    return (xc * lax.rsqrt(var + LN_EPS) * g.astype(jnp.float32) + b.astype(jnp.float32)).astype(x.dtype)


def rms_norm(x, g):
    xf = x.astype(jnp.float32)
    return xf * lax.rsqrt(jnp.mean(xf * xf, axis=-1, keepdims=True) + LN_EPS) * g.astype(jnp.float32)


def rope(x, pos):
    d = x.shape[-1]
    half = d // 2
    inv = ROPE_THETA ** (-jnp.arange(half, dtype=jnp.float32) / half)
    ang = pos.astype(jnp.float32)[:, None] * inv[None, :]
    shp = (1, x.shape[1]) + (1,) * (x.ndim - 3) + (half,)
    cos = jnp.cos(ang).reshape(shp)
    sin = jnp.sin(ang).reshape(shp)
    xf = x.astype(jnp.float32)
    x1, x2 = xf[..., :half], xf[..., half:]
    return jnp.concatenate([x1 * cos - x2 * sin, x2 * cos + x1 * sin], axis=-1).astype(x.dtype)


def masked_softmax(s, mask):
    s = jnp.where(mask, s.astype(jnp.float32), NEG)
    p = jax.nn.softmax(s, axis=-1)
    return jnp.where(mask, p, 0.0)


def hgrn2_mixer(h, w_in, lb_logits, norm_g, w_out, layer):
    b, s, _ = h.shape
    nh, dk = N_HEADS, HEAD_DIM
    c = HG_CHUNK
    nc = s // c
    q, f, i, g = jnp.split(h @ w_in, 4, axis=-1)
    lb = jnp.cumsum(jax.nn.softmax(lb_logits.astype(jnp.float32), axis=0), axis=0)[layer]
    forget = lb + (1.0 - lb) * jax.nn.sigmoid(f.astype(jnp.float32))
    log_f = jnp.log(forget)
    k = 1.0 - forget
    q = jax.nn.silu(q.astype(jnp.float32))
    v = i.astype(jnp.float32)

    def to_chunks(t):
        return t.reshape(b, nc, c, nh, dk).transpose(1, 0, 3, 2, 4)

    tri = jnp.tril(jnp.ones((c, c), bool))[:, :, None]

    def step(state, inp):
        qc, kc, vc, lf = inp
        cum = jnp.cumsum(lf, axis=2)
        rel = jnp.where(tri, cum[:, :, :, None, :] - cum[:, :, None, :, :], -jnp.inf)
        scores = jnp.einsum('bhtd,bhsd,bhtsd->bhts', qc, kc, jnp.exp(rel))
        out = scores @ vc + jnp.einsum('bhtd,bhdv->bhtv', qc * jnp.exp(cum), state)
        last = cum[:, :, -1:, :]
        new_state = jnp.exp(last[:, :, 0, :, None]) * state + jnp.einsum('bhsd,bhsv->bhdv', kc * jnp.exp(last - cum), vc)
        return new_state, out

    state0 = jnp.zeros((b, nh, dk, dk), jnp.float32)
    _, o = lax.scan(step, state0, (to_chunks(q), to_chunks(k), to_chunks(v), to_chunks(log_f)))
    o = o.transpose(1, 0, 3, 2, 4).reshape(b, s, nh, dk)
    o = rms_norm(o, norm_g) * jax.nn.sigmoid(g.astype(jnp.float32)).reshape(b, s, nh, dk)
    return o.reshape(b, s, D_MODEL).astype(h.dtype) @ w_out


def diff_attention_mixer(h, w_in, lam, norm_g, w_out, pos, layer):
    b, s, _ = h.shape
    q, k, v = jnp.split(h @ w_in, 3, axis=-1)
    q = rope(q.reshape(b, s, N_HEADS, 2, DA_HALF), pos).transpose(0, 2, 3, 1, 4)
    k = rope(k.reshape(b, s, N_HEADS, 2, DA_HALF), pos).transpose(0, 2, 3, 1, 4)
    v = v.reshape(b, s, N_HEADS, HEAD_DIM).transpose(0, 2, 1, 3)
    lam_init = 0.8 - 0.6 * math.exp(-0.3 * layer)
    lf = lam.astype(jnp.float32)
    lmb = jnp.exp(jnp.sum(lf[0] * lf[1])) - jnp.exp(jnp.sum(lf[2] * lf[3])) + lam_init
    kpos = jnp.arange(s)
    scale = DA_HALF ** -0.5

    def block(bi):
        s0 = bi * Q_BLOCK
        qb = lax.dynamic_slice_in_dim(q, s0, Q_BLOCK, axis=3)
        sc = jnp.einsum('bhcqd,bhckd->bhcqk', qb, k).astype(jnp.float32) * scale
        mask = (s0 + jnp.arange(Q_BLOCK))[:, None] >= kpos[None, :]
        p = masked_softmax(sc, mask)
        attn = p[:, :, 0] - lmb * p[:, :, 1]
        return jnp.einsum('bhqk,bhkd->bqhd', attn.astype(v.dtype), v)

    o = lax.map(block, jnp.arange(s // Q_BLOCK))
    o = o.transpose(1, 0, 2, 3, 4).reshape(b, s, N_HEADS, HEAD_DIM)
    o = rms_norm(o, norm_g) * (1.0 - lam_init)
    return o.reshape(b, s, D_MODEL).astype(h.dtype) @ w_out


def nsa_mixer(h, w_in, cmp_pe, cmp_w1, cmp_w2, w_out, pos):
    b, s, _ = h.shape
    G, HG, dk = NSA_GROUPS, NSA_HPG, HEAD_DIM
    proj = h @ w_in
    q, kv, gates = jnp.split(proj, [D_MODEL, D_MODEL + 6 * G * dk], axis=-1)
    q = q.reshape(b, s, N_HEADS, dk)
    kv = kv.reshape(b, s, 6, G, dk)
    k_cmp, v_cmp, k_slc, v_slc, k_win, v_win = [kv[:, :, j] for j in range(6)]
    gates = jax.nn.sigmoid(gates.astype(jnp.float32)).reshape(b, s, N_HEADS, 3)
    q_rot = rope(q, pos)
    k_slc = rope(k_slc, pos)
    k_win = rope(k_win, pos)

    n_cmp = (s - CMP_BLOCK) // CMP_STRIDE + 1
    tok_idx = np.arange(n_cmp)[:, None] * CMP_STRIDE + np.arange(CMP_BLOCK)[None, :]

    def compress(t, j):
        blk = t[:, tok_idx] + cmp_pe[j][None, None, :, None, :]
        blk = blk.transpose(0, 3, 1, 2, 4).reshape(b, G, n_cmp, CMP_BLOCK * dk)
        return jax.nn.gelu(blk @ cmp_w1[j]) @ cmp_w2[j]

    kc = compress(k_cmp, 0)
    vc = compress(v_cmp, 1)
    cmp_end = jnp.asarray(np.arange(n_cmp) * CMP_STRIDE + CMP_BLOCK - 1)
    n_slc = s // SLC_BLOCK
    cs = np.arange(n_cmp) * CMP_STRIDE
    ss_ = np.arange(n_slc) * SLC_BLOCK
    ov = np.clip(np.minimum(cs[:, None] + CMP_BLOCK, ss_[None, :] + SLC_BLOCK) - np.maximum(cs[:, None], ss_[None, :]), 0, None) / CMP_BLOCK
    cmp_to_slc = jnp.asarray(ov, jnp.float32)
    n_sel = min(SLC_TOPK, n_slc)
    ks_blk = k_slc.transpose(0, 2, 1, 3).reshape(b, G, n_slc, SLC_BLOCK * dk)
    vs_blk = v_slc.transpose(0, 2, 1, 3).reshape(b, G, n_slc, SLC_BLOCK * dk)
    kw = jnp.pad(k_win.transpose(0, 2, 1, 3), ((0, 0), (0, 0), (WINDOW, 0), (0, 0)))
    vw = jnp.pad(v_win.transpose(0, 2, 1, 3), ((0, 0), (0, 0), (WINDOW, 0), (0, 0)))
    qg = q.reshape(b, s, G, HG, dk).transpose(0, 2, 3, 1, 4)
    qrg = q_rot.reshape(b, s, G, HG, dk).transpose(0, 2, 3, 1, 4)
    scale = dk ** -0.5
    blk_id = jnp.arange(n_slc)
    qb_n = NSA_Q_BLOCK

    def block(bi):
        s0 = bi * qb_n
        tq = s0 + jnp.arange(qb_n)
        qb = lax.dynamic_slice_in_dim(qg, s0, qb_n, axis=3)
        qrb = lax.dynamic_slice_in_dim(qrg, s0, qb_n, axis=3)
        sc = jnp.einsum('bgiqd,bgcd->bgiqc', qb, kc).astype(jnp.float32) * scale
        pc = masked_softmax(sc, cmp_end[None, :] <= tq[:, None])
        o_cmp = jnp.einsum('bgiqc,bgcd->bgiqd', pc.astype(vc.dtype), vc)
        imp = jnp.einsum('bgqc,cn->bgqn', pc.sum(axis=2), cmp_to_slc)
        cur = tq // SLC_BLOCK
        forced = (blk_id[None, :] == 0) | (blk_id[None, :] == cur[:, None]) | (blk_id[None, :] == cur[:, None] - 1)
        imp = jnp.where(forced, FORCE, imp)
        imp = jnp.where(blk_id[None, :] <= cur[:, None], imp, NEG)
        top_s, top_i = lax.top_k(imp, n_sel)
        sel_ok = top_s > 0.5 * NEG
        gidx = top_i.reshape(b, G, qb_n * n_sel, 1)
        kg = jnp.take_along_axis(ks_blk, gidx, axis=2).reshape(b, G, qb_n, n_sel * SLC_BLOCK, dk)
        vg = jnp.take_along_axis(vs_blk, gidx, axis=2).reshape(b, G, qb_n, n_sel * SLC_BLOCK, dk)
        tok = top_i[..., None] * SLC_BLOCK + jnp.arange(SLC_BLOCK)
        ms = (sel_ok[..., None] & (tok <= tq[None, None, :, None, None])).reshape(b, G, 1, qb_n, n_sel * SLC_BLOCK)
        sg = jnp.einsum('bgiqd,bgqmd->bgiqm', qrb, kg).astype(jnp.float32) * scale
        ps = masked_softmax(sg, ms)
        o_slc = jnp.einsum('bgiqm,bgqmd->bgiqd', ps.astype(vg.dtype), vg)
        kwb = lax.dynamic_slice_in_dim(kw, s0, qb_n + WINDOW, axis=2)
        vwb = lax.dynamic_slice_in_dim(vw, s0, qb_n + WINDOW, axis=2)
        kp = s0 - WINDOW + jnp.arange(qb_n + WINDOW)
        mw = (kp[None, :] <= tq[:, None]) & (kp[None, :] > tq[:, None] - WINDOW) & (kp[None, :] >= 0)
        sw = jnp.einsum('bgiqd,bgkd->bgiqk', qrb, kwb).astype(jnp.float32) * scale
        pw = masked_softmax(sw, mw)
        o_win = jnp.einsum('bgiqk,bgkd->bgiqd', pw.astype(vwb.dtype), vwb)
        o = jnp.stack([o_cmp, o_slc, o_win], axis=-2)
        return o.transpose(0, 3, 1, 2, 4, 5).reshape(b, qb_n, N_HEADS, 3, dk)

    o = lax.map(block, jnp.arange(s // qb_n))
    o = o.transpose(1, 0, 2, 3, 4, 5).reshape(b, s, N_HEADS, 3, dk)
    o = jnp.einsum('bshc,bshcd->bshd', gates.astype(o.dtype), o)
    return o.reshape(b, s, D_MODEL) @ w_out


def stick_breaking_mixer(h, w_in, w_out):
    b, s, _ = h.shape
    q, k, v = jnp.split(h @ w_in, 3, axis=-1)
    q = q.reshape(b, s, N_HEADS, HEAD_DIM).transpose(0, 2, 1, 3)
    k = k.reshape(b, s, N_HEADS, HEAD_DIM).transpose(0, 2, 1, 3)
    v = v.reshape(b, s, N_HEADS, HEAD_DIM).transpose(0, 2, 1, 3)
    kpos = jnp.arange(s)
    scale = HEAD_DIM ** -0.5

    def block(bi):
        s0 = bi * Q_BLOCK
        qb = lax.dynamic_slice_in_dim(q, s0, Q_BLOCK, axis=2)
        z = jnp.einsum('bhqd,bhkd->bhqk', qb, k).astype(jnp.float32) * scale
        tq = s0 + jnp.arange(Q_BLOCK)
        strict = kpos[None, :] < tq[:, None]
        log_keep = jnp.where(strict, jax.nn.log_sigmoid(-z), 0.0)
        after = lax.cumsum(log_keep, axis=3, reverse=True) - log_keep
        a = jnp.where(strict, jnp.exp(jax.nn.log_sigmoid(z) + after), 0.0)
        return jnp.einsum('bhqk,bhkd->bqhd', a.astype(v.dtype), v)

    o = lax.map(block, jnp.arange(s // Q_BLOCK))
    o = o.transpose(1, 0, 2, 3, 4).reshape(b, s, D_MODEL)
    return o @ w_out


def swiglu(h, w_gate, w_up, w_down):
    return (jax.nn.silu(h @ w_gate) * (h @ w_up)) @ w_down


def moe_swiglu(h, w_router, w_gate, w_up, w_down):
    b, s, d = h.shape
    t = b * s
    hf = h.reshape(t, d)
    logits = (hf @ w_router).astype(jnp.float32)
    top_val, top_idx = lax.top_k(logits, TOP_K)
    gates = jax.nn.softmax(top_val, axis=-1)
    n_assign = t * TOP_K
    flat_e = top_idx.reshape(n_assign)
    flat_tok = jnp.repeat(jnp.arange(t, dtype=jnp.int32), TOP_K)
    order = jnp.argsort(flat_e)
    sorted_e = flat_e[order]
    sorted_tok = flat_tok[order]
    counts = jnp.bincount(flat_e, length=N_EXPERTS)
    padded = (counts + MOE_BLOCK - 1) // MOE_BLOCK * MOE_BLOCK
    pad_end = jnp.cumsum(padded)
    pad_start = pad_end - padded
    grp_start = jnp.cumsum(counts) - counts
    dest = (pad_start[sorted_e] + jnp.arange(n_assign) - grp_start[sorted_e]).astype(jnp.int32)
    n_blocks = -(-n_assign // MOE_BLOCK) + N_EXPERTS
    n_slots = n_blocks * MOE_BLOCK
    slot_tok = jnp.zeros((n_slots,), jnp.int32).at[dest].set(sorted_tok)
    block_e = jnp.minimum(jnp.searchsorted(pad_end, jnp.arange(n_blocks) * MOE_BLOCK, side='right'), N_EXPERTS - 1)
    xs = hf[slot_tok].reshape(n_blocks, MOE_BLOCK, d)

    def expert_block(args):
        xb, e = args
        return (jax.nn.silu(xb @ w_gate[e]) * (xb @ w_up[e])) @ w_down[e]

    ys = lax.map(expert_block, (xs, block_e)).reshape(n_slots, d)
    slot_of_assign = jnp.zeros((n_assign,), jnp.int32).at[order].set(dest)
    y = ys[slot_of_assign].reshape(t, TOP_K, d)
    out = jnp.einsum('tk,tkd->td', gates.astype(y.dtype), y)
    return out.reshape(b, s, d)


def setup_inputs(seed: int = 0) -> dict:
    key = jax.random.key(seed)
    ks = jax.random.split(key, 25)
    D = D_MODEL
    f32 = jnp.float32

    def nrm(k, shape, scale):
        return jax.random.normal(k, shape, f32) * scale

    nsa_in = D + 6 * NSA_GROUPS * HEAD_DIM + 3 * N_HEADS
    out_scale = DEEPNORM_BETA * D ** -0.5
    return {
        'x': nrm(ks[0], (BATCH, SEQ, D), 1.0),
        'hg_w_in': nrm(ks[1], (D, 4 * D), D ** -0.5),
        'hg_lb': nrm(ks[2], (DEPTH + 1, D), 0.1),
        'hg_norm_g': 1.0 + nrm(ks[3], (HEAD_DIM,), 0.01),
        'hg_w_out': nrm(ks[4], (D, D), out_scale),
        'da_w_in': nrm(ks[5], (D, 3 * D), D ** -0.5),
        'da_lam': nrm(ks[6], (4, DA_HALF), 0.1),
        'da_norm_g': 1.0 + nrm(ks[7], (HEAD_DIM,), 0.01),
        'da_w_out': nrm(ks[8], (D, D), out_scale),
        'nsa_w_in': nrm(ks[9], (D, nsa_in), D ** -0.5),
        'nsa_cmp_pe': nrm(ks[10], (2, CMP_BLOCK, HEAD_DIM), 0.02),
        'nsa_cmp_w1': nrm(ks[11], (2, CMP_BLOCK * HEAD_DIM, HEAD_DIM), (CMP_BLOCK * HEAD_DIM) ** -0.5),
        'nsa_cmp_w2': nrm(ks[12], (2, HEAD_DIM, HEAD_DIM), HEAD_DIM ** -0.5),
        'nsa_w_out': nrm(ks[13], (D, D), out_scale),
        'sb_w_in': nrm(ks[14], (D, 3 * D), D ** -0.5),
        'sb_w_out': nrm(ks[15], (D, D), out_scale),
        'ffn_w_gate': nrm(ks[16], (DEPTH // 2, D, D_FF), D ** -0.5),
        'ffn_w_up': nrm(ks[17], (DEPTH // 2, D, D_FF), D ** -0.5),
        'ffn_w_down': nrm(ks[18], (DEPTH // 2, D_FF, D), DEEPNORM_BETA * D_FF ** -0.5),
        'moe_w_router': nrm(ks[19], (DEPTH // 2, D, N_EXPERTS), D ** -0.5),
        'moe_w_gate': nrm(ks[20], (DEPTH // 2, N_EXPERTS, D, D_FF_EXPERT), D ** -0.5),
        'moe_w_up': nrm(ks[21], (DEPTH // 2, N_EXPERTS, D, D_FF_EXPERT), D ** -0.5),
        'moe_w_down': nrm(ks[22], (DEPTH // 2, N_EXPERTS, D_FF_EXPERT, D), DEEPNORM_BETA * D_FF_EXPERT ** -0.5),
        'ln_g': 1.0 + nrm(ks[23], (DEPTH, 2, D), 0.01),
        'ln_b': nrm(ks[24], (DEPTH, 2, D), 0.01),
    }


def reference(x, hg_w_in, hg_lb, hg_norm_g, hg_w_out, da_w_in, da_lam, da_norm_g, da_w_out,
              nsa_w_in, nsa_cmp_pe, nsa_cmp_w1, nsa_cmp_w2, nsa_w_out, sb_w_in, sb_w_out,
              ffn_w_gate, ffn_w_up, ffn_w_down, moe_w_router, moe_w_gate, moe_w_up, moe_w_down,
              ln_g, ln_b):
    pos = jnp.arange(x.shape[1])
    h = x
    for layer in range(DEPTH):
        m = layer % N_MIXERS
        if m == 0:
            y = hgrn2_mixer(h, hg_w_in, hg_lb, hg_norm_g, hg_w_out, layer)
        elif m == 1:
            y = diff_attention_mixer(h, da_w_in, da_lam, da_norm_g, da_w_out, pos, layer)
        elif m == 2:
            y = nsa_mixer(h, nsa_w_in, nsa_cmp_pe, nsa_cmp_w1, nsa_cmp_w2, nsa_w_out, pos)
        else:
            y = stick_breaking_mixer(h, sb_w_in, sb_w_out)
        h = layer_norm(DEEPNORM_ALPHA * h + y.astype(h.dtype), ln_g[layer, 0], ln_b[layer, 0])
        j = layer // 2
        if layer % 2 == 0:
            y = swiglu(h, ffn_w_gate[j], ffn_w_up[j], ffn_w_down[j])
        else:
            y = moe_swiglu(h, moe_w_router[j], moe_w_gate[j], moe_w_up[j], moe_w_down[j])
        h = layer_norm(DEEPNORM_ALPHA * h + y.astype(h.dtype), ln_g[layer, 1], ln_b[layer, 1])
    return h
```

```python
import math
import numpy as np
import concourse.bass as bass
import concourse.mybir as mybir
from concourse.bass_utils import run_bass_kernel_spmd

F32 = mybir.dt.float32
BF16 = mybir.dt.bfloat16
AF = mybir.ActivationFunctionType
ALU = mybir.AluOpType
AX = mybir.AxisListType

D_MODEL = 1024
BATCH = 4
SEQ = 8192
DEPTH = 4
N_HEADS = 8
HEAD_DIM = 128
D_FF = 2816
N_EXPERTS = 8
D_FF_EXPERT = 3584
LN_EPS = 1e-5
ALPHA = (2 * DEPTH) ** 0.25
NCORES = 8
T_ALL = BATCH * SEQ
TC = T_ALL // NCORES


class _Eng:
    def __init__(self, eng, sem, name, self_sync=True):
        self.eng = eng
        self.sem = sem
        self.name = name
        self.count = 0
        self.clock = {}
        self.self_sync = self_sync


class _Queue:
    def __init__(self, eng, sems, name):
        self.eng = eng
        self.sems = [[s, 0] for s in sems]
        self.rr = 0
        self.clock = {}
        self.name = name


class MK:
    def __init__(self, nc, stack, n_dma_sems=6):
        self.nc = nc
        self.stack = stack
        self.sem_names = {}
        self.engs = {}
        for name, eng, ss in (("pe", nc.tensor, False), ("act", nc.scalar, True),
                              ("dve", nc.vector, True), ("pool", nc.gpsimd, True)):
            sem = stack.enter_context(nc.semaphore("s_" + name))
            self.engs[name] = _Eng(eng, sem, name, ss)
        self.queues = {}
        for name, eng in (("sync", nc.sync),):
            sems = [stack.enter_context(nc.semaphore("q_%s%d" % (name, i))) for i in range(n_dma_sems)]
            self.queues[name] = _Queue(eng, sems, name)
        sems = [stack.enter_context(nc.semaphore("q_pool%d" % i)) for i in range(n_dma_sems)]
        self.queues["poolq"] = _Queue(nc.gpsimd, sems, "poolq")
        self.queues["poolq"].clock = self.engs["pool"].clock
        self.last_w = {}
        self.readers = {}
        self.n_ins = 0
        self.out_events = []
        self.cur = stack

    def sb(self, name, shape, dt=F32):
        self.uid = getattr(self, "uid", 0) + 1
        return self.cur.enter_context(self.nc.sbuf_tensor("%s_%d" % (name, self.uid), list(shape), dt))

    def ps(self, name, shape, dt=F32):
        self.uid = getattr(self, "uid", 0) + 1
        return self.cur.enter_context(self.nc.psum_tensor("%s_%d" % (name, self.uid), list(shape), dt))

    def begin_stage(self):
        from contextlib import ExitStack
        self.cur = ExitStack()

    def barrier(self):
        evs = []
        for e in self.engs.values():
            if e.count > 0:
                evs.append((e.sem, e.count))
        for qq in self.queues.values():
            for sem, cnt in qq.sems:
                if cnt > 0:
                    evs.append((sem, cnt))
        streams = [(e.eng, e.clock) for e in self.engs.values()] + [(self.queues["sync"].eng, self.queues["sync"].clock)]
        for eng, clock in streams:
            for sem, val in evs:
                if clock.get(id(sem), 0) < val:
                    eng.wait_ge(sem, val)
                    clock[id(sem)] = val
        self.last_w = {}
        self.readers = {}

    def end_stage(self):
        self.barrier()
        self.cur.close()
        self.cur = self.stack

    def _need(self, reads, writes):
        need = {}

        def add(ev):
            if ev is None:
                return
            k = id(ev[0])
            if k not in need or need[k][1] < ev[1]:
                need[k] = ev

        for k in reads:
            add(self.last_w.get(k))
        for k in writes:
            add(self.last_w.get(k))
            for ev in self.readers.get(k, {}).values():
                add(ev)
        return need

    def _record(self, ev, reads, writes):
        for k in reads:
            self.readers.setdefault(k, {})[id(ev[0])] = ev
        for k in writes:
            self.last_w[k] = ev
            self.readers[k] = {}

    def op(self, engname, fn, reads=(), writes=()):
        e = self.engs[engname]
        need = self._need(reads, writes)
        for k, (sem, val) in need.items():
            if sem is e.sem and not e.self_sync:
                continue
            if e.clock.get(k, 0) < val:
                e.eng.wait_ge(sem, val)
                e.clock[k] = val
        ins = fn(e.eng)
        e.count += 1
        ins.then_inc(e.sem, 1)
        self._record((e.sem, e.count), reads, writes)
        self.n_ins += 1
        return ins

    def dma(self, qname, out, in_, reads=(), writes=(), is_output=False, gather_idx=None):
        q = self.queues[qname]
        slot = q.sems[q.rr % len(q.sems)]
        q.rr += 1
        sem, cnt = slot
        need = self._need(reads, writes)
        if cnt > 0:
            k = id(sem)
            if k not in need or need[k][1] < cnt:
                need[k] = (sem, cnt)
        for k, (s, val) in need.items():
            if q.clock.get(k, 0) < val:
                q.eng.wait_ge(s, val)
                q.clock[k] = val
        if gather_idx is not None:
            ins = q.eng.indirect_dma_start(out=out, out_offset=None, in_=in_,
                                           in_offset=bass.IndirectOffsetOnAxis(ap=gather_idx, axis=0))
        else:
            ins = q.eng.dma_start(out=out, in_=in_)
        slot[1] = cnt + 16
        ins.then_inc(sem, 16)
        ev = (sem, cnt + 16)
        self._record(ev, reads, writes)
        if is_output:
            self.out_events.append(ev)
        self.n_ins += 1
        return ins

    def finish(self):
        q = self.queues["sync"]
        for qq in self.queues.values():
            for sem, cnt in qq.sems:
                if cnt > 0 and q.clock.get(id(sem), 0) < cnt:
                    q.eng.wait_ge(sem, cnt)
                    q.clock[id(sem)] = cnt
        for e in self.engs.values():
            if e.count > 0:
                q.eng.wait_ge(e.sem, e.count)


class IO:
    def __init__(self, nc, given=None):
        self.nc = nc
        self.given = given

    def tin(self, name, shape):
        if self.given is not None:
            return self.given[name]
        return self.nc.dram_tensor(name, list(shape), F32, kind="ExternalInput").ap()

    def tout(self, name, shape):
        if self.given is not None:
            return self.given[name]
        return self.nc.dram_tensor(name, list(shape), F32, kind="ExternalOutput").ap()


def emit_skewed(stages):
    n = len(stages[0])
    ns = len(stages)
    for step in range(n + ns - 1):
        for k in range(ns):
            i = step - (ns - 1 - k) if False else step - k
        for k in range(ns):
            i = step - k
            if 0 <= i < n:
                stages[k][i]()


def emit_loadT(mk, dst, dkey, src, n, tmps, ps, psk, ident, identk="ident"):
    g = 0
    for j0 in range(0, n, 4):
        m = min(4, n - j0)
        tmp, tk = tmps[g % len(tmps)]
        g += 1
        mk.dma("sync", tmp[:, 0:m, :], src[j0 * 128:(j0 + m) * 128, :].rearrange("(j p) d -> p j d", p=128), writes=[tk])
        for j in range(m):
            mk.op("pe", lambda e: e.matmul(ps[:, j * 128:(j + 1) * 128], lhsT=tmp[:, j, :], rhs=ident, start=True, stop=True),
                  reads=[tk, identk], writes=[psk])
        if g % 2 == 0:
            mk.op("act", lambda e: e.copy(out=dst[:, j0 * 128:(j0 + m) * 128], in_=ps[:, 0:m * 128]), reads=[psk], writes=[dkey])
        else:
            mk.op("dve", lambda e: e.tensor_copy(out=dst[:, j0 * 128:(j0 + m) * 128], in_=ps[:, 0:m * 128]), reads=[psk], writes=[dkey])

def emit_ln(mk, z, zk, gt, bt, out, outk, st, stk, n=D_MODEL):
    s1, nm, ss, rs = st[:, 0:1], st[:, 1:2], st[:, 2:3], st[:, 3:4]
    mk.op("dve", lambda e: e.tensor_reduce(out=s1, in_=z, axis=AX.X, op=ALU.add), reads=[zk], writes=[stk])
    mk.op("dve", lambda e: e.tensor_scalar(out=nm, in0=s1, scalar1=-1.0 / n, scalar2=None, op0=ALU.mult),
          reads=[stk], writes=[stk])
    mk.op("dve", lambda e: e.tensor_scalar(out=z, in0=z, scalar1=nm, scalar2=None, op0=ALU.add),
          reads=[stk, zk], writes=[zk])
    mk.op("act", lambda e: e.activation(out=out, in_=z, func=AF.Square, accum_out=ss),
          reads=[zk], writes=[outk, stk])
    mk.op("dve", lambda e: e.tensor_scalar(out=rs, in0=ss, scalar1=1.0 / n, scalar2=LN_EPS, op0=ALU.mult, op1=ALU.add),
          reads=[stk], writes=[stk])
    mk.op("act", lambda e: e.activation(out=rs, in_=rs, func=AF.Sqrt), reads=[stk], writes=[stk])
    mk.op("dve", lambda e: e.reciprocal(out=rs, in_=rs), reads=[stk], writes=[stk])
    mk.op("dve", lambda e: e.scalar_tensor_tensor(out=out, in0=z, scalar=rs, in1=gt, op0=ALU.mult, op1=ALU.mult),
          reads=[stk, zk, "lng"], writes=[outk])
    mk.op("dve", lambda e: e.tensor_tensor(out=out, in0=out, in1=bt, op=ALU.add), reads=[outk, "lnb"], writes=[outk])


def emit_gemm(mk, io, K, N, ln=False, rope_units=None, rope_half=0, ntok=TC, tm=False, gather=False):
    from contextlib import ExitStack
    nc = mk.nc
    KT = K // 128
    if tm:
        a_tok = io.tin("a", [ntok, K])
        ident_d = io.tin("ident", [128, 128])
    else:
        aT = io.tin("aT", [K, ntok])
    w = io.tin("w", [K, N])
    y = io.tout("y", [ntok, N])
    if ln:
        hres = io.tin("hres", [ntok, N])
        lng = io.tin("lng", [128, N])
        lnb = io.tin("lnb", [128, N])
    if rope_units:
        cosd = io.tin("cos", [ntok, rope_half])
        sind = io.tin("sin", [ntok, rope_half])
        yr = io.tout("yr", [ntok, N])
    with ExitStack() as st:
        wt = mk.sb("wt", [128, KT, N])
        wv = w.rearrange("(k p) n -> p k n", p=128)
        for k in range(KT):
            mk.dma("sync", wt[:, k, :], wv[:, k, :], writes=["w%d" % k])
        wkeys = ["w%d" % k for k in range(KT)]
        if ln:
            gt = mk.sb("gt", [128, N])
            bt = mk.sb("bt", [128, N])
            mk.dma("sync", gt[:], lng[:, :], writes=["lng"])
            mk.dma("sync", bt[:], lnb[:, :], writes=["lnb"])
        NB = 2
        at = [mk.sb("at%d" % i, [128, KT, 128]) for i in range(NB)]
        yt = [mk.sb("yt%d" % i, [128, N]) for i in range(NB)]
        if ln:
            ht = [mk.sb("ht%d" % i, [128, N]) for i in range(NB)]
            ot = [mk.sb("ot%d" % i, [128, N]) for i in range(NB)]
            stt = [mk.sb("st%d" % i, [128, 4]) for i in range(NB)]
        if rope_units:
            ct = [mk.sb("ct%d" % i, [128, rope_half]) for i in range(NB)]
            snt = [mk.sb("snt%d" % i, [128, rope_half]) for i in range(NB)]
            rt = [mk.sb("rt%d" % i, [128, N]) for i in range(NB)]
            tmp = [mk.sb("tmp%d" % i, [128, N]) for i in range(NB)]
        NCH = (N + 511) // 512
        pst = [mk.ps("ps%d" % i, [128, 512]) for i in range(4)]
        if tm:
            identt = mk.sb("identt", [128, 128])
            mk.dma("sync", identt[:], ident_d[:, :], writes=["ident"])
            atok = [mk.sb("atok%d" % i, [128, K]) for i in range(NB)]
            if gather:
                idx_sb = mk.sb("idx_sb", [128, ntok // 128], mybir.dt.int32)
                mk.dma("poolq", idx_sb[:], io.tin("tokidx", [128, ntok // 128]), writes=["tokidx"])
        else:
            aTv = aT.rearrange("(k p) t -> p k t", p=128)
        pi = 0
        for tt in range(ntok // 128):
            b = tt % NB
            tsl = slice(tt * 128, (tt + 1) * 128)
            if tm:
                if gather:
                    mk.dma("poolq", atok[b][:], a_tok[:, :], reads=["tokidx"], writes=["atok%d" % b], gather_idx=idx_sb[:, tt:tt + 1])
                else:
                    mk.dma("sync", atok[b][:], a_tok[tsl, :], writes=["atok%d" % b])
                for k0 in range(0, KT, 4):
                    p = pst[pi % 4]
                    pk = "ps%d" % (pi % 4)
                    pi += 1
                    m = min(4, KT - k0)
                    for k in range(k0, k0 + m):
                        mk.op("pe", lambda e: e.matmul(p[:, (k - k0) * 128:(k - k0 + 1) * 128], lhsT=atok[b][:, k * 128:(k + 1) * 128], rhs=identt[:],
                                                       start=True, stop=True), reads=["atok%d" % b, "ident"], writes=[pk])
                    mk.op("dve", lambda e: e.tensor_copy(out=at[b][:, k0:k0 + m, :].rearrange("p a b -> p (a b)"), in_=p[:, 0:m * 128]),
                          reads=[pk], writes=["at%d" % b])
            else:
                mk.dma("sync", at[b][:], aTv[:, :, tsl], writes=["at%d" % b])
            if ln and gather:
                mk.dma("poolq", ht[b][:], hres[:, :], reads=["tokidx"], writes=["ht%d" % b], gather_idx=idx_sb[:, tt:tt + 1])
            elif ln:
                mk.dma("poolq", ht[b][:], hres[tsl, :], writes=["ht%d" % b])
            if rope_units:
                mk.dma("poolq", ct[b][:], cosd[tsl, :], writes=["ct%d" % b])
                mk.dma("poolq", snt[b][:], sind[tsl, :], writes=["snt%d" % b])
            for c in range(NCH):
                c0 = c * 512
                cw = min(512, N - c0)
                p = pst[pi % 4]
                pk = "ps%d" % (pi % 4)
                pi += 1
                for k in range(KT):
                    mk.op("pe", lambda e, k=k, p=p: e.matmul(p[:, :cw], lhsT=at[b][:, k, :], rhs=wt[:, k, c0:c0 + cw],
                                                             start=(k == 0), stop=(k == KT - 1)),
                          reads=["at%d" % b, wkeys[k]], writes=[pk])
                if ln:
                    mk.op("dve", lambda e, p=p: e.scalar_tensor_tensor(out=yt[b][:, c0:c0 + cw], in0=ht[b][:, c0:c0 + cw],
                                                                       scalar=float(ALPHA), in1=p[:, :cw],
                                                                       op0=ALU.mult, op1=ALU.add),
                          reads=[pk, "ht%d" % b], writes=["yt%d" % b])
                else:
                    mk.op("act", lambda e, p=p: e.copy(out=yt[b][:, c0:c0 + cw], in_=p[:, :cw]),
                          reads=[pk], writes=["yt%d" % b])
            if ln:
                emit_ln(mk, yt[b][:], "yt%d" % b, gt[:], bt[:], ot[b][:], "ot%d" % b, stt[b], "st%d" % b, n=N)
                mk.dma("sync", y[tsl, :], ot[b][:], reads=["ot%d" % b], is_output=True)
            else:
                if rope_units:
                    h = rope_half
                    mk.op("pool", lambda e: e.tensor_copy(out=rt[b][:], in_=yt[b][:]), reads=["yt%d" % b], writes=["rt%d" % b])
                    for (u0, nu) in rope_units:
                        src = yt[b][:, u0:u0 + nu * 2 * h].rearrange("p (u two d) -> p u two d", two=2, d=h)
                        dst = rt[b][:, u0:u0 + nu * 2 * h].rearrange("p (u two d) -> p u two d", two=2, d=h)
                        tm = tmp[b][:, u0:u0 + nu * 2 * h].rearrange("p (u two d) -> p u two d", two=2, d=h)
                        cb = ct[b][:].unsqueeze(1).to_broadcast([128, nu, h])
                        sb_ = snt[b][:].unsqueeze(1).to_broadcast([128, nu, h])
                        x1, x2 = src[:, :, 0, :], src[:, :, 1, :]
                        rk = ["yt%d" % b, "ct%d" % b, "snt%d" % b]
                        mk.op("dve", lambda e: e.tensor_tensor(out=dst[:, :, 0, :], in0=x1, in1=cb, op=ALU.mult), reads=rk, writes=["rt%d" % b])
                        mk.op("dve", lambda e: e.tensor_tensor(out=tm[:, :, 0, :], in0=x2, in1=sb_, op=ALU.mult), reads=rk, writes=["tmp%d" % b])
                        mk.op("dve", lambda e: e.tensor_tensor(out=dst[:, :, 0, :], in0=dst[:, :, 0, :], in1=tm[:, :, 0, :], op=ALU.subtract),
                              reads=["tmp%d" % b, "rt%d" % b], writes=["rt%d" % b])
                        mk.op("dve", lambda e: e.tensor_tensor(out=dst[:, :, 1, :], in0=x2, in1=cb, op=ALU.mult), reads=rk, writes=["rt%d" % b])
                        mk.op("dve", lambda e: e.tensor_tensor(out=tm[:, :, 1, :], in0=x1, in1=sb_, op=ALU.mult), reads=rk, writes=["tmp%d" % b])
                        mk.op("dve", lambda e: e.tensor_tensor(out=dst[:, :, 1, :], in0=dst[:, :, 1, :], in1=tm[:, :, 1, :], op=ALU.add),
                              reads=["tmp%d" % b, "rt%d" % b], writes=["rt%d" % b])
                    mk.dma("sync", yr[tsl, :], rt[b][:], reads=["rt%d" % b], is_output=True)
                mk.dma("sync", y[tsl, :], yt[b][:], reads=["yt%d" % b], is_output=True)


def emit_ffn(mk, io, F, NE, ntok=TC, TCH=256, tm=False):
    from contextlib import ExitStack
    nc = mk.nc
    D = D_MODEL
    KT = D // 128
    FT = F // 128
    if tm:
        ident_d = io.tin("ident", [128, 128])
    else:
        hT = io.tin("hT", [D, ntok])
    h = io.tin("h", [ntok, D])
    wg = io.tin("wg", [NE, FT, 128, KT, 128])
    wu = io.tin("wu", [NE, FT, 128, KT, 128])
    wd = io.tin("wd", [NE, FT, 128, D])
    lng = io.tin("lng", [128, D])
    lnb = io.tin("lnb", [128, D])
    if NE > 1:
        wr = io.tin("wr", [D, NE])
    y = io.tout("y", [ntok, D])
    NTT = TCH // 128
    with ExitStack() as st:
        gt = mk.sb("gt", [128, D])
        bt = mk.sb("bt", [128, D])
        mk.dma("sync", gt[:], lng[:, :], writes=["lng"])
        mk.dma("sync", bt[:], lnb[:, :], writes=["lnb"])
        if NE > 1:
            wrt = mk.sb("wrt", [128, KT, NE])
            mk.dma("sync", wrt[:], wr.rearrange("(k p) e -> p k e", p=128), writes=["wr"])
        NB = 2
        hTt = [mk.sb("hTt%d" % i, [128, KT, TCH]) for i in range(NB)]
        ht = [mk.sb("ht%d" % i, [128, NTT, D]) for i in range(NB)]
        wgt = [mk.sb("wgt%d" % i, [128, KT, 128]) for i in range(3)]
        wut = [mk.sb("wut%d" % i, [128, KT, 128]) for i in range(3)]
        wdt = [mk.sb("wdt%d" % i, [128, D]) for i in range(3)]
        sg = [mk.sb("sg%d" % i, [128, TCH]) for i in range(NB)]
        ut = [mk.sb("ut%d" % i, [128, TCH]) for i in range(NB)]
        zt = [mk.sb("zt%d" % i, [128, D]) for i in range(NB)]
        ot = [mk.sb("ot%d" % i, [128, D]) for i in range(NB)]
        stt = [mk.sb("st%d" % i, [128, 4]) for i in range(NB)]
        if NE > 1:
            acc = [mk.sb("acc%d" % i, [128, D]) for i in range(NTT)]
            lg = [mk.sb("lg%d" % i, [128, NE]) for i in range(NTT)]
            mx = [mk.sb("mx%d" % i, [128, 8]) for i in range(NTT)]
            gate = [mk.sb("gate%d" % i, [128, NE]) for i in range(NTT)]
            gs = [mk.sb("gs%d" % i, [128, 2]) for i in range(NTT)]
        psg = [mk.ps("psg%d" % i, [128, 512]) for i in range(2)]
        psu = [mk.ps("psu%d" % i, [128, 512]) for i in range(2)]
        psy = [mk.ps("psy%d" % i, [128, 512]) for i in range(4)]
        if tm:
            identt = mk.sb("identt", [128, 128])
            mk.dma("sync", identt[:], ident_d[:, :], writes=["ident"])
        else:
            hTv = hT.rearrange("(k p) t -> p k t", p=128)
        wi = 0
        for ch in range(ntok // TCH):
            b = ch % NB
            t0 = ch * TCH
            mk.dma("poolq", ht[b][:], h[t0:t0 + TCH, :].rearrange("(j p) d -> p j d", p=128), writes=["ht%d" % b])
            if tm:
                tcount = 0
                for j in range(NTT):
                    for k0 in range(0, KT, 4):
                        p, pk = (psg[tcount % 2], "psg%d" % (tcount % 2)) if (tcount // 2) % 2 == 0 else (psu[tcount % 2], "psu%d" % (tcount % 2))
                        tcount += 1
                        for k in range(k0, k0 + 4):
                            mk.op("pe", lambda e: e.matmul(p[:, (k - k0) * 128:(k - k0 + 1) * 128], lhsT=ht[b][:, j, k * 128:(k + 1) * 128], rhs=identt[:],
                                                           start=True, stop=True), reads=["ht%d" % b, "ident"], writes=[pk])
                        for k in range(k0, k0 + 4):
                            mk.op("dve", lambda e: e.tensor_copy(out=hTt[b][:, k, j * 128:(j + 1) * 128], in_=p[:, (k - k0) * 128:(k - k0 + 1) * 128]),
                                  reads=[pk], writes=["hTt%d" % b])
            else:
                mk.dma("sync", hTt[b][:], hTv[:, :, t0:t0 + TCH], writes=["hTt%d" % b])
            if NE > 1:
                for j in range(NTT):
                    p = psg[0]
                    for k in range(KT):
                        mk.op("pe", lambda e, k=k, j=j: e.matmul(p[:, :NE], lhsT=hTt[b][:, k, j * 128:(j + 1) * 128], rhs=wrt[:, k, :],
                                                                 start=(k == 0), stop=(k == KT - 1)),
                              reads=["hTt%d" % b, "wr"], writes=["psg0"])
                    mk.op("dve", lambda e, j=j: e.tensor_copy(out=lg[j][:], in_=p[:, :NE]), reads=["psg0"], writes=["lg%d" % j])
                    mk.op("dve", lambda e, j=j: e.max(out=mx[j][:], in_=lg[j][:]), reads=["lg%d" % j], writes=["mx%d" % j])
                    mk.op("dve", lambda e, j=j: e.tensor_scalar(out=gs[j][:, 0:1], in0=mx[j][:, 0:1], scalar1=-1.0, scalar2=None, op0=ALU.mult),
                          reads=["mx%d" % j], writes=["gs%d" % j])
                    mk.op("act", lambda e, j=j: e.activation(out=gate[j][:], in_=lg[j][:], func=AF.Exp, bias=gs[j][:, 0:1], scale=1.0),
                          reads=["lg%d" % j, "gs%d" % j], writes=["gate%d" % j])
                    mk.op("act", lambda e, j=j: e.activation(out=gs[j][:, 1:2], in_=mx[j][:, 1:2], func=AF.Exp, bias=gs[j][:, 0:1], scale=1.0),
                          reads=["mx%d" % j, "gs%d" % j], writes=["gs%d" % j])
                    mk.op("dve", lambda e, j=j: e.tensor_scalar(out=gs[j][:, 1:2], in0=gs[j][:, 1:2], scalar1=1.0, scalar2=None, op0=ALU.add),
                          reads=["gs%d" % j], writes=["gs%d" % j])
                    mk.op("dve", lambda e, j=j: e.reciprocal(out=gs[j][:, 1:2], in_=gs[j][:, 1:2]), reads=["gs%d" % j], writes=["gs%d" % j])
                    mk.op("dve", lambda e, j=j: e.tensor_scalar(out=lg[j][:], in0=lg[j][:], scalar1=mx[j][:, 1:2], scalar2=gs[j][:, 1:2],
                                                                op0=ALU.is_ge, op1=ALU.mult),
                          reads=["lg%d" % j, "mx%d" % j, "gs%d" % j], writes=["lg%d" % j])
                    mk.op("dve", lambda e, j=j: e.tensor_tensor(out=gate[j][:], in0=gate[j][:], in1=lg[j][:], op=ALU.mult),
                          reads=["lg%d" % j, "gate%d" % j], writes=["gate%d" % j])
            stL, stA, stB = [], [], []
            for ex in range(NE):
                for f in range(FT):
                    def mk_item(ex=ex, f=f, wi=wi):
                        w3 = wi % 3
                        wb = wi % NB
                        pg, pu = psg[wb], psu[wb]

                        def L():
                            mk.dma("sync", wgt[w3][:], wg[ex, f], writes=["wgt%d" % w3])
                            mk.dma("poolq", wut[w3][:], wu[ex, f], writes=["wut%d" % w3])
                            mk.dma("sync", wdt[w3][:], wd[ex, f], writes=["wdt%d" % w3])

                        def A():
                            for k in range(KT):
                                mk.op("pe", lambda e, k=k: e.matmul(pg[:, :TCH], lhsT=wgt[w3][:, k, :], rhs=hTt[b][:, k, :],
                                                                    start=(k == 0), stop=(k == KT - 1)),
                                      reads=["wgt%d" % w3, "hTt%d" % b], writes=["psg%d" % wb])
                            for k in range(KT):
                                mk.op("pe", lambda e, k=k: e.matmul(pu[:, :TCH], lhsT=wut[w3][:, k, :], rhs=hTt[b][:, k, :],
                                                                    start=(k == 0), stop=(k == KT - 1)),
                                      reads=["wut%d" % w3, "hTt%d" % b], writes=["psu%d" % wb])
                            mk.op("act", lambda e: e.activation(out=sg[wb][:], in_=pg[:, :TCH], func=AF.Silu),
                                  reads=["psg%d" % wb], writes=["sg%d" % wb])
                            mk.op("dve", lambda e: e.tensor_tensor(out=ut[wb][:], in0=sg[wb][:], in1=pu[:, :TCH], op=ALU.mult),
                                  reads=["sg%d" % wb, "psu%d" % wb], writes=["ut%d" % wb])

                        def B():
                            for j in range(NTT):
                                for nh in range(2):
                                    mk.op("pe", lambda e, j=j, nh=nh: e.matmul(psy[j * 2 + nh][:, :], lhsT=ut[wb][:, j * 128:(j + 1) * 128],
                                                                               rhs=wdt[w3][:, nh * 512:(nh + 1) * 512],
                                                                               start=(f == 0), stop=(f == FT - 1)),
                                          reads=["ut%d" % wb, "wdt%d" % w3], writes=["psy%d" % (j * 2 + nh)])
                            if NE > 1 and f == FT - 1:
                                for j in range(NTT):
                                    for nh in range(2):
                                        sl = slice(nh * 512, (nh + 1) * 512)
                                        if ex == 0:
                                            mk.op("dve", lambda e, j=j, nh=nh, sl=sl: e.tensor_scalar(out=acc[j][:, sl], in0=psy[j * 2 + nh][:, :],
                                                                                                      scalar1=gate[j][:, ex:ex + 1], scalar2=None, op0=ALU.mult),
                                                  reads=["psy%d" % (j * 2 + nh), "gate%d" % j], writes=["acc%d" % j])
                                        else:
                                            mk.op("dve", lambda e, j=j, nh=nh, sl=sl: e.scalar_tensor_tensor(out=acc[j][:, sl], in0=psy[j * 2 + nh][:, :],
                                                                                                             scalar=gate[j][:, ex:ex + 1], in1=acc[j][:, sl],
                                                                                                             op0=ALU.mult, op1=ALU.add),
                                                  reads=["psy%d" % (j * 2 + nh), "gate%d" % j, "acc%d" % j], writes=["acc%d" % j])
                        return L, A, B
                    l_, a_, b_ = mk_item()
                    wi += 1
                    stL.append(l_)
                    stA.append(a_)
                    stB.append(b_)
            stAB = [(lambda a=a, b=b: (a(), b())) for a, b in zip(stA, stB)]
            emit_skewed([stL, [(lambda: None)] * len(stL), stAB])
            for j in range(NTT):
                zb = (ch * NTT + j) % NB
                for nh in range(2):
                    sl = slice(nh * 512, (nh + 1) * 512)
                    if NE > 1:
                        mk.op("dve", lambda e, sl=sl: e.scalar_tensor_tensor(out=zt[zb][:, sl], in0=ht[b][:, j, sl], scalar=float(ALPHA),
                                                                             in1=acc[j][:, sl], op0=ALU.mult, op1=ALU.add),
                              reads=["ht%d" % b, "acc%d" % j], writes=["zt%d" % zb])
                    else:
                        mk.op("dve", lambda e, sl=sl, nh=nh: e.scalar_tensor_tensor(out=zt[zb][:, sl], in0=ht[b][:, j, sl], scalar=float(ALPHA),
                                                                                    in1=psy[j * 2 + nh][:, :], op0=ALU.mult, op1=ALU.add),
                              reads=["ht%d" % b, "psy%d" % (j * 2 + nh)], writes=["zt%d" % zb])
                emit_ln(mk, zt[zb][:], "zt%d" % zb, gt[:], bt[:], ot[zb][:], "ot%d" % zb, stt[zb], "st%d" % zb)
                mk.dma("sync", y[t0 + j * 128:t0 + (j + 1) * 128, :], ot[zb][:], reads=["ot%d" % zb], is_output=True)


def pretile_w_in(w, F):
    D = w.shape[0]
    return np.ascontiguousarray(w.reshape(D // 128, 128, F // 128, 128).transpose(2, 1, 0, 3))


def run(nc, in_maps):
    res = run_bass_kernel_spmd(nc, in_maps, core_ids=list(range(len(in_maps))))
    return res.results


def attn_consts():
    p = np.arange(128)
    tri_incl = (p[None, :] >= p[:, None]).astype(np.float32)
    tri_strict = (p[None, :] > p[:, None]).astype(np.float32)
    U = (p[:, None] > p[None, :]).astype(np.float32)
    ones = np.ones((128, 128), np.float32)
    return np.ascontiguousarray(np.stack([tri_incl, tri_strict, U, ones], 1))


def emit_attn(mk, io, kind, NH=4, S=SEQ, lam_init=0.0, tm=False):
    from contextlib import ExitStack
    nc = mk.nc
    QC = 512
    NQC = S // QC
    NKT = S // 128
    if tm:
        q_tok = io.tin("q_tok", [S, NH * 128])
        k_tok = io.tin("k_tok", [S, NH * 128])
        v_tok = io.tin("v_tok", [S, NH * 128])
        ident_d = io.tin("ident", [128, 128])
    else:
        qT = io.tin("qT", [NH, 128, S])
        kT = io.tin("kT", [NH, 128, S])
        v = io.tin("v", [NH, S, 128])
    cst = io.tin("cst", [128, 4, 128])
    o = io.tout("o", [S, NH * 128])
    if kind == "da":
        lam = io.tin("lam", [128, 256])
        gn = io.tin("gn", [128, 128])
    VW = 132 if kind == "da" else 128
    scale = (64 ** -0.5) if kind == "da" else (128 ** -0.5)
    with ExitStack() as st:
        ct = mk.sb("ct", [128, 4, 128])
        mk.dma("sync", ct[:], cst[:, :, :], writes=["cst"])
        tri_incl, tri_strict, U, ones = ct[:, 0, :], ct[:, 1, :], ct[:, 2, :], ct[:, 3, :]
        kt_sb = mk.sb("kt_sb", [128, S])
        if tm:
            identt = mk.sb("identt", [128, 128])
            mk.dma("sync", identt[:], ident_d[:, :], writes=["ident"])
            tmps = [(mk.sb("ltmp%d" % i, [128, 4, 128]), "ltmp%d" % i) for i in range(2)]
            psTr = mk.ps("psTr", [128, 512])
        v_sb = mk.sb("v_sb", [128, NKT, VW])
        NB = 2
        q_sb = [mk.sb("q_sb%d" % i, [128, QC]) for i in range(NB)]
        pT = [mk.sb("pT%d" % i, [128, QC]) for i in range(3 if kind == "sb" else 6)]
        NS = 3 if kind == "sb" else 4
        psS = [mk.ps("psS%d" % i, [128, 512]) for i in range(NS)]
        o_sb = [mk.sb("o_sb%d" % i, [128, 4, 128]) for i in range(NB)]
        if kind == "da":
            lt = mk.sb("lt", [128, 256])
            gnt = mk.sb("gnt", [128, 128])
            lsc = mk.sb("lsc", [128, 8])
            mk.dma("sync", lt[:], lam[:, :], writes=["lam"])
            mk.dma("sync", gnt[:], gn[:, :], writes=["gn"])
            tmpl = mk.sb("tmpl", [128, 128])
            mk.op("dve", lambda e: e.tensor_tensor(out=tmpl[:, 0:64], in0=lt[:, 0:64], in1=lt[:, 64:128], op=ALU.mult), reads=["lam"], writes=["tmpl"])
            mk.op("dve", lambda e: e.tensor_tensor(out=tmpl[:, 64:128], in0=lt[:, 128:192], in1=lt[:, 192:256], op=ALU.mult), reads=["lam"], writes=["tmpl"])
            mk.op("dve", lambda e: e.tensor_reduce(out=lsc[:, 0:1], in_=tmpl[:, 0:64], axis=AX.X, op=ALU.add), reads=["tmpl"], writes=["lsc"])
            mk.op("dve", lambda e: e.tensor_reduce(out=lsc[:, 1:2], in_=tmpl[:, 64:128], axis=AX.X, op=ALU.add), reads=["tmpl"], writes=["lsc"])
            mk.op("act", lambda e: e.activation(out=lsc[:, 2:4], in_=lsc[:, 0:2], func=AF.Exp), reads=["lsc"], writes=["lsc"])
            mk.op("dve", lambda e: e.scalar_tensor_tensor(out=lsc[:, 4:5], in0=lsc[:, 3:4], scalar=-float(lam_init), in1=lsc[:, 2:3],
                                                          op0=ALU.add, op1=ALU.subtract), reads=["lsc"], writes=["lsc"])
            neglmb = lsc[:, 4:5]
            acc = [mk.ps("acc%d" % i, [128, 512]) for i in range(3)]
            fin = [mk.sb("fin%d" % i, [128, 8]) for i in range(NB)]
            o1 = [mk.sb("o1_%d" % i, [128, 128]) for i in range(NB)]
            o2 = [mk.sb("o2_%d" % i, [128, 128]) for i in range(NB)]
            sq = [mk.sb("sq%d" % i, [128, 128]) for i in range(NB)]

            def acc_ap(c, jq):
                if jq < 3:
                    return acc[c][:, jq * VW:(jq + 1) * VW], "acc%d" % c
                return acc[2][:, c * VW:(c + 1) * VW], "acc2"
        else:
            psA = [mk.ps("psA%d" % i, [128, 512]) for i in range(2)]
            acc1 = [mk.ps("accs%d" % i, [128, 512]) for i in range(2)]
            E_sb = [mk.sb("E%d" % i, [128, QC]) for i in range(3)]
            sp_sb = [mk.sb("sp%d" % i, [128, QC]) for i in range(3)]
            tm_sb = [mk.sb("tm%d" % i, [128, QC]) for i in range(3)]
            C_sb = mk.sb("C_sb", [128, QC])
        it = 0
        for hd in range(NH):
            hcs = slice(hd * 128, (hd + 1) * 128)
            if tm:
                emit_loadT(mk, kt_sb[:], "kt_sb", k_tok[:, hcs], NKT, tmps, psTr, "psTr", identt[:])
            else:
                for c4 in range(4):
                    sl = slice(c4 * (S // 4), (c4 + 1) * (S // 4))
                    mk.dma("sync", kt_sb[:, sl], kT[hd, :, sl], writes=["kt_sb"])
            for c4 in range(4):
                k0, k1 = c4 * (NKT // 4), (c4 + 1) * (NKT // 4)
                vsrc = v_tok[k0 * 128:k1 * 128, hcs] if tm else v[hd, k0 * 128:k1 * 128, :]
                mk.dma("poolq", v_sb[:, k0:k1, 0:128], vsrc.rearrange("(kt p) d -> p kt d", p=128), writes=["v_sb"])
            if kind == "da" and hd == 0:
                mk.op("dve", lambda e: e.memset(v_sb[:, :, 128:VW], 1.0), writes=["v_sb"])
            for qc in range(NQC):
                qb = (hd * NQC + qc) % NB
                if tm:
                    emit_loadT(mk, q_sb[qb][:], "q_sb%d" % qb, q_tok[qc * QC:(qc + 1) * QC, hcs], QC // 128, tmps, psTr, "psTr", identt[:])
                else:
                    mk.dma("sync", q_sb[qb][:], qT[hd, :, qc * QC:(qc + 1) * QC], writes=["q_sb%d" % qb])
                nkt = 4 * qc + 4
                ob = (hd * NQC + qc) % NB
                if kind == "da":
                    stA, stB = [], []
                    for kt in range(nkt):
                        def mk_item(kt=kt, it=it):
                            r = max(0, kt - 4 * qc)
                            c0 = 128 * r
                            bufs = []
                            for c in range(2):
                                bi = (it % 2) * 2 + c
                                pi_ = (it % 3) * 2 + c
                                bufs.append((psS[bi], "psS%d" % bi, pT[pi_], "pT%d" % pi_, slice(c * 64, (c + 1) * 64)))

                            def A():
                                for (pS, pk, pt, ptk, ps_) in bufs:
                                    mk.op("pe", lambda e: e.matmul(pS[:, c0:QC], lhsT=kt_sb[ps_, kt * 128:(kt + 1) * 128], rhs=q_sb[qb][ps_, c0:QC],
                                                                   start=True, stop=True),
                                          reads=["kt_sb", "q_sb%d" % qb], writes=[pk])
                                for (pS, pk, pt, ptk, ps_) in bufs:
                                    mk.op("act", lambda e: e.activation(out=pt[:, c0:QC], in_=pS[:, c0:QC], func=AF.Exp, scale=float(scale)),
                                          reads=[pk], writes=[ptk])
                                    if kt >= 4 * qc:
                                        mk.op("pool", lambda e: e.tensor_tensor(out=pt[:, c0:c0 + 128], in0=pt[:, c0:c0 + 128], in1=tri_incl, op=ALU.mult),
                                              reads=[ptk, "cst"], writes=[ptk])

                            def B():
                                for c, (pS, pk, pt, ptk, ps_) in enumerate(bufs):
                                    for jq in range(r, 4):
                                        ap, ak = acc_ap(c, jq)
                                        first = (kt == 0 and jq == 0) or (kt == 0 and jq == 3 and c == 0)
                                        mk.op("pe", lambda e: e.matmul(ap, lhsT=pt[:, jq * 128:(jq + 1) * 128], rhs=v_sb[:, kt, :],
                                                                       start=first, stop=(kt == 4 * qc + jq), skip_group_check=True),
                                              reads=[ptk, "v_sb"], writes=[ak])
                            return A, B
                        a_, b_ = mk_item()
                        it += 1
                        stA.append(a_)
                        stB.append(b_)
                    emit_skewed([stA, [(lambda: None)] * len(stA), stB])
                    for jq in range(4):
                        fb = (qc * 4 + jq) % NB
                        fk = "fin%d" % fb
                        a0, a0k = acc_ap(0, jq)
                        a1, a1k = acc_ap(1, jq)
                        mk.op("dve", lambda e: e.reciprocal(out=fin[fb][:, 0:1], in_=a0[:, 128:129]), reads=[a0k], writes=[fk])
                        mk.op("dve", lambda e: e.reciprocal(out=fin[fb][:, 1:2], in_=a1[:, 128:129]), reads=[a1k], writes=[fk])
                        mk.op("dve", lambda e: e.tensor_tensor(out=fin[fb][:, 1:2], in0=fin[fb][:, 1:2], in1=neglmb, op=ALU.mult),
                              reads=[fk, "lsc"], writes=[fk])
                        mk.op("dve", lambda e: e.tensor_scalar(out=o1[fb][:], in0=a0[:, 0:128], scalar1=fin[fb][:, 0:1], scalar2=None, op0=ALU.mult),
                              reads=[a0k, fk], writes=["o1_%d" % fb])
                        mk.op("dve", lambda e: e.scalar_tensor_tensor(out=o2[fb][:], in0=a1[:, 0:128], scalar=fin[fb][:, 1:2], in1=o1[fb][:],
                                                                      op0=ALU.mult, op1=ALU.add),
                              reads=[a1k, fk, "o1_%d" % fb], writes=["o2_%d" % fb])
                        mk.op("act", lambda e: e.activation(out=sq[fb][:], in_=o2[fb][:], func=AF.Square, accum_out=fin[fb][:, 2:3]),
                              reads=["o2_%d" % fb], writes=["sq%d" % fb, fk])
                        mk.op("dve", lambda e: e.tensor_scalar(out=fin[fb][:, 3:4], in0=fin[fb][:, 2:3], scalar1=1.0 / 128, scalar2=LN_EPS,
                                                               op0=ALU.mult, op1=ALU.add), reads=[fk], writes=[fk])
                        mk.op("act", lambda e: e.activation(out=fin[fb][:, 3:4], in_=fin[fb][:, 3:4], func=AF.Sqrt), reads=[fk], writes=[fk])
                        mk.op("dve", lambda e: e.reciprocal(out=fin[fb][:, 3:4], in_=fin[fb][:, 3:4]), reads=[fk], writes=[fk])
                        mk.op("dve", lambda e: e.tensor_scalar(out=fin[fb][:, 3:4], in0=fin[fb][:, 3:4], scalar1=float(1.0 - lam_init), scalar2=None,
                                                               op0=ALU.mult), reads=[fk], writes=[fk])
                        mk.op("dve", lambda e: e.scalar_tensor_tensor(out=o_sb[ob][:, jq, :], in0=o2[fb][:], scalar=fin[fb][:, 3:4], in1=gnt[:],
                                                                      op0=ALU.mult, op1=ALU.mult),
                              reads=["o2_%d" % fb, fk, "gn"], writes=["o_sb%d" % ob])
                else:
                    ac = acc1[(hd * NQC + qc) % 2]
                    ack = "accs%d" % ((hd * NQC + qc) % 2)
                    mk.op("pool", lambda e: e.memset(C_sb[:], 0.0), writes=["C_sb"])
                    stA, stB, stC = [], [], []
                    for kt in range(nkt - 1, -1, -1):
                        def mk_pair(kt=kt, it=it):
                            r = max(0, kt - 4 * qc)
                            c0 = 128 * r
                            diag = kt >= 4 * qc
                            pS = psS[it % NS]
                            pk = "psS%d" % (it % NS)
                            pA = psA[it % 2]
                            pak = "psA%d" % (it % 2)
                            pt = pT[it % 3]
                            ptk = "pT%d" % (it % 3)
                            Eb, Ek = E_sb[it % 3], "E%d" % (it % 3)
                            spb, spk = sp_sb[it % 3], "sp%d" % (it % 3)
                            tmb, tmk = tm_sb[it % 3], "tm%d" % (it % 3)

                            def A():
                                mk.op("pe", lambda e: e.matmul(pS[:, c0:QC], lhsT=kt_sb[:, kt * 128:(kt + 1) * 128], rhs=q_sb[qb][:, c0:QC],
                                                               start=True, stop=True),
                                      reads=["kt_sb", "q_sb%d" % qb], writes=[pk])
                                mk.op("act", lambda e: e.activation(out=Eb[:, c0:QC], in_=pS[:, c0:QC], func=AF.Exp, scale=float(scale)),
                                      reads=[pk], writes=[Ek])
                                mk.op("act", lambda e: e.activation(out=spb[:, c0:QC], in_=Eb[:, c0:QC], func=AF.Ln, bias=1.0, scale=1.0),
                                      reads=[Ek], writes=[spk])
                                if diag:
                                    mk.op("pool", lambda e: e.tensor_tensor(out=spb[:, c0:c0 + 128], in0=spb[:, c0:c0 + 128], in1=tri_strict, op=ALU.mult),
                                          reads=[spk, "cst"], writes=[spk])

                            def B():
                                mk.op("pe", lambda e: e.matmul(pA[:, c0:QC], lhsT=U, rhs=spb[:, c0:QC], start=True, stop=False),
                                      reads=[spk, "cst"], writes=[pak])
                                mk.op("pe", lambda e: e.matmul(pA[:, c0:QC], lhsT=ones, rhs=C_sb[:, c0:QC], start=False, stop=True),
                                      reads=["C_sb", "cst"], writes=[pak])
                                mk.op("pool", lambda e: e.tensor_tensor(out=C_sb[:, c0:QC], in0=C_sb[:, c0:QC], in1=spb[:, c0:QC], op=ALU.add),
                                      reads=["C_sb", spk], writes=["C_sb"])
                                mk.op("dve", lambda e: e.scalar_tensor_tensor(out=tmb[:, c0:QC], in0=pS[:, c0:QC], scalar=float(scale), in1=spb[:, c0:QC],
                                                                              op0=ALU.mult, op1=ALU.subtract),
                                      reads=[pk, spk], writes=[tmk])
                                mk.op("dve", lambda e: e.tensor_tensor(out=tmb[:, c0:QC], in0=tmb[:, c0:QC], in1=pA[:, c0:QC], op=ALU.subtract),
                                      reads=[tmk, pak], writes=[tmk])
                                mk.op("act", lambda e: e.activation(out=pt[:, c0:QC], in_=tmb[:, c0:QC], func=AF.Exp), reads=[tmk], writes=[ptk])
                                if diag:
                                    mk.op("pool", lambda e: e.tensor_tensor(out=pt[:, c0:c0 + 128], in0=pt[:, c0:c0 + 128], in1=tri_strict, op=ALU.mult),
                                          reads=[ptk, "cst"], writes=[ptk])

                            def C():
                                for jq in range(r, 4):
                                    mk.op("pe", lambda e: e.matmul(ac[:, jq * 128:(jq + 1) * 128], lhsT=pt[:, jq * 128:(jq + 1) * 128], rhs=v_sb[:, kt, :],
                                                                   start=(kt == nkt - 1), stop=(kt == 0), skip_group_check=True),
                                          reads=[ptk, "v_sb"], writes=[ack])
                            return A, B, C
                        a_, b_, c_ = mk_pair()
                        it += 1
                        stA.append(a_)
                        stB.append(b_)
                        stC.append(c_)
                    emit_skewed([stA, stB, stC])
                    mk.op("act", lambda e: e.copy(out=o_sb[ob][:].rearrange("p a b -> p (a b)"), in_=ac[:, :]), reads=[ack], writes=["o_sb%d" % ob])
                mk.dma("sync", o[qc * QC:(qc + 1) * QC, hd * 128:(hd + 1) * 128].rearrange("(j p) d -> p j d", p=128), o_sb[ob][:],
                       reads=["o_sb%d" % ob], is_output=True)


def hg_consts(W):
    p = np.arange(128)
    m = np.ones((128, W), np.float32)
    m[:, ::64] = 0.0
    tri = (p[None, :] >= p[:, None]).astype(np.float32)
    ident = np.eye(128, dtype=np.float32)
    return m, np.ascontiguousarray(np.stack([tri, ident], 1))


def emit_hgrn(mk, io, NH=4, S=SEQ, layer=0, W=2048, tm=False):
    from contextlib import ExitStack
    nc = mk.nc
    C = 64
    NW = S // W
    NCW = W // C
    if tm:
        q_tok = io.tin("q_tok", [S, NH * 128])
        f_tok = io.tin("f_tok", [S, NH * 128])
        v_tok = io.tin("v_tok", [S, NH * 128])
        g_tok = io.tin("g_tok", [S, NH * 128])
    else:
        qT = io.tin("qT", [NH, 128, S])
        fT = io.tin("fT", [NH, 128, S])
        v = io.tin("v", [NH, S, 128])
        g = io.tin("g", [NH, S, 128])
    lbT = io.tin("lbT", [NH, 128, 5])
    gn = io.tin("gn", [128, 128])
    msk = io.tin("msk", [128, W])
    cst = io.tin("cst", [128, 2, 128])
    o = io.tout("o", [S, NH * 128])
    with ExitStack() as st:
        ct = mk.sb("ct", [128, 2, 128])
        mt = mk.sb("mt", [128, W])
        gnt = mk.sb("gnt", [128, 128])
        mk.dma("sync", ct[:], cst[:, :, :], writes=["cst"])
        mk.dma("sync", mt[:], msk[:, :], writes=["msk"])
        mk.dma("sync", gnt[:], gn[:, :], writes=["gn"])
        tri, ident = ct[:, 0, :], ct[:, 1, :]
        lbt = mk.sb("lbt", [128, 5])
        lbs = mk.sb("lbs", [128, 8])
        q_sb = mk.sb("q_sb", [128, W])
        f_sb = mk.sb("f_sb", [128, W])
        kk_sb = mk.sb("kk_sb", [128, W])
        cum_sb = mk.sb("cum_sb", [128, W])
        E_sb = mk.sb("E_sb", [128, W])
        En_sb = mk.sb("En_sb", [128, W])
        qt_sb = mk.sb("qt_sb", [128, W])
        kt_sb = mk.sb("kt_sb", [128, W])
        v_sb = mk.sb("v_sb", [64, NCW, 128])
        g_sb = mk.sb("g_sb", [64, NCW, 128])
        o_sb = mk.sb("o_sb", [64, NCW, 128])
        state = [mk.sb("state%d" % i, [128, 128]) for i in range(2)]
        st1 = mk.sb("st1", [128, 128])
        ktok = [mk.sb("ktok%d" % i, [64, 128]) for i in range(2)]
        AT = [mk.sb("AT%d" % i, [64, 64]) for i in range(2)]
        sq = [mk.sb("sq%d" % i, [64, 128]) for i in range(2)]
        fin = [mk.sb("fin%d" % i, [64, 4]) for i in range(2)]
        psT = [mk.ps("psT%d" % i, [128, 512]) for i in range(2)]
        psS = [mk.ps("psS%d" % i, [128, 512]) for i in range(2)]
        psO = [mk.ps("psO%d" % i, [128, 512]) for i in range(2)]
        if tm:
            psD0 = mk.ps("psD0", [128, 512])
            psD = [psD0, psD0]
            psTr = mk.ps("psTr", [128, 512])
            tmps = [(mk.sb("ltmp%d" % i, [128, 4, 128]), "ltmp%d" % i) for i in range(2)]
        else:
            psD = [mk.ps("psD%d" % i, [128, 512]) for i in range(2)]
        NPD = 1 if tm else 2
        it = 0
        for hd in range(NH):
            mk.dma("sync", lbt[:], lbT[hd], writes=["lbt"])
            mk.op("act", lambda e: e.activation(out=lbt[:], in_=lbt[:], func=AF.Exp), reads=["lbt"], writes=["lbt"])
            mk.op("dve", lambda e: e.tensor_reduce(out=lbs[:, 0:1], in_=lbt[:], axis=AX.X, op=ALU.add), reads=["lbt"], writes=["lbs"])
            mk.op("dve", lambda e: e.tensor_reduce(out=lbs[:, 1:2], in_=lbt[:, 0:layer + 1], axis=AX.X, op=ALU.add), reads=["lbt"], writes=["lbs"])
            mk.op("dve", lambda e: e.reciprocal(out=lbs[:, 0:1], in_=lbs[:, 0:1]), reads=["lbs"], writes=["lbs"])
            mk.op("dve", lambda e: e.tensor_tensor(out=lbs[:, 2:3], in0=lbs[:, 1:2], in1=lbs[:, 0:1], op=ALU.mult), reads=["lbs"], writes=["lbs"])
            mk.op("dve", lambda e: e.tensor_scalar(out=lbs[:, 3:4], in0=lbs[:, 2:3], scalar1=-1.0, scalar2=1.0, op0=ALU.mult, op1=ALU.add),
                  reads=["lbs"], writes=["lbs"])
            lb, oml = lbs[:, 2:3], lbs[:, 3:4]
            mk.op("dve", lambda e: e.memset(state[0][:], 0.0), writes=["state0"])
            for w in range(NW):
                t0 = w * W
                hcs = slice(hd * 128, (hd + 1) * 128)
                if tm:
                    emit_loadT(mk, q_sb[:], "q_sb", q_tok[t0:t0 + W, hcs], W // 128, tmps, psTr, "psTr", ident, identk="cst")
                    emit_loadT(mk, f_sb[:], "f_sb", f_tok[t0:t0 + W, hcs], W // 128, tmps, psTr, "psTr", ident, identk="cst")
                    mk.dma("poolq", v_sb[:], v_tok[t0:t0 + W, hcs].rearrange("(c p) d -> p c d", p=64), writes=["v_sb"])
                    mk.dma("poolq", g_sb[:], g_tok[t0:t0 + W, hcs].rearrange("(c p) d -> p c d", p=64), writes=["g_sb"])
                else:
                    mk.dma("sync", q_sb[:], qT[hd, :, t0:t0 + W], writes=["q_sb"])
                    mk.dma("sync", f_sb[:], fT[hd, :, t0:t0 + W], writes=["f_sb"])
                    mk.dma("poolq", v_sb[:], v[hd, t0:t0 + W, :].rearrange("(c p) d -> p c d", p=64), writes=["v_sb"])
                    mk.dma("poolq", g_sb[:], g[hd, t0:t0 + W, :].rearrange("(c p) d -> p c d", p=64), writes=["g_sb"])
                mk.op("act", lambda e: e.activation(out=f_sb[:], in_=f_sb[:], func=AF.Sigmoid), reads=["f_sb"], writes=["f_sb"])
                mk.op("dve", lambda e: e.tensor_scalar(out=f_sb[:], in0=f_sb[:], scalar1=oml, scalar2=lb, op0=ALU.mult, op1=ALU.add),
                      reads=["f_sb", "lbs"], writes=["f_sb"])
                mk.op("dve", lambda e: e.tensor_scalar(out=kk_sb[:], in0=f_sb[:], scalar1=-1.0, scalar2=1.0, op0=ALU.mult, op1=ALU.add),
                      reads=["f_sb"], writes=["kk_sb"])
                mk.op("act", lambda e: e.activation(out=f_sb[:], in_=f_sb[:], func=AF.Ln), reads=["f_sb"], writes=["f_sb"])
                mk.op("dve", lambda e: e.tensor_tensor_scan(out=cum_sb[:], data0=mt[:], data1=f_sb[:], initial=0.0, op0=ALU.mult, op1=ALU.add),
                      reads=["f_sb", "msk"], writes=["cum_sb"])
                mk.op("act", lambda e: e.activation(out=E_sb[:], in_=cum_sb[:], func=AF.Exp), reads=["cum_sb"], writes=["E_sb"])
                mk.op("act", lambda e: e.activation(out=En_sb[:], in_=cum_sb[:], func=AF.Exp, scale=-1.0), reads=["cum_sb"], writes=["En_sb"])
                mk.op("act", lambda e: e.activation(out=q_sb[:], in_=q_sb[:], func=AF.Silu), reads=["q_sb"], writes=["q_sb"])
                mk.op("act", lambda e: e.activation(out=g_sb[:], in_=g_sb[:], func=AF.Sigmoid), reads=["g_sb"], writes=["g_sb"])
                mk.op("dve", lambda e: e.tensor_tensor(out=qt_sb[:], in0=q_sb[:], in1=E_sb[:], op=ALU.mult), reads=["q_sb", "E_sb"], writes=["qt_sb"])
                mk.op("pool", lambda e: e.tensor_tensor(out=kt_sb[:], in0=kk_sb[:], in1=En_sb[:], op=ALU.mult), reads=["kk_sb", "En_sb"], writes=["kt_sb"])
                for c in range(NCW):
                    cs = slice(c * C, (c + 1) * C)
                    i2 = it % 2
                    gi = hd * (S // C) + w * NCW + c
                    sc, sn = state[gi % 2], state[(gi + 1) % 2]
                    sck, snk = "state%d" % (gi % 2), "state%d" % ((gi + 1) % 2)
                    it += 1
                    mk.op("pe", lambda e: e.matmul(psT[i2][0:64, 0:128], lhsT=kt_sb[:, cs], rhs=ident, start=True, stop=True),
                          reads=["kt_sb", "cst"], writes=["psT%d" % i2])
                    mk.op("act", lambda e: e.copy(out=ktok[i2][:], in_=psT[i2][0:64, 0:128]), reads=["psT%d" % i2], writes=["ktok%d" % i2])
                    mk.op("pe", lambda e: e.matmul(psS[i2][0:64, 0:64], lhsT=kt_sb[:, cs], rhs=qt_sb[:, cs], start=True, stop=True),
                          reads=["kt_sb", "qt_sb"], writes=["psS%d" % i2])
                    mk.op("dve", lambda e: e.tensor_tensor(out=AT[i2][:], in0=psS[i2][0:64, 0:64], in1=tri[0:64, 0:64], op=ALU.mult),
                          reads=["psS%d" % i2, "cst"], writes=["AT%d" % i2])
                    mk.op("pe", lambda e: e.matmul(psO[i2][0:64, 0:128], lhsT=AT[i2][:], rhs=v_sb[:, c, :], start=True, stop=False),
                          reads=["AT%d" % i2, "v_sb"], writes=["psO%d" % i2])
                    mk.op("pe", lambda e: e.matmul(psO[i2][0:64, 0:128], lhsT=qt_sb[:, cs], rhs=sc[:], start=False, stop=True),
                          reads=["qt_sb", sck], writes=["psO%d" % i2])
                    mk.op("pe", lambda e: e.matmul(psD[i2][:, 0:128], lhsT=ktok[i2][:], rhs=v_sb[:, c, :], start=True, stop=True),
                          reads=["ktok%d" % i2, "v_sb"], writes=["psD%d" % (i2 % NPD)])
                    el = E_sb[:, c * C + C - 1:c * C + C]
                    mk.op("pool", lambda e: e.tensor_scalar(out=st1[:], in0=sc[:], scalar1=el, scalar2=None, op0=ALU.mult),
                          reads=[sck, "E_sb"], writes=["st1"])
                    mk.op("dve", lambda e: e.scalar_tensor_tensor(out=sn[:], in0=psD[i2][:, 0:128], scalar=el, in1=st1[:], op0=ALU.mult, op1=ALU.add),
                          reads=["psD%d" % (i2 % NPD), "st1", "E_sb"], writes=[snk])
                    fk = "fin%d" % i2
                    mk.op("act", lambda e: e.activation(out=sq[i2][:], in_=psO[i2][0:64, 0:128], func=AF.Square, accum_out=fin[i2][:, 0:1]),
                          reads=["psO%d" % i2], writes=["sq%d" % i2, fk])
                    mk.op("dve", lambda e: e.tensor_scalar(out=fin[i2][:, 1:2], in0=fin[i2][:, 0:1], scalar1=1.0 / 128, scalar2=LN_EPS, op0=ALU.mult, op1=ALU.add),
                          reads=[fk], writes=[fk])
                    mk.op("act", lambda e: e.activation(out=fin[i2][:, 1:2], in_=fin[i2][:, 1:2], func=AF.Sqrt), reads=[fk], writes=[fk])
                    mk.op("dve", lambda e: e.reciprocal(out=fin[i2][:, 1:2], in_=fin[i2][:, 1:2]), reads=[fk], writes=[fk])
                    mk.op("dve", lambda e: e.scalar_tensor_tensor(out=sq[i2][:], in0=psO[i2][0:64, 0:128], scalar=fin[i2][:, 1:2], in1=gnt[0:64, :],
                                                                  op0=ALU.mult, op1=ALU.mult),
                          reads=["psO%d" % i2, fk, "gn", "sq%d" % i2], writes=["sq%d" % i2])
                    mk.op("pool", lambda e: e.tensor_tensor(out=o_sb[:, c, :], in0=sq[i2][:], in1=g_sb[:, c, :], op=ALU.mult),
                          reads=["sq%d" % i2, "g_sb"], writes=["o_sb"])
                mk.dma("sync", o[t0:t0 + W, hd * 128:(hd + 1) * 128].rearrange("(c p) d -> p c d", p=64), o_sb[:], reads=["o_sb"], is_output=True)


def _standalone(emit, *args, **kw):
    from contextlib import ExitStack
    nc = bass.Bass("TRN2", target_bir_lowering=False)
    with ExitStack() as st:
        mk = MK(nc, st)
        emit(mk, IO(nc), *args, **kw)
        mk.finish()
    return nc


def build_gemm(K, N, ln=False, rope_units=None, rope_half=0, ntok=TC):
    return _standalone(emit_gemm, K, N, ln=ln, rope_units=rope_units, rope_half=rope_half, ntok=ntok)


def build_ffn(F, NE, ntok=TC, TCH=256):
    return _standalone(emit_ffn, F, NE, ntok=ntok, TCH=TCH)


def build_attn(kind, NH=4, S=SEQ, lam_init=0.0):
    return _standalone(emit_attn, kind, NH=NH, S=S, lam_init=lam_init)


def build_hgrn(NH=4, S=SEQ, layer=0, W=2048):
    return _standalone(emit_hgrn, NH=NH, S=S, layer=layer, W=W)


def build_nsa(S=SEQ):
    return _standalone(emit_nsa, S=S)


_PROGS = {}


def _prog(key, fn):
    if key not in _PROGS:
        _PROGS[key] = fn()
    return _PROGS[key]


def _c(a):
    return np.ascontiguousarray(a, dtype=np.float32)


def _rep(vec, n=128):
    return _c(np.broadcast_to(np.asarray(vec).reshape(1, -1), (n, np.asarray(vec).size)))


def _rope_tables(half):
    pos = np.arange(SEQ, dtype=np.float32)
    inv = (10000.0 ** (-np.arange(half, dtype=np.float32) / half)).astype(np.float32)
    ang = pos[:, None] * inv[None, :]
    return np.cos(ang).astype(np.float32), np.sin(ang).astype(np.float32)


def run_proj(h, w, rope_units=None, rope_half=0):
    N = w.shape[1]
    key = ("gemm", w.shape[0], N, False, tuple(rope_units or ()), rope_half)
    nc = _prog(key, lambda: build_gemm(w.shape[0], N, ln=False, rope_units=rope_units, rope_half=rope_half))
    ims = []
    if rope_units:
        cos, sin = _rope_tables(rope_half)
    for c in range(NCORES):
        sl = slice(c * TC, (c + 1) * TC)
        im = {"aT": _c(h[sl].T), "w": _c(w)}
        if rope_units:
            p0 = (c * TC) % SEQ
            im["cos"] = _c(cos[p0:p0 + TC])
            im["sin"] = _c(sin[p0:p0 + TC])
        ims.append(im)
    res = run(nc, ims)
    y = np.concatenate([r["y"] for r in res], 0)
    yr = np.concatenate([r["yr"] for r in res], 0) if rope_units else None
    return y, yr


def run_lrln(a, w, h, g, b):
    key = ("gemm", w.shape[0], w.shape[1], True)
    nc = _prog(key, lambda: build_gemm(w.shape[0], w.shape[1], ln=True))
    ims = []
    for c in range(NCORES):
        sl = slice(c * TC, (c + 1) * TC)
        ims.append({"aT": _c(a[sl].T), "w": _c(w), "hres": _c(h[sl]), "lng": _rep(g), "lnb": _rep(b)})
    res = run(nc, ims)
    return np.concatenate([r["y"] for r in res], 0)


def run_ffn(h, wg, wu, wd, g, b, wr=None):
    NE = 1 if wr is None else wg.shape[0]
    F = wg.shape[-1]
    nc = _prog(("ffn", F, NE), lambda: build_ffn(F, NE))
    if NE == 1:
        wg, wu, wd = wg[None], wu[None], wd[None]
    wgt = np.stack([pretile_w_in(wg[e], F) for e in range(NE)])
    wut = np.stack([pretile_w_in(wu[e], F) for e in range(NE)])
    wdt = _c(wd.reshape(NE, F // 128, 128, D_MODEL))
    lng, lnb = _rep(g), _rep(b)
    ims = []
    for c in range(NCORES):
        sl = slice(c * TC, (c + 1) * TC)
        im = {"hT": _c(h[sl].T), "h": _c(h[sl]), "wg": wgt, "wu": wut, "wd": wdt, "lng": lng, "lnb": lnb}
        if NE > 1:
            im["wr"] = _c(wr)
        ims.append(im)
    res = run(nc, ims)
    return np.concatenate([r["y"] for r in res], 0)


def _heads_T(x, b, hh):
    xb = x.reshape(BATCH, SEQ, N_HEADS, HEAD_DIM)[b, :, 4 * hh:4 * hh + 4, :]
    return _c(xb.transpose(1, 2, 0))


def _heads_tok(x, b, hh):
    xb = x.reshape(BATCH, SEQ, N_HEADS, HEAD_DIM)[b, :, 4 * hh:4 * hh + 4, :]
    return _c(xb.transpose(1, 0, 2))


def _gather_o(res):
    o = np.zeros((BATCH, SEQ, D_MODEL), np.float32)
    for c in range(NCORES):
        o[c // 2, :, (c % 2) * 512:(c % 2 + 1) * 512] = res[c]["o"]
    return o.reshape(T_ALL, D_MODEL)


def mixer_hgrn(h, w_in, lb, norm_g, layer):
    proj, _ = run_proj(h, w_in)
    D = D_MODEL
    q, f, i, g = proj[:, 0:D], proj[:, D:2 * D], proj[:, 2 * D:3 * D], proj[:, 3 * D:4 * D]
    W = 2048
    nc = _prog(("hgrn", layer), lambda: build_hgrn(NH=4, S=SEQ, layer=layer, W=W))
    m, cst = hg_consts(W)
    ims = []
    for c in range(NCORES):
        b, hh = c // 2, c % 2
        lbT = _c(lb.T.reshape(N_HEADS, 128, lb.shape[0])[4 * hh:4 * hh + 4])
        ims.append({"qT": _heads_T(q, b, hh), "fT": _heads_T(f, b, hh), "v": _heads_tok(i, b, hh), "g": _heads_tok(g, b, hh),
                    "lbT": lbT, "gn": _rep(norm_g), "msk": m, "cst": cst})
    return _gather_o(run(nc, ims))


def mixer_da(h, w_in, lam, norm_g, layer):
    proj, projr = run_proj(h, w_in, rope_units=[(0, 32)], rope_half=32)
    D = D_MODEL
    q, k, v = projr[:, 0:D], projr[:, D:2 * D], proj[:, 2 * D:3 * D]
    lam_init = 0.8 - 0.6 * math.exp(-0.3 * layer)
    nc = _prog(("da", layer), lambda: build_attn("da", NH=4, S=SEQ, lam_init=lam_init))
    cst = attn_consts()
    ims = []
    for c in range(NCORES):
        b, hh = c // 2, c % 2
        ims.append({"qT": _heads_T(q, b, hh), "kT": _heads_T(k, b, hh), "v": _heads_tok(v, b, hh), "cst": cst,
                    "lam": _rep(lam.reshape(-1)), "gn": _rep(norm_g)})
    return _gather_o(run(nc, ims))


def mixer_sb(h, w_in):
    proj, _ = run_proj(h, w_in)
    D = D_MODEL
    q, k, v = proj[:, 0:D], proj[:, D:2 * D], proj[:, 2 * D:3 * D]
    nc = _prog(("sb",), lambda: build_attn("sb", NH=4, S=SEQ))
    cst = attn_consts()
    ims = []
    for c in range(NCORES):
        b, hh = c // 2, c % 2
        ims.append({"qT": _heads_T(q, b, hh), "kT": _heads_T(k, b, hh), "v": _heads_tok(v, b, hh), "cst": cst})
    return _gather_o(run(nc, ims))


def _log(*a):
    import sys, time
    print("[kernel %.0f]" % time.time(), *a, file=sys.stderr, flush=True)


def kernel_unfused(x, hg_w_in, hg_lb, hg_norm_g, hg_w_out, da_w_in, da_lam, da_norm_g, da_w_out,
           nsa_w_in, nsa_cmp_pe, nsa_cmp_w1, nsa_cmp_w2, nsa_w_out, sb_w_in, sb_w_out,
           ffn_w_gate, ffn_w_up, ffn_w_down, moe_w_router, moe_w_gate, moe_w_up, moe_w_down,
           ln_g, ln_b):
    f = lambda a: np.asarray(a, dtype=np.float32)
    h = f(x).reshape(T_ALL, D_MODEL)
    ln_g, ln_b = f(ln_g), f(ln_b)
    for layer in range(DEPTH):
        m = layer % 4
        if m == 0:
            o = mixer_hgrn(h, f(hg_w_in), f(hg_lb), f(hg_norm_g), layer)
            w_out = f(hg_w_out)
        elif m == 1:
            o = mixer_da(h, f(da_w_in), f(da_lam), f(da_norm_g), layer)
            w_out = f(da_w_out)
        elif m == 2:
            o = mixer_nsa(h, f(nsa_w_in), f(nsa_cmp_pe), f(nsa_cmp_w1), f(nsa_cmp_w2))
            w_out = f(nsa_w_out)
        else:
            o = mixer_sb(h, f(sb_w_in))
            w_out = f(sb_w_out)
        _log("mixer", layer)
        h = run_lrln(o, w_out, h, ln_g[layer, 0], ln_b[layer, 0])
        _log("lrln", layer)
        j = layer // 2
        if layer % 2 == 0:
            h = run_ffn(h, f(ffn_w_gate)[j], f(ffn_w_up)[j], f(ffn_w_down)[j], ln_g[layer, 1], ln_b[layer, 1])
        else:
            h = run_ffn(h, f(moe_w_gate)[j], f(moe_w_up)[j], f(moe_w_down)[j], ln_g[layer, 1], ln_b[layer, 1], wr=f(moe_w_router)[j])
        _log("ffn", layer)
    return h.reshape(BATCH, SEQ, D_MODEL)


def nsa_consts(S=SEQ):
    n_cmp = (S - 32) // 16 + 1
    n_slc = S // 64
    cs = np.arange(n_cmp) * 16
    ss_ = np.arange(n_slc) * 64
    ov = np.clip(np.minimum(cs[:, None] + 32, ss_[None, :] + 64) - np.maximum(cs[:, None], ss_[None, :]), 0, None) / 32.0
    NCT = (n_cmp + 127) // 128
    M = np.zeros((NCT * 128, 128), np.float32)
    M[:n_cmp, :n_slc] = ov
    M = np.ascontiguousarray(M.reshape(NCT, 128, 128).transpose(1, 0, 2))
    NKT = S // 128
    E = np.zeros((128, NKT, 128), np.float32)
    for kt in range(NKT):
        for half in range(2):
            if 2 * kt + half < 128:
                E[2 * kt + half, kt, half * 64:(half + 1) * 64] = 32768.0
    return M, E


def emit_nsa(mk, io, S=SEQ, tm=False):
    from contextlib import ExitStack
    import ml_dtypes
    nc = mk.nc
    NH = 4
    QC = 512
    NQC = S // QC
    NKT = S // 128
    NCMP = (S - 32) // 16 + 1
    NCT = (NCMP + 127) // 128
    NCP = NCT * 128
    scale = 128 ** -0.5
    if tm:
        q_tok = io.tin("q_tok", [S, NH * 128])
        qr_tok = io.tin("qr_tok", [S, NH * 128])
        kcv_tok = [io.tin("kc_tok", [S, 128]), io.tin("vc_tok", [S, 128])]
        ks_tok = io.tin("ks_tok", [S, 128])
        kw_tok = io.tin("kw_tok", [S, 128])
    else:
        qT = io.tin("qT", [NH, 128, S])
        qrT = io.tin("qrT", [NH, 128, S])
        kcv = io.tin("kcv", [2, 128, S])
        ksT = io.tin("ksT", [128, S])
        kwT = io.tin("kwT", [128, S])
    vs = io.tin("vs", [S, 128])
    vw = io.tin("vw", [S, 128])
    gat = io.tin("gat", [S, 12])
    w1 = io.tin("w1", [128, 2 * 32 * 128])
    peT = io.tin("peT", [128, 64])
    w2 = io.tin("w2", [128, 256])
    Mc = io.tin("Mc", [128, NCT, 128])
    Ec = io.tin("Ec", [128, NKT, 128])
    cst = io.tin("cst", [128, 4, 128])
    ident_d = io.tin("ident", [128, 128])
    o = io.tout("o", [S, NH * 128])
    with ExitStack() as st:
        ct = mk.sb("ct", [128, 4, 128])
        mk.dma("sync", ct[:], cst[:, :, :], writes=["cst"])
        tri_incl, tri_strict, U, ones = ct[:, 0, :], ct[:, 1, :], ct[:, 2, :], ct[:, 3, :]
        ident = mk.sb("identt", [128, 128])
        mk.dma("sync", ident[:], ident_d[:, :], writes=["ident"])
        bufA = mk.sb("bufA", [128, S])
        bufB = mk.sb("bufB", [128, max(S, 8192)])
        vs_sb = mk.sb("vs_sb", [128, NKT, 129])
        vw_sb = mk.sb("vw_sb", [128, NKT, 129])
        EW = min(4, NKT)
        Ef = mk.sb("Ef", [128, EW * 128])
        Eb = mk.sb("Eb", [128, NKT, 128], BF16)
        Mt = mk.sb("Mt", [128, NCT, 128])
        pet = mk.sb("pet", [128, 64])
        w2t = mk.sb("w2t", [128, 256])
        kcmpT = mk.sb("kcmpT", [128, NCP])
        vcm = mk.sb("vcm", [128, NCT, 256])
        pT = [mk.sb("pT%d" % i, [128, QC]) for i in range(3)]
        gu, g2, gl = pT[0], pT[1], pT[2]
        bia = mk.sb("bia", [128, 2])
        psS = [mk.ps("psS%d" % i, [128, 512]) for i in range(2)]
        psC = [mk.ps("psC%d" % i, [128, 512]) for i in range(2)]
        psX = mk.ps("psX", [128, 512])
        psY = mk.ps("psY", [128, 512])
        psZ = mk.ps("psZ", [128, 512])
        psT = mk.ps("psT", [128, 512])
        if tm:
            tmps = [(mk.sb("ltmp%d" % i, [128, 4, 128]), "ltmp%d" % i) for i in range(2)]
        mk.dma("sync", Mt[:], Mc[:, :, :], writes=["Mt"])
        mk.dma("sync", pet[:], peT[:, :], writes=["pet"])
        mk.dma("sync", w2t[:], w2[:, :], writes=["w2t"])
        for c4 in range(4):
            mk.dma("sync", bufB[:, c4 * 2048:(c4 + 1) * 2048], w1[:, c4 * 2048:(c4 + 1) * 2048], writes=["bufB"])
        for c4 in range(NKT // EW):
            mk.dma("poolq", Ef[:], Ec[:, c4 * EW:(c4 + 1) * EW, :].rearrange("p a b -> p (a b)"), writes=["Ef"])
            mk.op("dve", lambda e: e.tensor_copy(out=Eb[:, c4 * EW:(c4 + 1) * EW, :].rearrange("p a b -> p (a b)"), in_=Ef[:]),
                  reads=["Ef"], writes=["Eb"])
        mk.op("dve", lambda e: e.memset(vs_sb[:, :, 128:129], 1.0), writes=["vs_sb"])
        mk.op("dve", lambda e: e.memset(vw_sb[:, :, 128:129], 1.0), writes=["vw_sb"])
        mk.op("dve", lambda e: e.memset(gl[:], 0.0), writes=["pT2"])
        for c4 in range(4):
            k0, k1 = c4 * (NKT // 4), (c4 + 1) * (NKT // 4)
            mk.dma("poolq", vs_sb[:, k0:k1, 0:128], vs[k0 * 128:k1 * 128, :].rearrange("(kt p) d -> p kt d", p=128), writes=["vs_sb"])
            mk.dma("poolq", vw_sb[:, k0:k1, 0:128], vw[k0 * 128:k1 * 128, :].rearrange("(kt p) d -> p kt d", p=128), writes=["vw_sb"])
        w1v = bufB[:].rearrange("p (j i n) -> p j i n", j=2, i=32)
        for j in range(2):
            if tm:
                emit_loadT(mk, bufA[:], "bufA", kcv_tok[j], NKT, tmps, psT, "psT", ident[:])
            else:
                for c4 in range(4):
                    sl = slice(c4 * (S // 4), (c4 + 1) * (S // 4))
                    mk.dma("sync", bufA[:, sl], kcv[j, :, sl], writes=["bufA"])
            for i in range(32):
                mk.op("pe", lambda e: e.matmul(psS[0][:, 0:NCMP], lhsT=w1v[:, j, i, :], rhs=bufA[:, i:i + 16 * (NCMP - 1) + 1:16],
                                               start=(i == 0), stop=(i == 31)),
                      reads=["bufA", "bufB"], writes=["psS0"])
            for i in range(32):
                mk.op("pe", lambda e: e.matmul(psS[1][:, 0:1], lhsT=w1v[:, j, i, :], rhs=pet[:, j * 32 + i:j * 32 + i + 1],
                                               start=(i == 0), stop=(i == 31)),
                      reads=["pet", "bufB"], writes=["psS1"])
            mk.op("dve", lambda e: e.tensor_copy(out=bia[:, j:j + 1], in_=psS[1][:, 0:1]), reads=["psS1"], writes=["bia"])
            mk.op("act", lambda e: e.activation(out=gu[:, 0:NCMP], in_=psS[0][:, 0:NCMP], func=AF.Identity, bias=bia[:, j:j + 1], scale=1.0),
                  reads=["psS0", "bia"], writes=["pT0"])
            mk.op("dve", lambda e: e.tensor_tensor(out=g2[:, 0:NCMP], in0=gu[:, 0:NCMP], in1=gu[:, 0:NCMP], op=ALU.mult), reads=["pT0"], writes=["pT1"])
            mk.op("dve", lambda e: e.tensor_scalar(out=g2[:, 0:NCMP], in0=g2[:, 0:NCMP], scalar1=0.044715, scalar2=1.0, op0=ALU.mult, op1=ALU.add),
                  reads=["pT1"], writes=["pT1"])
            mk.op("dve", lambda e: e.tensor_tensor(out=g2[:, 0:NCMP], in0=g2[:, 0:NCMP], in1=gu[:, 0:NCMP], op=ALU.mult), reads=["pT1", "pT0"], writes=["pT1"])
            mk.op("act", lambda e: e.activation(out=g2[:, 0:NCMP], in_=g2[:, 0:NCMP], func=AF.Tanh, scale=0.7978845608028654), reads=["pT1"], writes=["pT1"])
            mk.op("dve", lambda e: e.tensor_scalar(out=g2[:, 0:NCMP], in0=g2[:, 0:NCMP], scalar1=0.5, scalar2=0.5, op0=ALU.mult, op1=ALU.add),
                  reads=["pT1"], writes=["pT1"])
            mk.op("dve", lambda e: e.tensor_tensor(out=gl[:, 0:NCMP], in0=g2[:, 0:NCMP], in1=gu[:, 0:NCMP], op=ALU.mult), reads=["pT1", "pT0", "pT2"], writes=["pT2"])
            if j == 0:
                mk.op("pe", lambda e: e.matmul(psS[0][:, 0:NCP], lhsT=w2t[:, 0:128], rhs=gl[:, 0:NCP], start=True, stop=True),
                      reads=["pT2", "w2t"], writes=["psS0"])
                mk.op("act", lambda e: e.copy(out=kcmpT[:], in_=psS[0][:, 0:NCP]), reads=["psS0"], writes=["kcmpT"])
            else:
                for t4 in range(NCT):
                    mk.op("pe", lambda e: e.matmul(psS[0][:, t4 * 128:(t4 + 1) * 128], lhsT=gl[:, t4 * 128:(t4 + 1) * 128], rhs=w2t[:, 128:256],
                                                   start=True, stop=True),
                          reads=["pT2", "w2t"], writes=["psS0"])
                mk.op("act", lambda e: e.copy(out=vcm[:, :, 0:128], in_=psS[0][:, 0:NCP].rearrange("p (a b) -> p a b", b=128)),
                      reads=["psS0"], writes=["vcm"])
                mk.op("dve", lambda e: e.tensor_copy(out=vcm[:, :, 128:256], in_=Mt[:]), reads=["Mt"], writes=["vcm"])
        if tm:
            emit_loadT(mk, bufA[:], "bufA", ks_tok, NKT, tmps, psT, "psT", ident[:])
            emit_loadT(mk, bufB[:], "bufB", kw_tok, NKT, tmps, psT, "psT", ident[:])
        else:
            for c4 in range(4):
                sl = slice(c4 * (S // 4), (c4 + 1) * (S // 4))
                mk.dma("sync", bufA[:, sl], ksT[:, sl], writes=["bufA"])
                mk.dma("sync", bufB[:, sl], kwT[:, sl], writes=["bufB"])
        cmask = mk.sb("cmask", [128, 5, QC])
        mk.op("dve", lambda e: e.memset(cmask[:], 1.0), writes=["cmask"])
        for dd in range(5):
            mk.op("pool", lambda e: e.affine_select(out=cmask[:, dd, :], in_=cmask[:, dd, :], pattern=[[1, QC]], compare_op=ALU.is_ge, fill=0.0,
                                                    base=512 * dd - 31, channel_multiplier=-16), reads=["cmask"], writes=["cmask"])
        MA = mk.sb("MA", [128, 256])
        MV = mk.sb("MV", [128, 256])
        FA = mk.sb("FA", [128, 256])
        FV = mk.sb("FV", [128, 256])
        mk.op("dve", lambda e: e.memset(MA[:], 1.0), writes=["masters"])
        mk.op("dve", lambda e: e.memset(MV[:], 1.0), reads=["masters"], writes=["masters"])
        mk.op("pool", lambda e: e.affine_select(out=MA[:], in_=MA[:], pattern=[[-64, 256]], compare_op=ALU.is_ge, fill=0.0,
                                                base=8064, channel_multiplier=1), reads=["masters"], writes=["masters"])
        mk.op("pool", lambda e: e.affine_select(out=MV[:], in_=MV[:], pattern=[[-64, 256]], compare_op=ALU.is_ge, fill=0.0,
                                                base=8192, channel_multiplier=1), reads=["masters"], writes=["masters"])
        mk.op("dve", lambda e: e.tensor_scalar(out=FA[:], in0=MA[:], scalar1=-1e9, scalar2=1e9, op0=ALU.mult, op1=ALU.add), reads=["masters"], writes=["masters"])
        mk.op("dve", lambda e: e.tensor_scalar(out=FV[:], in0=MV[:], scalar1=1e30, scalar2=-1e30, op0=ALU.mult, op1=ALU.add), reads=["masters"], writes=["masters"])
        NB = 2
        q_sb = [mk.sb("q_sb%d" % i, [128, QC]) for i in range(NB)]
        qr_sb = q_sb
        ocmp = mk.sb("ocmp", [128, 4, NH, 128])
        imp = mk.sb("imp", [128, 4, 128])
        imx = mk.sb("imx", [128, 4, 128])
        selm = mk.sb("selm", [128, 4, 128])
        vmk = imx
        mx8 = mk.sb("mx8", [128, 16])
        rs = mk.sb("rs", [128, 8])
        selT = mk.sb("selT", [128, QC], BF16)
        gt_sb = mk.sb("gt_sb", [128, 4, 12])
        o_sb = [mk.sb("o_sb%d" % i, [128, 4, 128]) for i in range(NB)]
        fin = mk.sb("fin", [128, 8])
        it = 0

        def acc_ap(bank, bk, jq, idx):
            if jq < 3:
                return bank[:, jq * 129:(jq + 1) * 129], bk
            return psZ[:, idx * 129:(idx + 1) * 129], "psZ"

        for qc in range(NQC):
            q0 = qc * QC
            mk.dma("poolq", gt_sb[:], gat[q0:q0 + QC, :].rearrange("(j p) g -> p j g", p=128), writes=["gt_sb"])
            mk.op("act", lambda e: e.activation(out=gt_sb[:], in_=gt_sb[:], func=AF.Sigmoid), reads=["gt_sb"], writes=["gt_sb"])
            nct = min(NCT, (32 * qc + 30) // 128 + 1)
            for hd in range(NH):
                qb = (qc * NH + hd) % NB
                if tm:
                    emit_loadT(mk, q_sb[qb][:], "q_sb%d" % qb, q_tok[q0:q0 + QC, hd * 128:(hd + 1) * 128], QC // 128, tmps, psT, "psT", ident[:])
                else:
                    mk.dma("sync", q_sb[qb][:], qT[hd, :, q0:q0 + QC], writes=["q_sb%d" % qb])
                for c_t in range(nct):
                    pS, pk = psS[it % 2], "psS%d" % (it % 2)
                    pt, ptk = pT[it % 3], "pT%d" % (it % 3)
                    it += 1
                    mk.op("pe", lambda e: e.matmul(pS[:, 0:QC], lhsT=kcmpT[:, c_t * 128:(c_t + 1) * 128], rhs=q_sb[qb][:, :], start=True, stop=True),
                          reads=["kcmpT", "q_sb%d" % qb], writes=[pk])
                    mk.op("act", lambda e: e.activation(out=pt[:, :], in_=pS[:, 0:QC], func=AF.Exp, scale=float(scale)), reads=[pk], writes=[ptk])
                    delta = q0 - 2048 * c_t
                    if delta < 2063:
                        mk.op("pool", lambda e: e.tensor_tensor(out=pt[:, :], in0=pt[:, :], in1=cmask[:, delta // 512, :], op=ALU.mult),
                              reads=[ptk, "cmask"], writes=[ptk])
                    for jq in range(4):
                        bank = psC[jq // 2]
                        mk.op("pe", lambda e: e.matmul(bank[:, (jq % 2) * 256:(jq % 2 + 1) * 256], lhsT=pt[:, jq * 128:(jq + 1) * 128], rhs=vcm[:, c_t, :],
                                                       start=(c_t == 0 and jq % 2 == 0), stop=(c_t == nct - 1), skip_group_check=True),
                              reads=[ptk, "vcm"], writes=["psC%d" % (jq // 2)])
                for jq in range(4):
                    bank = psC[jq // 2]
                    cb = (jq % 2) * 256
                    mk.op("dve", lambda e: e.tensor_reduce(out=rs[:, 0:1], in_=bank[:, cb + 128:cb + 256], axis=AX.X, op=ALU.add),
                          reads=["psC%d" % (jq // 2)], writes=["rs"])
                    mk.op("dve", lambda e: e.tensor_scalar(out=rs[:, 0:1], in0=rs[:, 0:1], scalar1=1e-30, scalar2=None, op0=ALU.max), reads=["rs"], writes=["rs"])
                    mk.op("dve", lambda e: e.reciprocal(out=rs[:, 1:2], in_=rs[:, 0:1]), reads=["rs"], writes=["rs"])
                    mk.op("dve", lambda e: e.tensor_scalar(out=ocmp[:, jq, hd, :], in0=bank[:, cb:cb + 128], scalar1=rs[:, 1:2], scalar2=None, op0=ALU.mult),
                          reads=["psC%d" % (jq // 2), "rs"], writes=["ocmp"])
                    if hd == 0:
                        mk.op("dve", lambda e: e.tensor_scalar(out=imp[:, jq, :], in0=bank[:, cb + 128:cb + 256], scalar1=rs[:, 1:2], scalar2=None, op0=ALU.mult),
                              reads=["psC%d" % (jq // 2), "rs"], writes=["imp"])
                    else:
                        mk.op("dve", lambda e: e.scalar_tensor_tensor(out=imp[:, jq, :], in0=bank[:, cb + 128:cb + 256], scalar=rs[:, 1:2], in1=imp[:, jq, :],
                                                                      op0=ALU.mult, op1=ALU.add),
                              reads=["psC%d" % (jq // 2), "rs", "imp"], writes=["imp"])
            for jq in range(4):
                tb = q0 + jq * 128
                x = imp[:, jq, :]
                tt2 = 2 * (tb // 128)
                msl = slice(128 - tt2, 256 - tt2)
                mk.op("pool", lambda e: e.tensor_tensor(out=x, in0=x, in1=MA[:, msl], op=ALU.mult), reads=["imp", "masters"], writes=["imp"])
                mk.op("pool", lambda e: e.tensor_tensor(out=x, in0=x, in1=FA[:, msl], op=ALU.add), reads=["imp", "masters"], writes=["imp"])
                mk.op("dve", lambda e: e.memset(imp[:, jq, 0:1], 1e9), reads=["imp"], writes=["imp"])
                mk.op("pool", lambda e: e.tensor_tensor(out=x, in0=x, in1=MV[:, msl], op=ALU.mult), reads=["imp", "masters"], writes=["imp"])
                mk.op("pool", lambda e: e.tensor_tensor(out=x, in0=x, in1=FV[:, msl], op=ALU.add), reads=["imp", "masters"], writes=["imp"])
                mk.op("dve", lambda e: e.max(out=mx8[:, 0:8], in_=x), reads=["imp"], writes=["mx8"])
                mk.op("dve", lambda e: e.match_replace(out=imx[:, jq, :], in_to_replace=mx8[:, 0:8], in_values=x, imm_value=-3e38),
                      reads=["imp", "mx8"], writes=["imx"])
                mk.op("dve", lambda e: e.max(out=mx8[:, 8:16], in_=imx[:, jq, :]), reads=["imx"], writes=["mx8"])
                mk.op("dve", lambda e: e.tensor_scalar(out=selm[:, jq, :], in0=x, scalar1=mx8[:, 15:16], scalar2=None, op0=ALU.is_ge),
                      reads=["imp", "mx8"], writes=["selm"])
                mk.op("dve", lambda e: e.tensor_scalar(out=vmk[:, jq, :], in0=x, scalar1=-5e29, scalar2=None, op0=ALU.is_gt), reads=["imp"], writes=["imx"])
                mk.op("dve", lambda e: e.tensor_tensor(out=selm[:, jq, :], in0=selm[:, jq, :], in1=vmk[:, jq, :], op=ALU.mult), reads=["selm", "imx"], writes=["selm"])
                mk.op("dve", lambda e: e.tensor_scalar(out=selm[:, jq, :], in0=selm[:, jq, :], scalar1=-1.0, scalar2=None, op0=ALU.add), reads=["selm"], writes=["selm"])
                mk.op("pe", lambda e: e.matmul(psT[:, jq * 128:(jq + 1) * 128], lhsT=selm[:, jq, :], rhs=ident[:], start=True, stop=True),
                      reads=["selm", "ident"], writes=["psT"])
            mk.op("act", lambda e: e.copy(out=selT[:], in_=psT[:, 0:QC]), reads=["psT"], writes=["selT"])
            for hd in range(NH):
                qb = (qc * NH + hd) % NB
                ob = (qc * NH + hd) % NB
                if tm:
                    emit_loadT(mk, qr_sb[qb][:], "q_sb%d" % qb, qr_tok[q0:q0 + QC, hd * 128:(hd + 1) * 128], QC // 128, tmps, psT, "psT", ident[:])
                else:
                    mk.dma("sync", qr_sb[qb][:], qrT[hd, :, q0:q0 + QC], writes=["q_sb%d" % qb])
                nkt = 4 * qc + 4
                stA, stB = [], []
                for kt in range(nkt):
                    def mk_item(kt=kt, it=it):
                        r = max(0, kt - 4 * qc)
                        c0 = 128 * r
                        pS, pk = psS[it % 2], "psS%d" % (it % 2)
                        pt, ptk = pT[it % 3], "pT%d" % (it % 3)

                        def A():
                            mk.op("pe", lambda e: e.matmul(pS[:, c0:QC], lhsT=bufA[:, kt * 128:(kt + 1) * 128], rhs=qr_sb[qb][:, c0:QC], start=True, stop=False),
                                  reads=["bufA", "q_sb%d" % qb], writes=[pk])
                            mk.op("pe", lambda e: e.matmul(pS[:, c0:QC], lhsT=Eb[:, kt, :], rhs=selT[:, c0:QC], start=False, stop=True),
                                  reads=["Eb", "selT"], writes=[pk])
                            mk.op("act", lambda e: e.activation(out=pt[:, c0:QC], in_=pS[:, c0:QC], func=AF.Exp, scale=float(scale)), reads=[pk], writes=[ptk])
                            if kt >= 4 * qc:
                                mk.op("pool", lambda e: e.tensor_tensor(out=pt[:, c0:c0 + 128], in0=pt[:, c0:c0 + 128], in1=tri_incl, op=ALU.mult),
                                      reads=[ptk, "cst"], writes=[ptk])

                        def B():
                            for jq in range(r, 4):
                                ap, ak = acc_ap(psX, "psX", jq, 0)
                                mk.op("pe", lambda e: e.matmul(ap, lhsT=pt[:, jq * 128:(jq + 1) * 128], rhs=vs_sb[:, kt, :],
                                                               start=(kt == 0 and jq in (0, 3)), stop=(kt == 4 * qc + jq), skip_group_check=True),
                                      reads=[ptk, "vs_sb"], writes=[ak])
                        return A, B
                    a_, b_ = mk_item()
                    it += 1
                    stA.append(a_)
                    stB.append(b_)
                emit_skewed([stA, stB])
                first_w = {}
                for kt in range(max(0, 4 * qc - 4), nkt):
                    far = kt < 4 * qc
                    r = kt - (4 * qc - 4) if far else kt - 4 * qc
                    if far:
                        cA, cB = 0, 128 * (r + 1)
                        slices = range(0, r + 1)
                    else:
                        cA, cB = 128 * r, QC
                        slices = range(r, 4)
                    pS, pk = psS[it % 2], "psS%d" % (it % 2)
                    pt, ptk = pT[it % 3], "pT%d" % (it % 3)
                    it += 1
                    mk.op("pe", lambda e: e.matmul(pS[:, cA:cB], lhsT=bufB[:, kt * 128:(kt + 1) * 128], rhs=qr_sb[qb][:, cA:cB], start=True, stop=True),
                          reads=["bufB", "q_sb%d" % qb], writes=[pk])
                    mk.op("act", lambda e: e.activation(out=pt[:, cA:cB], in_=pS[:, cA:cB], func=AF.Exp, scale=float(scale)), reads=[pk], writes=[ptk])
                    if far:
                        mk.op("pool", lambda e: e.tensor_tensor(out=pt[:, 128 * r:128 * r + 128], in0=pt[:, 128 * r:128 * r + 128], in1=U, op=ALU.mult),
                              reads=[ptk, "cst"], writes=[ptk])
                    else:
                        mk.op("pool", lambda e: e.tensor_tensor(out=pt[:, cA:cA + 128], in0=pt[:, cA:cA + 128], in1=tri_incl, op=ALU.mult),
                              reads=[ptk, "cst"], writes=[ptk])
                    for jq in slices:
                        ap, ak = acc_ap(psY, "psY", jq, 1)
                        isfirst = jq not in first_w
                        first_w[jq] = True
                        bank_first = isfirst and (jq == 3 or len(first_w) == 1 or (jq < 3 and all(x == 3 for x in first_w if x != jq)))
                        mk.op("pe", lambda e: e.matmul(ap, lhsT=pt[:, jq * 128:(jq + 1) * 128], rhs=vw_sb[:, kt, :],
                                                       start=bank_first, stop=(kt == 4 * qc + jq), skip_group_check=True),
                              reads=[ptk, "vw_sb"], writes=[ak])
                for jq in range(4):
                    aS, aSk = acc_ap(psX, "psX", jq, 0)
                    aW, aWk = acc_ap(psY, "psY", jq, 1)
                    mk.op("dve", lambda e: e.reciprocal(out=fin[:, 0:1], in_=aS[:, 128:129]), reads=[aSk], writes=["fin"])
                    mk.op("dve", lambda e: e.reciprocal(out=fin[:, 1:2], in_=aW[:, 128:129]), reads=[aWk], writes=["fin"])
                    mk.op("dve", lambda e: e.tensor_tensor(out=fin[:, 0:1], in0=fin[:, 0:1], in1=gt_sb[:, jq, hd * 3 + 1:hd * 3 + 2], op=ALU.mult),
                          reads=["fin", "gt_sb"], writes=["fin"])
                    mk.op("dve", lambda e: e.tensor_tensor(out=fin[:, 1:2], in0=fin[:, 1:2], in1=gt_sb[:, jq, hd * 3 + 2:hd * 3 + 3], op=ALU.mult),
                          reads=["fin", "gt_sb"], writes=["fin"])
                    mk.op("dve", lambda e: e.tensor_scalar(out=o_sb[ob][:, jq, :], in0=ocmp[:, jq, hd, :], scalar1=gt_sb[:, jq, hd * 3:hd * 3 + 1], scalar2=None, op0=ALU.mult),
                          reads=["ocmp", "gt_sb"], writes=["o_sb%d" % ob])
                    mk.op("dve", lambda e: e.scalar_tensor_tensor(out=o_sb[ob][:, jq, :], in0=aS[:, 0:128], scalar=fin[:, 0:1], in1=o_sb[ob][:, jq, :],
                                                                  op0=ALU.mult, op1=ALU.add),
                          reads=[aSk, "fin", "o_sb%d" % ob], writes=["o_sb%d" % ob])
                    mk.op("dve", lambda e: e.scalar_tensor_tensor(out=o_sb[ob][:, jq, :], in0=aW[:, 0:128], scalar=fin[:, 1:2], in1=o_sb[ob][:, jq, :],
                                                                  op0=ALU.mult, op1=ALU.add),
                          reads=[aWk, "fin", "o_sb%d" % ob], writes=["o_sb%d" % ob])
                mk.dma("sync", o[q0:q0 + QC, hd * 128:(hd + 1) * 128].rearrange("(j p) d -> p j d", p=128), o_sb[ob][:],
                       reads=["o_sb%d" % ob], is_output=True)


def mixer_nsa(h, w_in, cmp_pe, cmp_w1, cmp_w2):
    D = D_MODEL
    proj, projr = run_proj(h, w_in, rope_units=[(0, 8), (1536, 2), (2048, 2)], rope_half=64)
    nc = _prog(("nsa",), lambda: build_nsa(SEQ))
    Mc, Ec = nsa_consts(SEQ)
    cst = attn_consts()
    q, qr = proj[:, 0:D], projr[:, 0:D]
    kv = proj[:, D:D + 1536].reshape(BATCH, SEQ, 6, 2, 128)
    kvr = projr[:, D:D + 1536].reshape(BATCH, SEQ, 6, 2, 128)
    gates = proj[:, D + 1536:D + 1536 + 24].reshape(BATCH, SEQ, 8, 3)
    w1 = _c(cmp_w1.reshape(2, 32, 128, 128).transpose(2, 0, 1, 3).reshape(128, 2 * 32 * 128))
    peT = _c(cmp_pe.transpose(2, 0, 1).reshape(128, 64))
    w2 = _c(cmp_w2.transpose(1, 0, 2).reshape(128, 256))
    ident = np.eye(128, dtype=np.float32)
    ims = []
    for c in range(NCORES):
        b, g = c // 2, c % 2
        ims.append({"qT": _heads_T(q, b, g), "qrT": _heads_T(qr, b, g),
                    "kcv": _c(np.stack([kv[b, :, 0, g, :].T, kv[b, :, 1, g, :].T])),
                    "ksT": _c(kvr[b, :, 2, g, :].T), "kwT": _c(kvr[b, :, 4, g, :].T),
                    "vs": _c(kv[b, :, 3, g, :]), "vw": _c(kv[b, :, 5, g, :]),
                    "gat": _c(gates[b, :, 4 * g:4 * g + 4, :].reshape(SEQ, 12)),
                    "w1": w1, "peT": peT, "w2": w2, "Mc": Mc, "Ec": Ec, "cst": cst, "ident": ident})
    return _gather_o(run(nc, ims))


def build_fused(layers=(0, 1, 2, 3), S=SEQ, debug_outs=False):
    from contextlib import ExitStack
    nc = bass.Bass("TRN2", target_bir_lowering=False)
    D = D_MODEL

    def tin(name, shape):
        return nc.dram_tensor(name, list(shape), F32, kind="ExternalInput").ap()

    def scratch(name, shape):
        return nc.dram_tensor(name, list(shape), F32, kind="Internal").ap()

    x = tin("x", [S, D])
    SH = S // 2
    y_out = nc.dram_tensor("y", [SH, D], F32, kind="ExternalOutput").ap()
    tokidx = nc.dram_tensor("tokidx", [128, SH // 128], mybir.dt.int32, kind="ExternalInput").ap()
    ident = tin("ident", [128, 128])
    cst = tin("cst", [128, 4, 128])
    lng = {(l, j): tin("lng%d%d" % (l, j), [128, D]) for l in layers for j in range(2)}
    lnb = {(l, j): tin("lnb%d%d" % (l, j), [128, D]) for l in layers for j in range(2)}
    proj = scratch("proj", [S, 4096])
    projr = scratch("projr", [S, 4096])
    o_d = scratch("o_d", [S, D])
    hA = scratch("hA", [S, D])
    hB = scratch("hB", [S, D])
    FTd, FTe = D_FF // 128, D_FF_EXPERT // 128
    with ExitStack() as st:
        mk = MK(nc, st)

        def stage(emit, given, *a, **kw):
            mk.begin_stage()
            emit(mk, IO(nc, given), *a, **kw)
            mk.end_stage()

        h_in = x
        for li, layer in enumerate(layers):
            m = layer % 4
            last = (li == len(layers) - 1)
            if m == 0:
                N = 4096
                w_in = tin("hg_w_in", [D, N])
                w_out = tin("hg_w_out", [D, D])
                stage(emit_gemm, {"a": h_in, "ident": ident, "w": w_in, "y": proj[:, 0:N]}, D, N, ntok=S, tm=True)
                W = 2048
                given = {"q_tok": proj[:, 0:D], "f_tok": proj[:, D:2 * D], "v_tok": proj[:, 2 * D:3 * D], "g_tok": proj[:, 3 * D:4 * D],
                         "lbT": tin("hg_lbT", [8, 128, 5]), "gn": tin("hg_gn", [128, 128]), "msk": tin("hg_msk", [128, W]),
                         "cst": tin("hg_cst", [128, 2, 128]), "o": o_d}
                stage(emit_hgrn, given, NH=8, S=S, layer=layer, W=W, tm=True)
            elif m == 1:
                N = 3072
                w_in = tin("da_w_in", [D, N])
                w_out = tin("da_w_out", [D, D])
                stage(emit_gemm, {"a": h_in, "ident": ident, "w": w_in, "y": proj[:, 0:N], "yr": projr[:, 0:N],
                                  "cos": tin("cos32", [S, 32]), "sin": tin("sin32", [S, 32])},
                      D, N, rope_units=[(0, 32)], rope_half=32, ntok=S, tm=True)
                lam_init = 0.8 - 0.6 * math.exp(-0.3 * layer)
                given = {"q_tok": projr[:, 0:D], "k_tok": projr[:, D:2 * D], "v_tok": proj[:, 2 * D:3 * D], "ident": ident, "cst": cst,
                         "lam": tin("da_lam", [128, 256]), "gn": tin("da_gn", [128, 128]), "o": o_d}
                stage(emit_attn, given, "da", NH=8, S=S, lam_init=lam_init, tm=True)
            elif m == 2:
                N = 2584
                w_in = tin("nsa_w_in", [D, N])
                w_out = tin("nsa_w_out", [D, D])
                stage(emit_gemm, {"a": h_in, "ident": ident, "w": w_in, "y": proj[:, 0:N], "yr": projr[:, 0:N],
                                  "cos": tin("cos64", [S, 64]), "sin": tin("sin64", [S, 64])},
                      D, N, rope_units=[(0, 8), (1536, 2), (2048, 2)], rope_half=64, ntok=S, tm=True)
                NKT = S // 128
                NCT = ((S - 32) // 16 + 1 + 127) // 128
                nsa_c = {"w1": tin("nsa_w1", [128, 2 * 32 * 128]), "peT": tin("nsa_peT", [128, 64]), "w2": tin("nsa_w2", [128, 256]),
                         "Mc": tin("nsa_Mc", [128, NCT, 128]), "Ec": tin("nsa_Ec", [128, NKT, 128])}
                for g in range(2):
                    def kvc(t, j):
                        c0 = D + j * 256 + g * 128
                        return t[:, c0:c0 + 128]
                    given = dict(nsa_c)
                    given.update({"q_tok": proj[:, g * 512:(g + 1) * 512], "qr_tok": projr[:, g * 512:(g + 1) * 512],
                                  "kc_tok": kvc(proj, 0), "vc_tok": kvc(proj, 1), "ks_tok": kvc(projr, 2), "vs": kvc(proj, 3),
                                  "kw_tok": kvc(projr, 4), "vw": kvc(proj, 5), "gat": proj[:, 2560 + 12 * g:2560 + 12 * g + 12],
                                  "cst": cst, "ident": ident, "o": o_d[:, g * 512:(g + 1) * 512]})
                    stage(emit_nsa, given, S=S, tm=True)
            else:
                N = 3072
                w_in = tin("sb_w_in", [D, N])
                w_out = tin("sb_w_out", [D, D])
                stage(emit_gemm, {"a": h_in, "ident": ident, "w": w_in, "y": proj[:, 0:N]}, D, N, ntok=S, tm=True)
                given = {"q_tok": proj[:, 0:D], "k_tok": proj[:, D:2 * D], "v_tok": proj[:, 2 * D:3 * D], "ident": ident, "cst": cst, "o": o_d}
                stage(emit_attn, given, "sb", NH=8, S=S, tm=True)
            nt = SH if last else S
            given = {"a": o_d, "ident": ident, "w": w_out, "hres": h_in, "lng": lng[(layer, 0)], "lnb": lnb[(layer, 0)], "y": hA[0:nt, :]}
            if last:
                given["tokidx"] = tokidx
            stage(emit_gemm, given, D, D, ln=True, ntok=nt, tm=True, gather=last)
            h_out = y_out if last else hB
            j = layer // 2
            if layer % 2 == 0:
                given = {"h": hA[0:nt, :], "ident": ident, "wg": tin("ffn_wg%d" % j, [1, FTd, 128, 8, 128]), "wu": tin("ffn_wu%d" % j, [1, FTd, 128, 8, 128]),
                         "wd": tin("ffn_wd%d" % j, [1, FTd, 128, D]), "lng": lng[(layer, 1)], "lnb": lnb[(layer, 1)], "y": h_out}
                stage(emit_ffn, given, D_FF, 1, ntok=nt, tm=True)
            else:
                given = {"h": hA[0:nt, :], "ident": ident, "wg": tin("moe_wg%d" % j, [8, FTe, 128, 8, 128]), "wu": tin("moe_wu%d" % j, [8, FTe, 128, 8, 128]),
                         "wd": tin("moe_wd%d" % j, [8, FTe, 128, D]), "wr": tin("moe_wr%d" % j, [D, 8]),
                         "lng": lng[(layer, 1)], "lnb": lnb[(layer, 1)], "y": h_out}
                stage(emit_ffn, given, D_FF_EXPERT, N_EXPERTS, ntok=nt, tm=True)
            h_in = hB
        mk.finish()
    return nc


def fused_inputs(inp, layers=(0, 1, 2, 3), S=SEQ):
    f = lambda a: np.asarray(a, dtype=np.float32)
    sh = {"ident": np.eye(128, dtype=np.float32), "cst": attn_consts()}
    ln_g, ln_b = f(inp["ln_g"]), f(inp["ln_b"])
    for l in layers:
        for j in range(2):
            sh["lng%d%d" % (l, j)] = _rep(ln_g[l, j])
            sh["lnb%d%d" % (l, j)] = _rep(ln_b[l, j])
        m = l % 4
        jj = l // 2
        if m == 0:
            sh["hg_w_in"], sh["hg_w_out"] = _c(inp["hg_w_in"]), _c(inp["hg_w_out"])
            lb = f(inp["hg_lb"])
            sh["hg_lbT"] = _c(lb.T.reshape(N_HEADS, 128, lb.shape[0]))
            sh["hg_gn"] = _rep(f(inp["hg_norm_g"]))
            sh["hg_msk"], sh["hg_cst"] = hg_consts(2048)
        elif m == 1:
            sh["da_w_in"], sh["da_w_out"] = _c(inp["da_w_in"]), _c(inp["da_w_out"])
            sh["da_lam"] = _rep(f(inp["da_lam"]).reshape(-1))
            sh["da_gn"] = _rep(f(inp["da_norm_g"]))
            cos, sin = _rope_tables(32)
            sh["cos32"], sh["sin32"] = _c(cos[:S]), _c(sin[:S])
        elif m == 2:
            sh["nsa_w_in"], sh["nsa_w_out"] = _c(inp["nsa_w_in"]), _c(inp["nsa_w_out"])
            cos, sin = _rope_tables(64)
            sh["cos64"], sh["sin64"] = _c(cos[:S]), _c(sin[:S])
            sh["nsa_w1"] = _c(f(inp["nsa_cmp_w1"]).reshape(2, 32, 128, 128).transpose(2, 0, 1, 3).reshape(128, 2 * 32 * 128))
            sh["nsa_peT"] = _c(f(inp["nsa_cmp_pe"]).transpose(2, 0, 1).reshape(128, 64))
            sh["nsa_w2"] = _c(f(inp["nsa_cmp_w2"]).transpose(1, 0, 2).reshape(128, 256))
            sh["nsa_Mc"], sh["nsa_Ec"] = nsa_consts(S)
        else:
            sh["sb_w_in"], sh["sb_w_out"] = _c(inp["sb_w_in"]), _c(inp["sb_w_out"])
        if l % 2 == 0:
            F = D_FF
            sh["ffn_wg%d" % jj] = pretile_w_in(f(inp["ffn_w_gate"])[jj], F)[None]
            sh["ffn_wu%d" % jj] = pretile_w_in(f(inp["ffn_w_up"])[jj], F)[None]
            sh["ffn_wd%d" % jj] = _c(f(inp["ffn_w_down"])[jj].reshape(1, F // 128, 128, D_MODEL))
        else:
            F = D_FF_EXPERT
            sh["moe_wg%d" % jj] = np.stack([pretile_w_in(f(inp["moe_w_gate"])[jj, e], F) for e in range(N_EXPERTS)])
            sh["moe_wu%d" % jj] = np.stack([pretile_w_in(f(inp["moe_w_up"])[jj, e], F) for e in range(N_EXPERTS)])
            sh["moe_wd%d" % jj] = _c(f(inp["moe_w_down"])[jj].reshape(N_EXPERTS, F // 128, 128, D_MODEL))
            sh["moe_wr%d" % jj] = _c(f(inp["moe_w_router"])[jj])
    return sh


def kernel(**inp):
    _log("build start")
    nc = build_fused()
    _log("build done", nc.n_instructions())
    sh = fused_inputs(inp)
    x = np.asarray(inp["x"], dtype=np.float32)
    ims = []
    SH = SEQ // 2
    for c in range(NCORES):
        im = dict(sh)
        im["x"] = _c(x[c // 2])
        im["tokidx"] = fused_tokidx(c)
        ims.append(im)
    _log("inputs ready")
    res = run(nc, ims)
    _log("run done")
    return np.stack([np.concatenate([res[2 * b]["y"], res[2 * b + 1]["y"]], 0) for b in range(BATCH)], 0)


def fused_tokidx(c, S=SEQ):
    SH = S // 2
    base = (c % 2) * SH
    return np.ascontiguousarray((base + np.arange(SH, dtype=np.int32)).reshape(SH // 128, 128).T)
```

```python
import math
import numpy as np
import concourse.bass as bass
import concourse.mybir as mybir
from concourse.bass_utils import run_bass_kernel_spmd

F32 = mybir.dt.float32
BF16 = mybir.dt.bfloat16
AF = mybir.ActivationFunctionType
ALU = mybir.AluOpType
AX = mybir.AxisListType

D_MODEL = 1024
BATCH = 4
SEQ = 8192
DEPTH = 4
N_HEADS = 8
HEAD_DIM = 128
D_FF = 2816
N_EXPERTS = 8
D_FF_EXPERT = 3584
LN_EPS = 1e-5
ALPHA = (2 * DEPTH) ** 0.25
NCORES = 8
T_ALL = BATCH * SEQ
TC = T_ALL // NCORES


class _Eng:
    def __init__(self, eng, sem, name, self_sync=True):
        self.eng = eng
        self.sem = sem
        self.name = name
        self.count = 0
        self.clock = {}
        self.self_sync = self_sync


class _Queue:
    def __init__(self, eng, sems, name):
        self.eng = eng
        self.sems = [[s, 0] for s in sems]
        self.rr = 0
        self.clock = {}
        self.name = name


class MK:
    def __init__(self, nc, stack, n_dma_sems=6):
        self.nc = nc
        self.stack = stack
        self.sem_names = {}
        self.engs = {}
        for name, eng, ss in (("pe", nc.tensor, False), ("act", nc.scalar, True),
                              ("dve", nc.vector, True), ("pool", nc.gpsimd, True)):
            sem = stack.enter_context(nc.semaphore("s_" + name))
            self.engs[name] = _Eng(eng, sem, name, ss)
        self.queues = {}
        for name, eng in (("sync", nc.sync),):
            sems = [stack.enter_context(nc.semaphore("q_%s%d" % (name, i))) for i in range(n_dma_sems)]
            self.queues[name] = _Queue(eng, sems, name)
        sems = [stack.enter_context(nc.semaphore("q_pool%d" % i)) for i in range(n_dma_sems)]
        self.queues["poolq"] = _Queue(nc.gpsimd, sems, "poolq")
        self.queues["poolq"].clock = self.engs["pool"].clock
        self.last_w = {}
        self.readers = {}
        self.n_ins = 0
        self.out_events = []
        self.cur = stack

    def sb(self, name, shape, dt=F32):
        self.uid = getattr(self, "uid", 0) + 1
        return self.cur.enter_context(self.nc.sbuf_tensor("%s_%d" % (name, self.uid), list(shape), dt))

    def ps(self, name, shape, dt=F32):
        self.uid = getattr(self, "uid", 0) + 1
        return self.cur.enter_context(self.nc.psum_tensor("%s_%d" % (name, self.uid), list(shape), dt))

    def begin_stage(self):
        from contextlib import ExitStack
        self.cur = ExitStack()

    def barrier(self):
        evs = []
        for e in self.engs.values():
            if e.count > 0:
                evs.append((e.sem, e.count))
        for qq in self.queues.values():
            for sem, cnt in qq.sems:
                if cnt > 0:
                    evs.append((sem, cnt))
        streams = [(e.eng, e.clock) for e in self.engs.values()] + [(self.queues["sync"].eng, self.queues["sync"].clock)]
        for eng, clock in streams:
            for sem, val in evs:
                if clock.get(id(sem), 0) < val:
                    eng.wait_ge(sem, val)
                    clock[id(sem)] = val
        self.last_w = {}
        self.readers = {}

    def end_stage(self):
        self.barrier()
        self.cur.close()
        self.cur = self.stack

    def _need(self, reads, writes):
        need = {}

        def add(ev):
            if ev is None:
                return
            k = id(ev[0])
            if k not in need or need[k][1] < ev[1]:
                need[k] = ev

        for k in reads:
            add(self.last_w.get(k))
        for k in writes:
            add(self.last_w.get(k))
            for ev in self.readers.get(k, {}).values():
                add(ev)
        return need

    def _record(self, ev, reads, writes):
        for k in reads:
            self.readers.setdefault(k, {})[id(ev[0])] = ev
        for k in writes:
            self.last_w[k] = ev
            self.readers[k] = {}

    def op(self, engname, fn, reads=(), writes=()):
        e = self.engs[engname]
        need = self._need(reads, writes)
        for k, (sem, val) in need.items():
            if sem is e.sem and not e.self_sync:
                continue
            if e.clock.get(k, 0) < val:
                e.eng.wait_ge(sem, val)
                e.clock[k] = val
        ins = fn(e.eng)
        e.count += 1
        ins.then_inc(e.sem, 1)
        self._record((e.sem, e.count), reads, writes)
        self.n_ins += 1
        return ins

    def dma(self, qname, out, in_, reads=(), writes=(), is_output=False, gather_idx=None):
        q = self.queues[qname]
        slot = q.sems[q.rr % len(q.sems)]
        q.rr += 1
        sem, cnt = slot
        need = self._need(reads, writes)
        if cnt > 0:
            k = id(sem)
            if k not in need or need[k][1] < cnt:
                need[k] = (sem, cnt)
        for k, (s, val) in need.items():
            if q.clock.get(k, 0) < val:
                q.eng.wait_ge(s, val)
                q.clock[k] = val
        if gather_idx is not None:
            ins = q.eng.indirect_dma_start(out=out, out_offset=None, in_=in_,
                                           in_offset=bass.IndirectOffsetOnAxis(ap=gather_idx, axis=0))
        else:
            ins = q.eng.dma_start(out=out, in_=in_)
        slot[1] = cnt + 16
        ins.then_inc(sem, 16)
        ev = (sem, cnt + 16)
        self._record(ev, reads, writes)
        if is_output:
            self.out_events.append(ev)
        self.n_ins += 1
        return ins

    def finish(self):
        q = self.queues["sync"]
        for qq in self.queues.values():
            for sem, cnt in qq.sems:
                if cnt > 0 and q.clock.get(id(sem), 0) < cnt:
                    q.eng.wait_ge(sem, cnt)
                    q.clock[id(sem)] = cnt
        for e in self.engs.values():
            if e.count > 0:
                q.eng.wait_ge(e.sem, e.count)


class IO:
    def __init__(self, nc, given=None):
        self.nc = nc
        self.given = given

    def tin(self, name, shape):
        if self.given is not None:
            return self.given[name]
        return self.nc.dram_tensor(name, list(shape), F32, kind="ExternalInput").ap()

    def tout(self, name, shape):
        if self.given is not None:
            return self.given[name]
        return self.nc.dram_tensor(name, list(shape), F32, kind="ExternalOutput").ap()


def emit_skewed(stages):
    n = len(stages[0])
    ns = len(stages)
    for step in range(n + ns - 1):
        for k in range(ns):
            i = step - (ns - 1 - k) if False else step - k
        for k in range(ns):
            i = step - k
            if 0 <= i < n:
                stages[k][i]()


def emit_loadT(mk, dst, dkey, src, n, tmps, ps, psk, ident, identk="ident"):
    g = 0
    for j0 in range(0, n, 4):
        m = min(4, n - j0)
        tmp, tk = tmps[g % len(tmps)]
        g += 1
        mk.dma("sync", tmp[:, 0:m, :], src[j0 * 128:(j0 + m) * 128, :].rearrange("(j p) d -> p j d", p=128), writes=[tk])
        for j in range(m):
            mk.op("pe", lambda e: e.matmul(ps[:, j * 128:(j + 1) * 128], lhsT=tmp[:, j, :], rhs=ident, start=True, stop=True),
                  reads=[tk, identk], writes=[psk])
        if g % 2 == 0:
            mk.op("act", lambda e: e.copy(out=dst[:, j0 * 128:(j0 + m) * 128], in_=ps[:, 0:m * 128]), reads=[psk], writes=[dkey])
        else:
            mk.op("dve", lambda e: e.tensor_copy(out=dst[:, j0 * 128:(j0 + m) * 128], in_=ps[:, 0:m * 128]), reads=[psk], writes=[dkey])

def emit_ln(mk, z, zk, gt, bt, out, outk, st, stk, n=D_MODEL):
    s1, nm, ss, rs = st[:, 0:1], st[:, 1:2], st[:, 2:3], st[:, 3:4]
    mk.op("dve", lambda e: e.tensor_reduce(out=s1, in_=z, axis=AX.X, op=ALU.add), reads=[zk], writes=[stk])
    mk.op("dve", lambda e: e.tensor_scalar(out=nm, in0=s1, scalar1=-1.0 / n, scalar2=None, op0=ALU.mult),
          reads=[stk], writes=[stk])
    mk.op("dve", lambda e: e.tensor_scalar(out=z, in0=z, scalar1=nm, scalar2=None, op0=ALU.add),
          reads=[stk, zk], writes=[zk])
    mk.op("act", lambda e: e.activation(out=out, in_=z, func=AF.Square, accum_out=ss),
          reads=[zk], writes=[outk, stk])
    mk.op("dve", lambda e: e.tensor_scalar(out=rs, in0=ss, scalar1=1.0 / n, scalar2=LN_EPS, op0=ALU.mult, op1=ALU.add),
          reads=[stk], writes=[stk])
    mk.op("act", lambda e: e.activation(out=rs, in_=rs, func=AF.Sqrt), reads=[stk], writes=[stk])
    mk.op("dve", lambda e: e.reciprocal(out=rs, in_=rs), reads=[stk], writes=[stk])
    mk.op("dve", lambda e: e.scalar_tensor_tensor(out=out, in0=z, scalar=rs, in1=gt, op0=ALU.mult, op1=ALU.mult),
          reads=[stk, zk, "lng"], writes=[outk])
    mk.op("dve", lambda e: e.tensor_tensor(out=out, in0=out, in1=bt, op=ALU.add), reads=[outk, "lnb"], writes=[outk])


def emit_gemm(mk, io, K, N, ln=False, rope_units=None, rope_half=0, ntok=TC, tm=False, gather=False):
    from contextlib import ExitStack
    nc = mk.nc
    KT = K // 128
    if tm:
        a_tok = io.tin("a", [ntok, K])
        ident_d = io.tin("ident", [128, 128])
    else:
        aT = io.tin("aT", [K, ntok])
    w = io.tin("w", [K, N])
    y = io.tout("y", [ntok, N])
    if ln:
        hres = io.tin("hres", [ntok, N])
        lng = io.tin("lng", [128, N])
        lnb = io.tin("lnb", [128, N])
    if rope_units:
        cosd = io.tin("cos", [ntok, rope_half])
        sind = io.tin("sin", [ntok, rope_half])
        yr = io.tout("yr", [ntok, N])
    with ExitStack() as st:
        wt = mk.sb("wt", [128, KT, N])
        wv = w.rearrange("(k p) n -> p k n", p=128)
        for k in range(KT):
            mk.dma("sync", wt[:, k, :], wv[:, k, :], writes=["w%d" % k])
        wkeys = ["w%d" % k for k in range(KT)]
        if ln:
            gt = mk.sb("gt", [128, N])
            bt = mk.sb("bt", [128, N])
            mk.dma("sync", gt[:], lng[:, :], writes=["lng"])
            mk.dma("sync", bt[:], lnb[:, :], writes=["lnb"])
        NB = 2
        at = [mk.sb("at%d" % i, [128, KT, 128]) for i in range(NB)]
        yt = [mk.sb("yt%d" % i, [128, N]) for i in range(NB)]
        if ln:
            ht = [mk.sb("ht%d" % i, [128, N]) for i in range(NB)]
            ot = [mk.sb("ot%d" % i, [128, N]) for i in range(NB)]
            stt = [mk.sb("st%d" % i, [128, 4]) for i in range(NB)]
        if rope_units:
            ct = [mk.sb("ct%d" % i, [128, rope_half]) for i in range(NB)]
            snt = [mk.sb("snt%d" % i, [128, rope_half]) for i in range(NB)]
            rt = [mk.sb("rt%d" % i, [128, N]) for i in range(NB)]
            tmp = [mk.sb("tmp%d" % i, [128, N]) for i in range(NB)]
        NCH = (N + 511) // 512
        pst = [mk.ps("ps%d" % i, [128, 512]) for i in range(4)]
        if tm:
            identt = mk.sb("identt", [128, 128])
            mk.dma("sync", identt[:], ident_d[:, :], writes=["ident"])
            atok = [mk.sb("atok%d" % i, [128, K]) for i in range(NB)]
            if gather:
                idx_sb = mk.sb("idx_sb", [128, ntok // 128], mybir.dt.int32)
                mk.dma("poolq", idx_sb[:], io.tin("tokidx", [128, ntok // 128]), writes=["tokidx"])
        else:
            aTv = aT.rearrange("(k p) t -> p k t", p=128)
        pi = 0
        for tt in range(ntok // 128):
            b = tt % NB
            tsl = slice(tt * 128, (tt + 1) * 128)
            if tm:
                if gather:
                    mk.dma("poolq", atok[b][:], a_tok[:, :], reads=["tokidx"], writes=["atok%d" % b], gather_idx=idx_sb[:, tt:tt + 1])
                else:
                    mk.dma("sync", atok[b][:], a_tok[tsl, :], writes=["atok%d" % b])
                for k0 in range(0, KT, 4):
                    p = pst[pi % 4]
                    pk = "ps%d" % (pi % 4)
                    pi += 1
                    m = min(4, KT - k0)
                    for k in range(k0, k0 + m):
                        mk.op("pe", lambda e: e.matmul(p[:, (k - k0) * 128:(k - k0 + 1) * 128], lhsT=atok[b][:, k * 128:(k + 1) * 128], rhs=identt[:],
                                                       start=True, stop=True), reads=["atok%d" % b, "ident"], writes=[pk])
                    mk.op("dve", lambda e: e.tensor_copy(out=at[b][:, k0:k0 + m, :].rearrange("p a b -> p (a b)"), in_=p[:, 0:m * 128]),
                          reads=[pk], writes=["at%d" % b])
            else:
                mk.dma("sync", at[b][:], aTv[:, :, tsl], writes=["at%d" % b])
            if ln and gather:
                mk.dma("poolq", ht[b][:], hres[:, :], reads=["tokidx"], writes=["ht%d" % b], gather_idx=idx_sb[:, tt:tt + 1])
            elif ln:
                mk.dma("poolq", ht[b][:], hres[tsl, :], writes=["ht%d" % b])
            if rope_units:
                mk.dma("poolq", ct[b][:], cosd[tsl, :], writes=["ct%d" % b])
                mk.dma("poolq", snt[b][:], sind[tsl, :], writes=["snt%d" % b])
            for c in range(NCH):
                c0 = c * 512
                cw = min(512, N - c0)
                p = pst[pi % 4]
                pk = "ps%d" % (pi % 4)
                pi += 1
                for k in range(KT):
                    mk.op("pe", lambda e, k=k, p=p: e.matmul(p[:, :cw], lhsT=at[b][:, k, :], rhs=wt[:, k, c0:c0 + cw],
                                                             start=(k == 0), stop=(k == KT - 1)),
                          reads=["at%d" % b, wkeys[k]], writes=[pk])
                if ln:
                    mk.op("dve", lambda e, p=p: e.scalar_tensor_tensor(out=yt[b][:, c0:c0 + cw], in0=ht[b][:, c0:c0 + cw],
                                                                       scalar=float(ALPHA), in1=p[:, :cw],
                                                                       op0=ALU.mult, op1=ALU.add),
                          reads=[pk, "ht%d" % b], writes=["yt%d" % b])
                else:
                    mk.op("act", lambda e, p=p: e.copy(out=yt[b][:, c0:c0 + cw], in_=p[:, :cw]),
                          reads=[pk], writes=["yt%d" % b])
            if ln:
                emit_ln(mk, yt[b][:], "yt%d" % b, gt[:], bt[:], ot[b][:], "ot%d" % b, stt[b], "st%d" % b, n=N)
                mk.dma("sync", y[tsl, :], ot[b][:], reads=["ot%d" % b], is_output=True)
            else:
                if rope_units:
                    h = rope_half
                    mk.op("pool", lambda e: e.tensor_copy(out=rt[b][:], in_=yt[b][:]), reads=["yt%d" % b], writes=["rt%d" % b])
                    for (u0, nu) in rope_units:
                        src = yt[b][:, u0:u0 + nu * 2 * h].rearrange("p (u two d) -> p u two d", two=2, d=h)
                        dst = rt[b][:, u0:u0 + nu * 2 * h].rearrange("p (u two d) -> p u two d", two=2, d=h)
                        tm = tmp[b][:, u0:u0 + nu * 2 * h].rearrange("p (u two d) -> p u two d", two=2, d=h)
                        cb = ct[b][:].unsqueeze(1).to_broadcast([128, nu, h])
                        sb_ = snt[b][:].unsqueeze(1).to_broadcast([128, nu, h])
                        x1, x2 = src[:, :, 0, :], src[:, :, 1, :]
                        rk = ["yt%d" % b, "ct%d" % b, "snt%d" % b]
                        mk.op("dve", lambda e: e.tensor_tensor(out=dst[:, :, 0, :], in0=x1, in1=cb, op=ALU.mult), reads=rk, writes=["rt%d" % b])
                        mk.op("dve", lambda e: e.tensor_tensor(out=tm[:, :, 0, :], in0=x2, in1=sb_, op=ALU.mult), reads=rk, writes=["tmp%d" % b])
                        mk.op("dve", lambda e: e.tensor_tensor(out=dst[:, :, 0, :], in0=dst[:, :, 0, :], in1=tm[:, :, 0, :], op=ALU.subtract),
                              reads=["tmp%d" % b, "rt%d" % b], writes=["rt%d" % b])
                        mk.op("dve", lambda e: e.tensor_tensor(out=dst[:, :, 1, :], in0=x2, in1=cb, op=ALU.mult), reads=rk, writes=["rt%d" % b])
                        mk.op("dve", lambda e: e.tensor_tensor(out=tm[:, :, 1, :], in0=x1, in1=sb_, op=ALU.mult), reads=rk, writes=["tmp%d" % b])
                        mk.op("dve", lambda e: e.tensor_tensor(out=dst[:, :, 1, :], in0=dst[:, :, 1, :], in1=tm[:, :, 1, :], op=ALU.add),
                              reads=["tmp%d" % b, "rt%d" % b], writes=["rt%d" % b])
                    mk.dma("sync", yr[tsl, :], rt[b][:], reads=["rt%d" % b], is_output=True)
                mk.dma("sync", y[tsl, :], yt[b][:], reads=["yt%d" % b], is_output=True)


def emit_ffn(mk, io, F, NE, ntok=TC, TCH=256, tm=False):
    from contextlib import ExitStack
    nc = mk.nc
    D = D_MODEL
    KT = D // 128
    FT = F // 128
    if tm:
        ident_d = io.tin("ident", [128, 128])
    else:
        hT = io.tin("hT", [D, ntok])
    h = io.tin("h", [ntok, D])
    wg = io.tin("wg", [NE, FT, 128, KT, 128])
    wu = io.tin("wu", [NE, FT, 128, KT, 128])
    wd = io.tin("wd", [NE, FT, 128, D])
    lng = io.tin("lng", [128, D])
    lnb = io.tin("lnb", [128, D])
    if NE > 1:
        wr = io.tin("wr", [D, NE])
    y = io.tout("y", [ntok, D])
    NTT = TCH // 128
    with ExitStack() as st:
        gt = mk.sb("gt", [128, D])
        bt = mk.sb("bt", [128, D])
        mk.dma("sync", gt[:], lng[:, :], writes=["lng"])
        mk.dma("sync", bt[:], lnb[:, :], writes=["lnb"])
        if NE > 1:
            wrt = mk.sb("wrt", [128, KT, NE])
            mk.dma("sync", wrt[:], wr.rearrange("(k p) e -> p k e", p=128), writes=["wr"])
        NB = 2
        hTt = [mk.sb("hTt%d" % i, [128, KT, TCH]) for i in range(NB)]
        ht = [mk.sb("ht%d" % i, [128, NTT, D]) for i in range(NB)]
        wgt = [mk.sb("wgt%d" % i, [128, KT, 128]) for i in range(NB)]
        wut = [mk.sb("wut%d" % i, [128, KT, 128]) for i in range(NB)]
        wdt = [mk.sb("wdt%d" % i, [128, D]) for i in range(NB)]
        sg = [mk.sb("sg%d" % i, [128, TCH]) for i in range(NB)]
        ut = [mk.sb("ut%d" % i, [128, TCH]) for i in range(NB)]
        zt = [mk.sb("zt%d" % i, [128, D]) for i in range(NB)]
        ot = [mk.sb("ot%d" % i, [128, D]) for i in range(NB)]
        stt = [mk.sb("st%d" % i, [128, 4]) for i in range(NB)]
        if NE > 1:
            acc = [mk.sb("acc%d" % i, [128, D]) for i in range(NTT)]
            lg = [mk.sb("lg%d" % i, [128, NE]) for i in range(NTT)]
            mx = [mk.sb("mx%d" % i, [128, 8]) for i in range(NTT)]
            gate = [mk.sb("gate%d" % i, [128, NE]) for i in range(NTT)]
            gs = [mk.sb("gs%d" % i, [128, 2]) for i in range(NTT)]
        psg = [mk.ps("psg%d" % i, [128, 512]) for i in range(2)]
        psu = [mk.ps("psu%d" % i, [128, 512]) for i in range(2)]
        psy = [mk.ps("psy%d" % i, [128, 512]) for i in range(4)]
        if tm:
            identt = mk.sb("identt", [128, 128])
            mk.dma("sync", identt[:], ident_d[:, :], writes=["ident"])
        else:
            hTv = hT.rearrange("(k p) t -> p k t", p=128)
        wi = 0
        for ch in range(ntok // TCH):
            b = ch % NB
            t0 = ch * TCH
            mk.dma("poolq", ht[b][:], h[t0:t0 + TCH, :].rearrange("(j p) d -> p j d", p=128), writes=["ht%d" % b])
            if tm:
                tcount = 0
                for j in range(NTT):
                    for k0 in range(0, KT, 4):
                        p, pk = (psg[tcount % 2], "psg%d" % (tcount % 2)) if (tcount // 2) % 2 == 0 else (psu[tcount % 2], "psu%d" % (tcount % 2))
                        tcount += 1
                        for k in range(k0, k0 + 4):
                            mk.op("pe", lambda e: e.matmul(p[:, (k - k0) * 128:(k - k0 + 1) * 128], lhsT=ht[b][:, j, k * 128:(k + 1) * 128], rhs=identt[:],
                                                           start=True, stop=True), reads=["ht%d" % b, "ident"], writes=[pk])
                        for k in range(k0, k0 + 4):
                            mk.op("dve", lambda e: e.tensor_copy(out=hTt[b][:, k, j * 128:(j + 1) * 128], in_=p[:, (k - k0) * 128:(k - k0 + 1) * 128]),
                                  reads=[pk], writes=["hTt%d" % b])
            else:
                mk.dma("sync", hTt[b][:], hTv[:, :, t0:t0 + TCH], writes=["hTt%d" % b])
            if NE > 1:
                for j in range(NTT):
                    p = psg[0]
                    for k in range(KT):
                        mk.op("pe", lambda e, k=k, j=j: e.matmul(p[:, :NE], lhsT=hTt[b][:, k, j * 128:(j + 1) * 128], rhs=wrt[:, k, :],
                                                                 start=(k == 0), stop=(k == KT - 1)),
                              reads=["hTt%d" % b, "wr"], writes=["psg0"])
                    mk.op("dve", lambda e, j=j: e.tensor_copy(out=lg[j][:], in_=p[:, :NE]), reads=["psg0"], writes=["lg%d" % j])
                    mk.op("dve", lambda e, j=j: e.max(out=mx[j][:], in_=lg[j][:]), reads=["lg%d" % j], writes=["mx%d" % j])
                    mk.op("dve", lambda e, j=j: e.tensor_scalar(out=gs[j][:, 0:1], in0=mx[j][:, 0:1], scalar1=-1.0, scalar2=None, op0=ALU.mult),
                          reads=["mx%d" % j], writes=["gs%d" % j])
                    mk.op("act", lambda e, j=j: e.activation(out=gate[j][:], in_=lg[j][:], func=AF.Exp, bias=gs[j][:, 0:1], scale=1.0),
                          reads=["lg%d" % j, "gs%d" % j], writes=["gate%d" % j])
                    mk.op("act", lambda e, j=j: e.activation(out=gs[j][:, 1:2], in_=mx[j][:, 1:2], func=AF.Exp, bias=gs[j][:, 0:1], scale=1.0),
                          reads=["mx%d" % j, "gs%d" % j], writes=["gs%d" % j])
                    mk.op("dve", lambda e, j=j: e.tensor_scalar(out=gs[j][:, 1:2], in0=gs[j][:, 1:2], scalar1=1.0, scalar2=None, op0=ALU.add),
                          reads=["gs%d" % j], writes=["gs%d" % j])
                    mk.op("dve", lambda e, j=j: e.reciprocal(out=gs[j][:, 1:2], in_=gs[j][:, 1:2]), reads=["gs%d" % j], writes=["gs%d" % j])
                    mk.op("dve", lambda e, j=j: e.tensor_scalar(out=lg[j][:], in0=lg[j][:], scalar1=mx[j][:, 1:2], scalar2=gs[j][:, 1:2],
                                                                op0=ALU.is_ge, op1=ALU.mult),
                          reads=["lg%d" % j, "mx%d" % j, "gs%d" % j], writes=["lg%d" % j])
                    mk.op("dve", lambda e, j=j: e.tensor_tensor(out=gate[j][:], in0=gate[j][:], in1=lg[j][:], op=ALU.mult),
                          reads=["lg%d" % j, "gate%d" % j], writes=["gate%d" % j])
            for ex in range(NE):
                for f in range(FT):
                    wb = wi % NB
                    wi += 1
                    mk.dma("sync", wgt[wb][:], wg[ex, f], writes=["wgt%d" % wb])
                    mk.dma("poolq", wut[wb][:], wu[ex, f], writes=["wut%d" % wb])
                    mk.dma("sync", wdt[wb][:], wd[ex, f], writes=["wdt%d" % wb])
                    pg, pu = psg[wb], psu[wb]
                    for k in range(KT):
                        mk.op("pe", lambda e, k=k: e.matmul(pg[:, :TCH], lhsT=wgt[wb][:, k, :], rhs=hTt[b][:, k, :],
                                                            start=(k == 0), stop=(k == KT - 1)),
                              reads=["wgt%d" % wb, "hTt%d" % b], writes=["psg%d" % wb])
                    for k in range(KT):
                        mk.op("pe", lambda e, k=k: e.matmul(pu[:, :TCH], lhsT=wut[wb][:, k, :], rhs=hTt[b][:, k, :],
                                                            start=(k == 0), stop=(k == KT - 1)),
                              reads=["wut%d" % wb, "hTt%d" % b], writes=["psu%d" % wb])
                    mk.op("act", lambda e: e.activation(out=sg[wb][:], in_=pg[:, :TCH], func=AF.Silu),
                          reads=["psg%d" % wb], writes=["sg%d" % wb])
                    mk.op("dve", lambda e: e.tensor_tensor(out=ut[wb][:], in0=sg[wb][:], in1=pu[:, :TCH], op=ALU.mult),
                          reads=["sg%d" % wb, "psu%d" % wb], writes=["ut%d" % wb])
                    for j in range(NTT):
                        for nh in range(2):
                            mk.op("pe", lambda e, j=j, nh=nh: e.matmul(psy[j * 2 + nh][:, :], lhsT=ut[wb][:, j * 128:(j + 1) * 128],
                                                                       rhs=wdt[wb][:, nh * 512:(nh + 1) * 512],
                                                                       start=(f == 0), stop=(f == FT - 1)),
                                  reads=["ut%d" % wb, "wdt%d" % wb], writes=["psy%d" % (j * 2 + nh)])
                if NE > 1:
                    for j in range(NTT):
                        for nh in range(2):
                            sl = slice(nh * 512, (nh + 1) * 512)
                            if ex == 0:
                                mk.op("dve", lambda e, j=j, nh=nh, sl=sl: e.tensor_scalar(out=acc[j][:, sl], in0=psy[j * 2 + nh][:, :],
                                                                                          scalar1=gate[j][:, ex:ex + 1], scalar2=None, op0=ALU.mult),
                                      reads=["psy%d" % (j * 2 + nh), "gate%d" % j], writes=["acc%d" % j])
                            else:
                                mk.op("dve", lambda e, j=j, nh=nh, sl=sl: e.scalar_tensor_tensor(out=acc[j][:, sl], in0=psy[j * 2 + nh][:, :],
                                                                                                 scalar=gate[j][:, ex:ex + 1], in1=acc[j][:, sl],
                                                                                                 op0=ALU.mult, op1=ALU.add),
                                      reads=["psy%d" % (j * 2 + nh), "gate%d" % j, "acc%d" % j], writes=["acc%d" % j])
            for j in range(NTT):
                zb = (ch * NTT + j) % NB
                for nh in range(2):
                    sl = slice(nh * 512, (nh + 1) * 512)
                    if NE > 1:
                        mk.op("dve", lambda e, sl=sl: e.scalar_tensor_tensor(out=zt[zb][:, sl], in0=ht[b][:, j, sl], scalar=float(ALPHA),
                                                                             in1=acc[j][:, sl], op0=ALU.mult, op1=ALU.add),
                              reads=["ht%d" % b, "acc%d" % j], writes=["zt%d" % zb])
                    else:
                        mk.op("dve", lambda e, sl=sl, nh=nh: e.scalar_tensor_tensor(out=zt[zb][:, sl], in0=ht[b][:, j, sl], scalar=float(ALPHA),
                                                                                    in1=psy[j * 2 + nh][:, :], op0=ALU.mult, op1=ALU.add),
                              reads=["ht%d" % b, "psy%d" % (j * 2 + nh)], writes=["zt%d" % zb])
                emit_ln(mk, zt[zb][:], "zt%d" % zb, gt[:], bt[:], ot[zb][:], "ot%d" % zb, stt[zb], "st%d" % zb)
                mk.dma("sync", y[t0 + j * 128:t0 + (j + 1) * 128, :], ot[zb][:], reads=["ot%d" % zb], is_output=True)


def pretile_w_in(w, F):
    D = w.shape[0]
    return np.ascontiguousarray(w.reshape(D // 128, 128, F // 128, 128).transpose(2, 1, 0, 3))


def run(nc, in_maps):
    res = run_bass_kernel_spmd(nc, in_maps, core_ids=list(range(len(in_maps))))
    return res.results


def attn_consts():
    p = np.arange(128)
    tri_incl = (p[None, :] >= p[:, None]).astype(np.float32)
    tri_strict = (p[None, :] > p[:, None]).astype(np.float32)
    U = (p[:, None] > p[None, :]).astype(np.float32)
    ones = np.ones((128, 128), np.float32)
    return np.ascontiguousarray(np.stack([tri_incl, tri_strict, U, ones], 1))


def emit_attn(mk, io, kind, NH=4, S=SEQ, lam_init=0.0, tm=False):
    from contextlib import ExitStack
    nc = mk.nc
    QC = 512
    NQC = S // QC
    NKT = S // 128
    if tm:
        q_tok = io.tin("q_tok", [S, NH * 128])
        k_tok = io.tin("k_tok", [S, NH * 128])
        v_tok = io.tin("v_tok", [S, NH * 128])
        ident_d = io.tin("ident", [128, 128])
    else:
        qT = io.tin("qT", [NH, 128, S])
        kT = io.tin("kT", [NH, 128, S])
        v = io.tin("v", [NH, S, 128])
    cst = io.tin("cst", [128, 4, 128])
    o = io.tout("o", [S, NH * 128])
    if kind == "da":
        lam = io.tin("lam", [128, 256])
        gn = io.tin("gn", [128, 128])
    VW = 132 if kind == "da" else 128
    scale = (64 ** -0.5) if kind == "da" else (128 ** -0.5)
    with ExitStack() as st:
        ct = mk.sb("ct", [128, 4, 128])
        mk.dma("sync", ct[:], cst[:, :, :], writes=["cst"])
        tri_incl, tri_strict, U, ones = ct[:, 0, :], ct[:, 1, :], ct[:, 2, :], ct[:, 3, :]
        kt_sb = mk.sb("kt_sb", [128, S])
        if tm:
            identt = mk.sb("identt", [128, 128])
            mk.dma("sync", identt[:], ident_d[:, :], writes=["ident"])
            tmps = [(mk.sb("ltmp%d" % i, [128, 4, 128]), "ltmp%d" % i) for i in range(2)]
            psTr = mk.ps("psTr", [128, 512])
        v_sb = mk.sb("v_sb", [128, NKT, VW])
        NB = 2
        q_sb = [mk.sb("q_sb%d" % i, [128, QC]) for i in range(NB)]
        pT = [mk.sb("pT%d" % i, [128, QC]) for i in range(3 if kind == "sb" else 6)]
        NS = 3 if kind == "sb" else 4
        psS = [mk.ps("psS%d" % i, [128, 512]) for i in range(NS)]
        o_sb = [mk.sb("o_sb%d" % i, [128, 4, 128]) for i in range(NB)]
        if kind == "da":
            lt = mk.sb("lt", [128, 256])
            gnt = mk.sb("gnt", [128, 128])
            lsc = mk.sb("lsc", [128, 8])
            mk.dma("sync", lt[:], lam[:, :], writes=["lam"])
            mk.dma("sync", gnt[:], gn[:, :], writes=["gn"])
            tmpl = mk.sb("tmpl", [128, 128])
            mk.op("dve", lambda e: e.tensor_tensor(out=tmpl[:, 0:64], in0=lt[:, 0:64], in1=lt[:, 64:128], op=ALU.mult), reads=["lam"], writes=["tmpl"])
            mk.op("dve", lambda e: e.tensor_tensor(out=tmpl[:, 64:128], in0=lt[:, 128:192], in1=lt[:, 192:256], op=ALU.mult), reads=["lam"], writes=["tmpl"])
            mk.op("dve", lambda e: e.tensor_reduce(out=lsc[:, 0:1], in_=tmpl[:, 0:64], axis=AX.X, op=ALU.add), reads=["tmpl"], writes=["lsc"])
            mk.op("dve", lambda e: e.tensor_reduce(out=lsc[:, 1:2], in_=tmpl[:, 64:128], axis=AX.X, op=ALU.add), reads=["tmpl"], writes=["lsc"])
            mk.op("act", lambda e: e.activation(out=lsc[:, 2:4], in_=lsc[:, 0:2], func=AF.Exp), reads=["lsc"], writes=["lsc"])
            mk.op("dve", lambda e: e.scalar_tensor_tensor(out=lsc[:, 4:5], in0=lsc[:, 3:4], scalar=-float(lam_init), in1=lsc[:, 2:3],
                                                          op0=ALU.add, op1=ALU.subtract), reads=["lsc"], writes=["lsc"])
            neglmb = lsc[:, 4:5]
            acc = [mk.ps("acc%d" % i, [128, 512]) for i in range(3)]
            fin = [mk.sb("fin%d" % i, [128, 8]) for i in range(NB)]
            o1 = [mk.sb("o1_%d" % i, [128, 128]) for i in range(NB)]
            o2 = [mk.sb("o2_%d" % i, [128, 128]) for i in range(NB)]
            sq = [mk.sb("sq%d" % i, [128, 128]) for i in range(NB)]

            def acc_ap(c, jq):
                if jq < 3:
                    return acc[c][:, jq * VW:(jq + 1) * VW], "acc%d" % c
                return acc[2][:, c * VW:(c + 1) * VW], "acc2"
        else:
            psA = [mk.ps("psA%d" % i, [128, 512]) for i in range(2)]
            acc1 = [mk.ps("accs%d" % i, [128, 512]) for i in range(2)]
            E_sb = [mk.sb("E%d" % i, [128, QC]) for i in range(3)]
            sp_sb = [mk.sb("sp%d" % i, [128, QC]) for i in range(3)]
            tm_sb = [mk.sb("tm%d" % i, [128, QC]) for i in range(3)]
            C_sb = mk.sb("C_sb", [128, QC])
        it = 0
        for hd in range(NH):
            hcs = slice(hd * 128, (hd + 1) * 128)
            if tm:
                emit_loadT(mk, kt_sb[:], "kt_sb", k_tok[:, hcs], NKT, tmps, psTr, "psTr", identt[:])
            else:
                for c4 in range(4):
                    sl = slice(c4 * (S // 4), (c4 + 1) * (S // 4))
                    mk.dma("sync", kt_sb[:, sl], kT[hd, :, sl], writes=["kt_sb"])
            for c4 in range(4):
                k0, k1 = c4 * (NKT // 4), (c4 + 1) * (NKT // 4)
                vsrc = v_tok[k0 * 128:k1 * 128, hcs] if tm else v[hd, k0 * 128:k1 * 128, :]
                mk.dma("poolq", v_sb[:, k0:k1, 0:128], vsrc.rearrange("(kt p) d -> p kt d", p=128), writes=["v_sb"])
            if kind == "da" and hd == 0:
                mk.op("dve", lambda e: e.memset(v_sb[:, :, 128:VW], 1.0), writes=["v_sb"])
            for qc in range(NQC):
                qb = (hd * NQC + qc) % NB
                if tm:
                    emit_loadT(mk, q_sb[qb][:], "q_sb%d" % qb, q_tok[qc * QC:(qc + 1) * QC, hcs], QC // 128, tmps, psTr, "psTr", identt[:])
                else:
                    mk.dma("sync", q_sb[qb][:], qT[hd, :, qc * QC:(qc + 1) * QC], writes=["q_sb%d" % qb])
                nkt = 4 * qc + 4
                ob = (hd * NQC + qc) % NB
                if kind == "da":
                    stA, stB = [], []
                    for kt in range(nkt):
                        def mk_item(kt=kt, it=it):
                            r = max(0, kt - 4 * qc)
                            c0 = 128 * r
                            bufs = []
                            for c in range(2):
                                bi = (it % 2) * 2 + c
                                pi_ = (it % 3) * 2 + c
                                bufs.append((psS[bi], "psS%d" % bi, pT[pi_], "pT%d" % pi_, slice(c * 64, (c + 1) * 64)))

                            def A():
                                for (pS, pk, pt, ptk, ps_) in bufs:
                                    mk.op("pe", lambda e: e.matmul(pS[:, c0:QC], lhsT=kt_sb[ps_, kt * 128:(kt + 1) * 128], rhs=q_sb[qb][ps_, c0:QC],
                                                                   start=True, stop=True),
                                          reads=["kt_sb", "q_sb%d" % qb], writes=[pk])
                                for (pS, pk, pt, ptk, ps_) in bufs:
                                    mk.op("act", lambda e: e.activation(out=pt[:, c0:QC], in_=pS[:, c0:QC], func=AF.Exp, scale=float(scale)),
                                          reads=[pk], writes=[ptk])
                                    if kt >= 4 * qc:
                                        mk.op("pool", lambda e: e.tensor_tensor(out=pt[:, c0:c0 + 128], in0=pt[:, c0:c0 + 128], in1=tri_incl, op=ALU.mult),
                                              reads=[ptk, "cst"], writes=[ptk])

                            def B():
                                for c, (pS, pk, pt, ptk, ps_) in enumerate(bufs):
                                    for jq in range(r, 4):
                                        ap, ak = acc_ap(c, jq)
                                        first = (kt == 0 and jq == 0) or (kt == 0 and jq == 3 and c == 0)
                                        mk.op("pe", lambda e: e.matmul(ap, lhsT=pt[:, jq * 128:(jq + 1) * 128], rhs=v_sb[:, kt, :],
                                                                       start=first, stop=(kt == 4 * qc + jq), skip_group_check=True),
                                              reads=[ptk, "v_sb"], writes=[ak])
                            return A, B
                        a_, b_ = mk_item()
                        it += 1
                        stA.append(a_)
                        stB.append(b_)
                    emit_skewed([stA, [(lambda: None)] * len(stA), stB])
                    for jq in range(4):
                        fb = (qc * 4 + jq) % NB
                        fk = "fin%d" % fb
                        a0, a0k = acc_ap(0, jq)
                        a1, a1k = acc_ap(1, jq)
                        mk.op("dve", lambda e: e.reciprocal(out=fin[fb][:, 0:1], in_=a0[:, 128:129]), reads=[a0k], writes=[fk])
                        mk.op("dve", lambda e: e.reciprocal(out=fin[fb][:, 1:2], in_=a1[:, 128:129]), reads=[a1k], writes=[fk])
                        mk.op("dve", lambda e: e.tensor_tensor(out=fin[fb][:, 1:2], in0=fin[fb][:, 1:2], in1=neglmb, op=ALU.mult),
                              reads=[fk, "lsc"], writes=[fk])
                        mk.op("dve", lambda e: e.tensor_scalar(out=o1[fb][:], in0=a0[:, 0:128], scalar1=fin[fb][:, 0:1], scalar2=None, op0=ALU.mult),
                              reads=[a0k, fk], writes=["o1_%d" % fb])
                        mk.op("dve", lambda e: e.scalar_tensor_tensor(out=o2[fb][:], in0=a1[:, 0:128], scalar=fin[fb][:, 1:2], in1=o1[fb][:],
                                                                      op0=ALU.mult, op1=ALU.add),
                              reads=[a1k, fk, "o1_%d" % fb], writes=["o2_%d" % fb])
                        mk.op("act", lambda e: e.activation(out=sq[fb][:], in_=o2[fb][:], func=AF.Square, accum_out=fin[fb][:, 2:3]),
                              reads=["o2_%d" % fb], writes=["sq%d" % fb, fk])
                        mk.op("dve", lambda e: e.tensor_scalar(out=fin[fb][:, 3:4], in0=fin[fb][:, 2:3], scalar1=1.0 / 128, scalar2=LN_EPS,
                                                               op0=ALU.mult, op1=ALU.add), reads=[fk], writes=[fk])
                        mk.op("act", lambda e: e.activation(out=fin[fb][:, 3:4], in_=fin[fb][:, 3:4], func=AF.Sqrt), reads=[fk], writes=[fk])
                        mk.op("dve", lambda e: e.reciprocal(out=fin[fb][:, 3:4], in_=fin[fb][:, 3:4]), reads=[fk], writes=[fk])
                        mk.op("dve", lambda e: e.tensor_scalar(out=fin[fb][:, 3:4], in0=fin[fb][:, 3:4], scalar1=float(1.0 - lam_init), scalar2=None,
                                                               op0=ALU.mult), reads=[fk], writes=[fk])
                        mk.op("dve", lambda e: e.scalar_tensor_tensor(out=o_sb[ob][:, jq, :], in0=o2[fb][:], scalar=fin[fb][:, 3:4], in1=gnt[:],
                                                                      op0=ALU.mult, op1=ALU.mult),
                              reads=["o2_%d" % fb, fk, "gn"], writes=["o_sb%d" % ob])
                else:
                    ac = acc1[(hd * NQC + qc) % 2]
                    ack = "accs%d" % ((hd * NQC + qc) % 2)
                    mk.op("pool", lambda e: e.memset(C_sb[:], 0.0), writes=["C_sb"])
                    stA, stB, stC = [], [], []
                    for kt in range(nkt - 1, -1, -1):
                        def mk_pair(kt=kt, it=it):
                            r = max(0, kt - 4 * qc)
                            c0 = 128 * r
                            diag = kt >= 4 * qc
                            pS = psS[it % NS]
                            pk = "psS%d" % (it % NS)
                            pA = psA[it % 2]
                            pak = "psA%d" % (it % 2)
                            pt = pT[it % 3]
                            ptk = "pT%d" % (it % 3)
                            Eb, Ek = E_sb[it % 3], "E%d" % (it % 3)
                            spb, spk = sp_sb[it % 3], "sp%d" % (it % 3)
                            tmb, tmk = tm_sb[it % 3], "tm%d" % (it % 3)

                            def A():
                                mk.op("pe", lambda e: e.matmul(pS[:, c0:QC], lhsT=kt_sb[:, kt * 128:(kt + 1) * 128], rhs=q_sb[qb][:, c0:QC],
                                                               start=True, stop=True),
                                      reads=["kt_sb", "q_sb%d" % qb], writes=[pk])
                                mk.op("act", lambda e: e.activation(out=Eb[:, c0:QC], in_=pS[:, c0:QC], func=AF.Exp, scale=float(scale)),
                                      reads=[pk], writes=[Ek])
                                mk.op("act", lambda e: e.activation(out=spb[:, c0:QC], in_=Eb[:, c0:QC], func=AF.Ln, bias=1.0, scale=1.0),
                                      reads=[Ek], writes=[spk])
                                if diag:
                                    mk.op("pool", lambda e: e.tensor_tensor(out=spb[:, c0:c0 + 128], in0=spb[:, c0:c0 + 128], in1=tri_strict, op=ALU.mult),
                                          reads=[spk, "cst"], writes=[spk])

                            def B():
                                mk.op("pe", lambda e: e.matmul(pA[:, c0:QC], lhsT=U, rhs=spb[:, c0:QC], start=True, stop=False),
                                      reads=[spk, "cst"], writes=[pak])
                                mk.op("pe", lambda e: e.matmul(pA[:, c0:QC], lhsT=ones, rhs=C_sb[:, c0:QC], start=False, stop=True),
                                      reads=["C_sb", "cst"], writes=[pak])
                                mk.op("pool", lambda e: e.tensor_tensor(out=C_sb[:, c0:QC], in0=C_sb[:, c0:QC], in1=spb[:, c0:QC], op=ALU.add),
                                      reads=["C_sb", spk], writes=["C_sb"])
                                mk.op("dve", lambda e: e.scalar_tensor_tensor(out=tmb[:, c0:QC], in0=pS[:, c0:QC], scalar=float(scale), in1=spb[:, c0:QC],
                                                                              op0=ALU.mult, op1=ALU.subtract),
                                      reads=[pk, spk], writes=[tmk])
                                mk.op("dve", lambda e: e.tensor_tensor(out=tmb[:, c0:QC], in0=tmb[:, c0:QC], in1=pA[:, c0:QC], op=ALU.subtract),
                                      reads=[tmk, pak], writes=[tmk])
                                mk.op("act", lambda e: e.activation(out=pt[:, c0:QC], in_=tmb[:, c0:QC], func=AF.Exp), reads=[tmk], writes=[ptk])
                                if diag:
                                    mk.op("pool", lambda e: e.tensor_tensor(out=pt[:, c0:c0 + 128], in0=pt[:, c0:c0 + 128], in1=tri_strict, op=ALU.mult),
                                          reads=[ptk, "cst"], writes=[ptk])

                            def C():
                                for jq in range(r, 4):
                                    mk.op("pe", lambda e: e.matmul(ac[:, jq * 128:(jq + 1) * 128], lhsT=pt[:, jq * 128:(jq + 1) * 128], rhs=v_sb[:, kt, :],
                                                                   start=(kt == nkt - 1), stop=(kt == 0), skip_group_check=True),
                                          reads=[ptk, "v_sb"], writes=[ack])
                            return A, B, C
                        a_, b_, c_ = mk_pair()
                        it += 1
                        stA.append(a_)
                        stB.append(b_)
                        stC.append(c_)
                    emit_skewed([stA, stB, stC])
                    mk.op("act", lambda e: e.copy(out=o_sb[ob][:].rearrange("p a b -> p (a b)"), in_=ac[:, :]), reads=[ack], writes=["o_sb%d" % ob])
                mk.dma("sync", o[qc * QC:(qc + 1) * QC, hd * 128:(hd + 1) * 128].rearrange("(j p) d -> p j d", p=128), o_sb[ob][:],
                       reads=["o_sb%d" % ob], is_output=True)


def hg_consts(W):
    p = np.arange(128)
    m = np.ones((128, W), np.float32)
    m[:, ::64] = 0.0
    tri = (p[None, :] >= p[:, None]).astype(np.float32)
    ident = np.eye(128, dtype=np.float32)
    return m, np.ascontiguousarray(np.stack([tri, ident], 1))


def emit_hgrn(mk, io, NH=4, S=SEQ, layer=0, W=2048, tm=False):
    from contextlib import ExitStack
    nc = mk.nc
    C = 64
    NW = S // W
    NCW = W // C
    if tm:
        q_tok = io.tin("q_tok", [S, NH * 128])
        f_tok = io.tin("f_tok", [S, NH * 128])
        v_tok = io.tin("v_tok", [S, NH * 128])
        g_tok = io.tin("g_tok", [S, NH * 128])
    else:
        qT = io.tin("qT", [NH, 128, S])
        fT = io.tin("fT", [NH, 128, S])
        v = io.tin("v", [NH, S, 128])
        g = io.tin("g", [NH, S, 128])
    lbT = io.tin("lbT", [NH, 128, 5])
    gn = io.tin("gn", [128, 128])
    msk = io.tin("msk", [128, W])
    cst = io.tin("cst", [128, 2, 128])
    o = io.tout("o", [S, NH * 128])
    with ExitStack() as st:
        ct = mk.sb("ct", [128, 2, 128])
        mt = mk.sb("mt", [128, W])
        gnt = mk.sb("gnt", [128, 128])
        mk.dma("sync", ct[:], cst[:, :, :], writes=["cst"])
        mk.dma("sync", mt[:], msk[:, :], writes=["msk"])
        mk.dma("sync", gnt[:], gn[:, :], writes=["gn"])
        tri, ident = ct[:, 0, :], ct[:, 1, :]
        lbt = mk.sb("lbt", [128, 5])
        lbs = mk.sb("lbs", [128, 8])
        q_sb = mk.sb("q_sb", [128, W])
        f_sb = mk.sb("f_sb", [128, W])
        kk_sb = mk.sb("kk_sb", [128, W])
        cum_sb = mk.sb("cum_sb", [128, W])
        E_sb = mk.sb("E_sb", [128, W])
        En_sb = mk.sb("En_sb", [128, W])
        qt_sb = mk.sb("qt_sb", [128, W])
        kt_sb = mk.sb("kt_sb", [128, W])
        v_sb = mk.sb("v_sb", [64, NCW, 128])
        g_sb = mk.sb("g_sb", [64, NCW, 128])
        o_sb = mk.sb("o_sb", [64, NCW, 128])
        state = [mk.sb("state%d" % i, [128, 128]) for i in range(2)]
        st1 = mk.sb("st1", [128, 128])
        ktok = [mk.sb("ktok%d" % i, [64, 128]) for i in range(2)]
        AT = [mk.sb("AT%d" % i, [64, 64]) for i in range(2)]
        sq = [mk.sb("sq%d" % i, [64, 128]) for i in range(2)]
        fin = [mk.sb("fin%d" % i, [64, 4]) for i in range(2)]
        psT = [mk.ps("psT%d" % i, [128, 512]) for i in range(2)]
        psS = [mk.ps("psS%d" % i, [128, 512]) for i in range(2)]
        psO = [mk.ps("psO%d" % i, [128, 512]) for i in range(2)]
        if tm:
            psD0 = mk.ps("psD0", [128, 512])
            psD = [psD0, psD0]
            psTr = mk.ps("psTr", [128, 512])
            tmps = [(mk.sb("ltmp%d" % i, [128, 4, 128]), "ltmp%d" % i) for i in range(2)]
        else:
            psD = [mk.ps("psD%d" % i, [128, 512]) for i in range(2)]
        NPD = 1 if tm else 2
        it = 0
        for hd in range(NH):
            mk.dma("sync", lbt[:], lbT[hd], writes=["lbt"])
            mk.op("act", lambda e: e.activation(out=lbt[:], in_=lbt[:], func=AF.Exp), reads=["lbt"], writes=["lbt"])
            mk.op("dve", lambda e: e.tensor_reduce(out=lbs[:, 0:1], in_=lbt[:], axis=AX.X, op=ALU.add), reads=["lbt"], writes=["lbs"])
            mk.op("dve", lambda e: e.tensor_reduce(out=lbs[:, 1:2], in_=lbt[:, 0:layer + 1], axis=AX.X, op=ALU.add), reads=["lbt"], writes=["lbs"])
            mk.op("dve", lambda e: e.reciprocal(out=lbs[:, 0:1], in_=lbs[:, 0:1]), reads=["lbs"], writes=["lbs"])
            mk.op("dve", lambda e: e.tensor_tensor(out=lbs[:, 2:3], in0=lbs[:, 1:2], in1=lbs[:, 0:1], op=ALU.mult), reads=["lbs"], writes=["lbs"])
            mk.op("dve", lambda e: e.tensor_scalar(out=lbs[:, 3:4], in0=lbs[:, 2:3], scalar1=-1.0, scalar2=1.0, op0=ALU.mult, op1=ALU.add),
                  reads=["lbs"], writes=["lbs"])
            lb, oml = lbs[:, 2:3], lbs[:, 3:4]
            mk.op("dve", lambda e: e.memset(state[0][:], 0.0), writes=["state0"])
            for w in range(NW):
                t0 = w * W
                hcs = slice(hd * 128, (hd + 1) * 128)
                if tm:
                    emit_loadT(mk, q_sb[:], "q_sb", q_tok[t0:t0 + W, hcs], W // 128, tmps, psTr, "psTr", ident, identk="cst")
                    emit_loadT(mk, f_sb[:], "f_sb", f_tok[t0:t0 + W, hcs], W // 128, tmps, psTr, "psTr", ident, identk="cst")
                    mk.dma("poolq", v_sb[:], v_tok[t0:t0 + W, hcs].rearrange("(c p) d -> p c d", p=64), writes=["v_sb"])
                    mk.dma("poolq", g_sb[:], g_tok[t0:t0 + W, hcs].rearrange("(c p) d -> p c d", p=64), writes=["g_sb"])
                else:
                    mk.dma("sync", q_sb[:], qT[hd, :, t0:t0 + W], writes=["q_sb"])
                    mk.dma("sync", f_sb[:], fT[hd, :, t0:t0 + W], writes=["f_sb"])
                    mk.dma("poolq", v_sb[:], v[hd, t0:t0 + W, :].rearrange("(c p) d -> p c d", p=64), writes=["v_sb"])
                    mk.dma("poolq", g_sb[:], g[hd, t0:t0 + W, :].rearrange("(c p) d -> p c d", p=64), writes=["g_sb"])
                mk.op("act", lambda e: e.activation(out=f_sb[:], in_=f_sb[:], func=AF.Sigmoid), reads=["f_sb"], writes=["f_sb"])
                mk.op("dve", lambda e: e.tensor_scalar(out=f_sb[:], in0=f_sb[:], scalar1=oml, scalar2=lb, op0=ALU.mult, op1=ALU.add),
                      reads=["f_sb", "lbs"], writes=["f_sb"])
                mk.op("dve", lambda e: e.tensor_scalar(out=kk_sb[:], in0=f_sb[:], scalar1=-1.0, scalar2=1.0, op0=ALU.mult, op1=ALU.add),
                      reads=["f_sb"], writes=["kk_sb"])
                mk.op("act", lambda e: e.activation(out=f_sb[:], in_=f_sb[:], func=AF.Ln), reads=["f_sb"], writes=["f_sb"])
                mk.op("dve", lambda e: e.tensor_tensor_scan(out=cum_sb[:], data0=mt[:], data1=f_sb[:], initial=0.0, op0=ALU.mult, op1=ALU.add),
                      reads=["f_sb", "msk"], writes=["cum_sb"])
                mk.op("act", lambda e: e.activation(out=E_sb[:], in_=cum_sb[:], func=AF.Exp), reads=["cum_sb"], writes=["E_sb"])
                mk.op("act", lambda e: e.activation(out=En_sb[:], in_=cum_sb[:], func=AF.Exp, scale=-1.0), reads=["cum_sb"], writes=["En_sb"])
                mk.op("act", lambda e: e.activation(out=q_sb[:], in_=q_sb[:], func=AF.Silu), reads=["q_sb"], writes=["q_sb"])
                mk.op("act", lambda e: e.activation(out=g_sb[:], in_=g_sb[:], func=AF.Sigmoid), reads=["g_sb"], writes=["g_sb"])
                mk.op("dve", lambda e: e.tensor_tensor(out=qt_sb[:], in0=q_sb[:], in1=E_sb[:], op=ALU.mult), reads=["q_sb", "E_sb"], writes=["qt_sb"])
                mk.op("pool", lambda e: e.tensor_tensor(out=kt_sb[:], in0=kk_sb[:], in1=En_sb[:], op=ALU.mult), reads=["kk_sb", "En_sb"], writes=["kt_sb"])
                for c in range(NCW):
                    cs = slice(c * C, (c + 1) * C)
                    i2 = it % 2
                    gi = hd * (S // C) + w * NCW + c
                    sc, sn = state[gi % 2], state[(gi + 1) % 2]
                    sck, snk = "state%d" % (gi % 2), "state%d" % ((gi + 1) % 2)
                    it += 1
                    mk.op("pe", lambda e: e.matmul(psT[i2][0:64, 0:128], lhsT=kt_sb[:, cs], rhs=ident, start=True, stop=True),
                          reads=["kt_sb", "cst"], writes=["psT%d" % i2])
                    mk.op("act", lambda e: e.copy(out=ktok[i2][:], in_=psT[i2][0:64, 0:128]), reads=["psT%d" % i2], writes=["ktok%d" % i2])
                    mk.op("pe", lambda e: e.matmul(psS[i2][0:64, 0:64], lhsT=kt_sb[:, cs], rhs=qt_sb[:, cs], start=True, stop=True),
                          reads=["kt_sb", "qt_sb"], writes=["psS%d" % i2])
                    mk.op("dve", lambda e: e.tensor_tensor(out=AT[i2][:], in0=psS[i2][0:64, 0:64], in1=tri[0:64, 0:64], op=ALU.mult),
                          reads=["psS%d" % i2, "cst"], writes=["AT%d" % i2])
                    mk.op("pe", lambda e: e.matmul(psO[i2][0:64, 0:128], lhsT=AT[i2][:], rhs=v_sb[:, c, :], start=True, stop=False),
                          reads=["AT%d" % i2, "v_sb"], writes=["psO%d" % i2])
                    mk.op("pe", lambda e: e.matmul(psO[i2][0:64, 0:128], lhsT=qt_sb[:, cs], rhs=sc[:], start=False, stop=True),
                          reads=["qt_sb", sck], writes=["psO%d" % i2])
                    mk.op("pe", lambda e: e.matmul(psD[i2][:, 0:128], lhsT=ktok[i2][:], rhs=v_sb[:, c, :], start=True, stop=True),
                          reads=["ktok%d" % i2, "v_sb"], writes=["psD%d" % (i2 % NPD)])
                    el = E_sb[:, c * C + C - 1:c * C + C]
                    mk.op("pool", lambda e: e.tensor_scalar(out=st1[:], in0=sc[:], scalar1=el, scalar2=None, op0=ALU.mult),
                          reads=[sck, "E_sb"], writes=["st1"])
                    mk.op("dve", lambda e: e.scalar_tensor_tensor(out=sn[:], in0=psD[i2][:, 0:128], scalar=el, in1=st1[:], op0=ALU.mult, op1=ALU.add),
                          reads=["psD%d" % (i2 % NPD), "st1", "E_sb"], writes=[snk])
                    fk = "fin%d" % i2
                    mk.op("act", lambda e: e.activation(out=sq[i2][:], in_=psO[i2][0:64, 0:128], func=AF.Square, accum_out=fin[i2][:, 0:1]),
                          reads=["psO%d" % i2], writes=["sq%d" % i2, fk])
                    mk.op("dve", lambda e: e.tensor_scalar(out=fin[i2][:, 1:2], in0=fin[i2][:, 0:1], scalar1=1.0 / 128, scalar2=LN_EPS, op0=ALU.mult, op1=ALU.add),
                          reads=[fk], writes=[fk])
                    mk.op("act", lambda e: e.activation(out=fin[i2][:, 1:2], in_=fin[i2][:, 1:2], func=AF.Sqrt), reads=[fk], writes=[fk])
                    mk.op("dve", lambda e: e.reciprocal(out=fin[i2][:, 1:2], in_=fin[i2][:, 1:2]), reads=[fk], writes=[fk])
                    mk.op("dve", lambda e: e.scalar_tensor_tensor(out=sq[i2][:], in0=psO[i2][0:64, 0:128], scalar=fin[i2][:, 1:2], in1=gnt[0:64, :],
                                                                  op0=ALU.mult, op1=ALU.mult),
                          reads=["psO%d" % i2, fk, "gn", "sq%d" % i2], writes=["sq%d" % i2])
                    mk.op("pool", lambda e: e.tensor_tensor(out=o_sb[:, c, :], in0=sq[i2][:], in1=g_sb[:, c, :], op=ALU.mult),
                          reads=["sq%d" % i2, "g_sb"], writes=["o_sb"])
                mk.dma("sync", o[t0:t0 + W, hd * 128:(hd + 1) * 128].rearrange("(c p) d -> p c d", p=64), o_sb[:], reads=["o_sb"], is_output=True)


def _standalone(emit, *args, **kw):
    from contextlib import ExitStack
    nc = bass.Bass("TRN2", target_bir_lowering=False)
    with ExitStack() as st:
        mk = MK(nc, st)
        emit(mk, IO(nc), *args, **kw)
        mk.finish()
    return nc


def build_gemm(K, N, ln=False, rope_units=None, rope_half=0, ntok=TC):
    return _standalone(emit_gemm, K, N, ln=ln, rope_units=rope_units, rope_half=rope_half, ntok=ntok)


def build_ffn(F, NE, ntok=TC, TCH=256):
    return _standalone(emit_ffn, F, NE, ntok=ntok, TCH=TCH)


def build_attn(kind, NH=4, S=SEQ, lam_init=0.0):
    return _standalone(emit_attn, kind, NH=NH, S=S, lam_init=lam_init)


def build_hgrn(NH=4, S=SEQ, layer=0, W=2048):
    return _standalone(emit_hgrn, NH=NH, S=S, layer=layer, W=W)


def build_nsa(S=SEQ):
    return _standalone(emit_nsa, S=S)


_PROGS = {}


def _prog(key, fn):
    if key not in _PROGS:
        _PROGS[key] = fn()
    return _PROGS[key]


def _c(a):
    return np.ascontiguousarray(a, dtype=np.float32)


def _rep(vec, n=128):
    return _c(np.broadcast_to(np.asarray(vec).reshape(1, -1), (n, np.asarray(vec).size)))


def _rope_tables(half):
    pos = np.arange(SEQ, dtype=np.float32)
    inv = (10000.0 ** (-np.arange(half, dtype=np.float32) / half)).astype(np.float32)
    ang = pos[:, None] * inv[None, :]
    return np.cos(ang).astype(np.float32), np.sin(ang).astype(np.float32)


def run_proj(h, w, rope_units=None, rope_half=0):
    N = w.shape[1]
    key = ("gemm", w.shape[0], N, False, tuple(rope_units or ()), rope_half)
    nc = _prog(key, lambda: build_gemm(w.shape[0], N, ln=False, rope_units=rope_units, rope_half=rope_half))
    ims = []
    if rope_units:
        cos, sin = _rope_tables(rope_half)
    for c in range(NCORES):
        sl = slice(c * TC, (c + 1) * TC)
        im = {"aT": _c(h[sl].T), "w": _c(w)}
        if rope_units:
            p0 = (c * TC) % SEQ
            im["cos"] = _c(cos[p0:p0 + TC])
            im["sin"] = _c(sin[p0:p0 + TC])
        ims.append(im)
    res = run(nc, ims)
    y = np.concatenate([r["y"] for r in res], 0)
    yr = np.concatenate([r["yr"] for r in res], 0) if rope_units else None
    return y, yr


def run_lrln(a, w, h, g, b):
    key = ("gemm", w.shape[0], w.shape[1], True)
    nc = _prog(key, lambda: build_gemm(w.shape[0], w.shape[1], ln=True))
    ims = []
    for c in range(NCORES):
        sl = slice(c * TC, (c + 1) * TC)
        ims.append({"aT": _c(a[sl].T), "w": _c(w), "hres": _c(h[sl]), "lng": _rep(g), "lnb": _rep(b)})
    res = run(nc, ims)
    return np.concatenate([r["y"] for r in res], 0)


def run_ffn(h, wg, wu, wd, g, b, wr=None):
    NE = 1 if wr is None else wg.shape[0]
    F = wg.shape[-1]
    nc = _prog(("ffn", F, NE), lambda: build_ffn(F, NE))
    if NE == 1:
        wg, wu, wd = wg[None], wu[None], wd[None]
    wgt = np.stack([pretile_w_in(wg[e], F) for e in range(NE)])
    wut = np.stack([pretile_w_in(wu[e], F) for e in range(NE)])
    wdt = _c(wd.reshape(NE, F // 128, 128, D_MODEL))
    lng, lnb = _rep(g), _rep(b)
    ims = []
    for c in range(NCORES):
        sl = slice(c * TC, (c + 1) * TC)
        im = {"hT": _c(h[sl].T), "h": _c(h[sl]), "wg": wgt, "wu": wut, "wd": wdt, "lng": lng, "lnb": lnb}
        if NE > 1:
            im["wr"] = _c(wr)
        ims.append(im)
    res = run(nc, ims)
    return np.concatenate([r["y"] for r in res], 0)


def _heads_T(x, b, hh):
    xb = x.reshape(BATCH, SEQ, N_HEADS, HEAD_DIM)[b, :, 4 * hh:4 * hh + 4, :]
    return _c(xb.transpose(1, 2, 0))


def _heads_tok(x, b, hh):
    xb = x.reshape(BATCH, SEQ, N_HEADS, HEAD_DIM)[b, :, 4 * hh:4 * hh + 4, :]
    return _c(xb.transpose(1, 0, 2))


def _gather_o(res):
    o = np.zeros((BATCH, SEQ, D_MODEL), np.float32)
    for c in range(NCORES):
        o[c // 2, :, (c % 2) * 512:(c % 2 + 1) * 512] = res[c]["o"]
    return o.reshape(T_ALL, D_MODEL)


def mixer_hgrn(h, w_in, lb, norm_g, layer):
    proj, _ = run_proj(h, w_in)
    D = D_MODEL
    q, f, i, g = proj[:, 0:D], proj[:, D:2 * D], proj[:, 2 * D:3 * D], proj[:, 3 * D:4 * D]
    W = 2048
    nc = _prog(("hgrn", layer), lambda: build_hgrn(NH=4, S=SEQ, layer=layer, W=W))
    m, cst = hg_consts(W)
    ims = []
    for c in range(NCORES):
        b, hh = c // 2, c % 2
        lbT = _c(lb.T.reshape(N_HEADS, 128, lb.shape[0])[4 * hh:4 * hh + 4])
        ims.append({"qT": _heads_T(q, b, hh), "fT": _heads_T(f, b, hh), "v": _heads_tok(i, b, hh), "g": _heads_tok(g, b, hh),
                    "lbT": lbT, "gn": _rep(norm_g), "msk": m, "cst": cst})
    return _gather_o(run(nc, ims))


def mixer_da(h, w_in, lam, norm_g, layer):
    proj, projr = run_proj(h, w_in, rope_units=[(0, 32)], rope_half=32)
    D = D_MODEL
    q, k, v = projr[:, 0:D], projr[:, D:2 * D], proj[:, 2 * D:3 * D]
    lam_init = 0.8 - 0.6 * math.exp(-0.3 * layer)
    nc = _prog(("da", layer), lambda: build_attn("da", NH=4, S=SEQ, lam_init=lam_init))
    cst = attn_consts()
    ims = []
    for c in range(NCORES):
        b, hh = c // 2, c % 2
        ims.append({"qT": _heads_T(q, b, hh), "kT": _heads_T(k, b, hh), "v": _heads_tok(v, b, hh), "cst": cst,
                    "lam": _rep(lam.reshape(-1)), "gn": _rep(norm_g)})
    return _gather_o(run(nc, ims))


def mixer_sb(h, w_in):
    proj, _ = run_proj(h, w_in)
    D = D_MODEL
    q, k, v = proj[:, 0:D], proj[:, D:2 * D], proj[:, 2 * D:3 * D]
    nc = _prog(("sb",), lambda: build_attn("sb", NH=4, S=SEQ))
    cst = attn_consts()
    ims = []
    for c in range(NCORES):
        b, hh = c // 2, c % 2
        ims.append({"qT": _heads_T(q, b, hh), "kT": _heads_T(k, b, hh), "v": _heads_tok(v, b, hh), "cst": cst})
    return _gather_o(run(nc, ims))


def _log(*a):
    import sys, time
    print("[kernel %.0f]" % time.time(), *a, file=sys.stderr, flush=True)


def kernel_unfused(x, hg_w_in, hg_lb, hg_norm_g, hg_w_out, da_w_in, da_lam, da_norm_g, da_w_out,
           nsa_w_in, nsa_cmp_pe, nsa_cmp_w1, nsa_cmp_w2, nsa_w_out, sb_w_in, sb_w_out,
           ffn_w_gate, ffn_w_up, ffn_w_down, moe_w_router, moe_w_gate, moe_w_up, moe_w_down,
           ln_g, ln_b):
    f = lambda a: np.asarray(a, dtype=np.float32)
    h = f(x).reshape(T_ALL, D_MODEL)
    ln_g, ln_b = f(ln_g), f(ln_b)
    for layer in range(DEPTH):
        m = layer % 4
        if m == 0:
            o = mixer_hgrn(h, f(hg_w_in), f(hg_lb), f(hg_norm_g), layer)
            w_out = f(hg_w_out)
        elif m == 1:
            o = mixer_da(h, f(da_w_in), f(da_lam), f(da_norm_g), layer)
            w_out = f(da_w_out)
        elif m == 2:
            o = mixer_nsa(h, f(nsa_w_in), f(nsa_cmp_pe), f(nsa_cmp_w1), f(nsa_cmp_w2))
            w_out = f(nsa_w_out)
        else:
            o = mixer_sb(h, f(sb_w_in))
            w_out = f(sb_w_out)
        _log("mixer", layer)
        h = run_lrln(o, w_out, h, ln_g[layer, 0], ln_b[layer, 0])
        _log("lrln", layer)
        j = layer // 2
        if layer % 2 == 0:
            h = run_ffn(h, f(ffn_w_gate)[j], f(ffn_w_up)[j], f(ffn_w_down)[j], ln_g[layer, 1], ln_b[layer, 1])
        else:
            h = run_ffn(h, f(moe_w_gate)[j], f(moe_w_up)[j], f(moe_w_down)[j], ln_g[layer, 1], ln_b[layer, 1], wr=f(moe_w_router)[j])
        _log("ffn", layer)
    return h.reshape(BATCH, SEQ, D_MODEL)


def nsa_consts(S=SEQ):
    n_cmp = (S - 32) // 16 + 1
    n_slc = S // 64
    cs = np.arange(n_cmp) * 16
    ss_ = np.arange(n_slc) * 64
    ov = np.clip(np.minimum(cs[:, None] + 32, ss_[None, :] + 64) - np.maximum(cs[:, None], ss_[None, :]), 0, None) / 32.0
    NCT = (n_cmp + 127) // 128
    M = np.zeros((NCT * 128, 128), np.float32)
    M[:n_cmp, :n_slc] = ov
    M = np.ascontiguousarray(M.reshape(NCT, 128, 128).transpose(1, 0, 2))
    NKT = S // 128
    E = np.zeros((128, NKT, 128), np.float32)
    for kt in range(NKT):
        for half in range(2):
            if 2 * kt + half < 128:
                E[2 * kt + half, kt, half * 64:(half + 1) * 64] = 32768.0
    return M, E


def emit_nsa(mk, io, S=SEQ, tm=False):
    from contextlib import ExitStack
    import ml_dtypes
    nc = mk.nc
    NH = 4
    QC = 512
    NQC = S // QC
    NKT = S // 128
    NCMP = (S - 32) // 16 + 1
    NCT = (NCMP + 127) // 128
    NCP = NCT * 128
    scale = 128 ** -0.5
    if tm:
        q_tok = io.tin("q_tok", [S, NH * 128])
        qr_tok = io.tin("qr_tok", [S, NH * 128])
        kcv_tok = [io.tin("kc_tok", [S, 128]), io.tin("vc_tok", [S, 128])]
        ks_tok = io.tin("ks_tok", [S, 128])
        kw_tok = io.tin("kw_tok", [S, 128])
    else:
        qT = io.tin("qT", [NH, 128, S])
        qrT = io.tin("qrT", [NH, 128, S])
        kcv = io.tin("kcv", [2, 128, S])
        ksT = io.tin("ksT", [128, S])
        kwT = io.tin("kwT", [128, S])
    vs = io.tin("vs", [S, 128])
    vw = io.tin("vw", [S, 128])
    gat = io.tin("gat", [S, 12])
    w1 = io.tin("w1", [128, 2 * 32 * 128])
    peT = io.tin("peT", [128, 64])
    w2 = io.tin("w2", [128, 256])
    Mc = io.tin("Mc", [128, NCT, 128])
    Ec = io.tin("Ec", [128, NKT, 128])
    cst = io.tin("cst", [128, 4, 128])
    ident_d = io.tin("ident", [128, 128])
    o = io.tout("o", [S, NH * 128])
    with ExitStack() as st:
        ct = mk.sb("ct", [128, 4, 128])
        mk.dma("sync", ct[:], cst[:, :, :], writes=["cst"])
        tri_incl, tri_strict, U, ones = ct[:, 0, :], ct[:, 1, :], ct[:, 2, :], ct[:, 3, :]
        ident = mk.sb("identt", [128, 128])
        mk.dma("sync", ident[:], ident_d[:, :], writes=["ident"])
        bufA = mk.sb("bufA", [128, S])
        bufB = mk.sb("bufB", [128, max(S, 8192)])
        vs_sb = mk.sb("vs_sb", [128, NKT, 129])
        vw_sb = mk.sb("vw_sb", [128, NKT, 129])
        EW = min(4, NKT)
        Ef = mk.sb("Ef", [128, EW * 128])
        Eb = mk.sb("Eb", [128, NKT, 128], BF16)
        Mt = mk.sb("Mt", [128, NCT, 128])
        pet = mk.sb("pet", [128, 64])
        w2t = mk.sb("w2t", [128, 256])
        kcmpT = mk.sb("kcmpT", [128, NCP])
        vcm = mk.sb("vcm", [128, NCT, 256])
        pT = [mk.sb("pT%d" % i, [128, QC]) for i in range(3)]
        gu, g2, gl = pT[0], pT[1], pT[2]
        bia = mk.sb("bia", [128, 2])
        psS = [mk.ps("psS%d" % i, [128, 512]) for i in range(2)]
        psC = [mk.ps("psC%d" % i, [128, 512]) for i in range(2)]
        psX = mk.ps("psX", [128, 512])
        psY = mk.ps("psY", [128, 512])
        psZ = mk.ps("psZ", [128, 512])
        psT = mk.ps("psT", [128, 512])
        if tm:
            tmps = [(mk.sb("ltmp%d" % i, [128, 4, 128]), "ltmp%d" % i) for i in range(2)]
        mk.dma("sync", Mt[:], Mc[:, :, :], writes=["Mt"])
        mk.dma("sync", pet[:], peT[:, :], writes=["pet"])
        mk.dma("sync", w2t[:], w2[:, :], writes=["w2t"])
        for c4 in range(4):
            mk.dma("sync", bufB[:, c4 * 2048:(c4 + 1) * 2048], w1[:, c4 * 2048:(c4 + 1) * 2048], writes=["bufB"])
        for c4 in range(NKT // EW):
            mk.dma("poolq", Ef[:], Ec[:, c4 * EW:(c4 + 1) * EW, :].rearrange("p a b -> p (a b)"), writes=["Ef"])
            mk.op("dve", lambda e: e.tensor_copy(out=Eb[:, c4 * EW:(c4 + 1) * EW, :].rearrange("p a b -> p (a b)"), in_=Ef[:]),
                  reads=["Ef"], writes=["Eb"])
        mk.op("dve", lambda e: e.memset(vs_sb[:, :, 128:129], 1.0), writes=["vs_sb"])
        mk.op("dve", lambda e: e.memset(vw_sb[:, :, 128:129], 1.0), writes=["vw_sb"])
        mk.op("dve", lambda e: e.memset(gl[:], 0.0), writes=["pT2"])
        for c4 in range(4):
            k0, k1 = c4 * (NKT // 4), (c4 + 1) * (NKT // 4)
            mk.dma("poolq", vs_sb[:, k0:k1, 0:128], vs[k0 * 128:k1 * 128, :].rearrange("(kt p) d -> p kt d", p=128), writes=["vs_sb"])
            mk.dma("poolq", vw_sb[:, k0:k1, 0:128], vw[k0 * 128:k1 * 128, :].rearrange("(kt p) d -> p kt d", p=128), writes=["vw_sb"])
        w1v = bufB[:].rearrange("p (j i n) -> p j i n", j=2, i=32)
        for j in range(2):
            if tm:
                emit_loadT(mk, bufA[:], "bufA", kcv_tok[j], NKT, tmps, psT, "psT", ident[:])
            else:
                for c4 in range(4):
                    sl = slice(c4 * (S // 4), (c4 + 1) * (S // 4))
                    mk.dma("sync", bufA[:, sl], kcv[j, :, sl], writes=["bufA"])
            for i in range(32):
                mk.op("pe", lambda e: e.matmul(psS[0][:, 0:NCMP], lhsT=w1v[:, j, i, :], rhs=bufA[:, i:i + 16 * (NCMP - 1) + 1:16],
                                               start=(i == 0), stop=(i == 31)),
                      reads=["bufA", "bufB"], writes=["psS0"])
            for i in range(32):
                mk.op("pe", lambda e: e.matmul(psS[1][:, 0:1], lhsT=w1v[:, j, i, :], rhs=pet[:, j * 32 + i:j * 32 + i + 1],
                                               start=(i == 0), stop=(i == 31)),
                      reads=["pet", "bufB"], writes=["psS1"])
            mk.op("dve", lambda e: e.tensor_copy(out=bia[:, j:j + 1], in_=psS[1][:, 0:1]), reads=["psS1"], writes=["bia"])
            mk.op("act", lambda e: e.activation(out=gu[:, 0:NCMP], in_=psS[0][:, 0:NCMP], func=AF.Identity, bias=bia[:, j:j + 1], scale=1.0),
                  reads=["psS0", "bia"], writes=["pT0"])
            mk.op("dve", lambda e: e.tensor_tensor(out=g2[:, 0:NCMP], in0=gu[:, 0:NCMP], in1=gu[:, 0:NCMP], op=ALU.mult), reads=["pT0"], writes=["pT1"])
            mk.op("dve", lambda e: e.tensor_scalar(out=g2[:, 0:NCMP], in0=g2[:, 0:NCMP], scalar1=0.044715, scalar2=1.0, op0=ALU.mult, op1=ALU.add),
                  reads=["pT1"], writes=["pT1"])
            mk.op("dve", lambda e: e.tensor_tensor(out=g2[:, 0:NCMP], in0=g2[:, 0:NCMP], in1=gu[:, 0:NCMP], op=ALU.mult), reads=["pT1", "pT0"], writes=["pT1"])
            mk.op("act", lambda e: e.activation(out=g2[:, 0:NCMP], in_=g2[:, 0:NCMP], func=AF.Tanh, scale=0.7978845608028654), reads=["pT1"], writes=["pT1"])
            mk.op("dve", lambda e: e.tensor_scalar(out=g2[:, 0:NCMP], in0=g2[:, 0:NCMP], scalar1=0.5, scalar2=0.5, op0=ALU.mult, op1=ALU.add),
                  reads=["pT1"], writes=["pT1"])
            mk.op("dve", lambda e: e.tensor_tensor(out=gl[:, 0:NCMP], in0=g2[:, 0:NCMP], in1=gu[:, 0:NCMP], op=ALU.mult), reads=["pT1", "pT0", "pT2"], writes=["pT2"])
            if j == 0:
                mk.op("pe", lambda e: e.matmul(psS[0][:, 0:NCP], lhsT=w2t[:, 0:128], rhs=gl[:, 0:NCP], start=True, stop=True),
                      reads=["pT2", "w2t"], writes=["psS0"])
                mk.op("act", lambda e: e.copy(out=kcmpT[:], in_=psS[0][:, 0:NCP]), reads=["psS0"], writes=["kcmpT"])
            else:
                for t4 in range(NCT):
                    mk.op("pe", lambda e: e.matmul(psS[0][:, t4 * 128:(t4 + 1) * 128], lhsT=gl[:, t4 * 128:(t4 + 1) * 128], rhs=w2t[:, 128:256],
                                                   start=True, stop=True),
                          reads=["pT2", "w2t"], writes=["psS0"])
                mk.op("act", lambda e: e.copy(out=vcm[:, :, 0:128], in_=psS[0][:, 0:NCP].rearrange("p (a b) -> p a b", b=128)),
                      reads=["psS0"], writes=["vcm"])
                mk.op("dve", lambda e: e.tensor_copy(out=vcm[:, :, 128:256], in_=Mt[:]), reads=["Mt"], writes=["vcm"])
        if tm:
            emit_loadT(mk, bufA[:], "bufA", ks_tok, NKT, tmps, psT, "psT", ident[:])
            emit_loadT(mk, bufB[:], "bufB", kw_tok, NKT, tmps, psT, "psT", ident[:])
        else:
            for c4 in range(4):
                sl = slice(c4 * (S // 4), (c4 + 1) * (S // 4))
                mk.dma("sync", bufA[:, sl], ksT[:, sl], writes=["bufA"])
                mk.dma("sync", bufB[:, sl], kwT[:, sl], writes=["bufB"])
        cmask = mk.sb("cmask", [128, 5, QC])
        mk.op("dve", lambda e: e.memset(cmask[:], 1.0), writes=["cmask"])
        for dd in range(5):
            mk.op("pool", lambda e: e.affine_select(out=cmask[:, dd, :], in_=cmask[:, dd, :], pattern=[[1, QC]], compare_op=ALU.is_ge, fill=0.0,
                                                    base=512 * dd - 31, channel_multiplier=-16), reads=["cmask"], writes=["cmask"])
        MA = mk.sb("MA", [128, 256])
        MV = mk.sb("MV", [128, 256])
        FA = mk.sb("FA", [128, 256])
        FV = mk.sb("FV", [128, 256])
        mk.op("dve", lambda e: e.memset(MA[:], 1.0), writes=["masters"])
        mk.op("dve", lambda e: e.memset(MV[:], 1.0), reads=["masters"], writes=["masters"])
        mk.op("pool", lambda e: e.affine_select(out=MA[:], in_=MA[:], pattern=[[-64, 256]], compare_op=ALU.is_ge, fill=0.0,
                                                base=8064, channel_multiplier=1), reads=["masters"], writes=["masters"])
        mk.op("pool", lambda e: e.affine_select(out=MV[:], in_=MV[:], pattern=[[-64, 256]], compare_op=ALU.is_ge, fill=0.0,
                                                base=8192, channel_multiplier=1), reads=["masters"], writes=["masters"])
        mk.op("dve", lambda e: e.tensor_scalar(out=FA[:], in0=MA[:], scalar1=-1e9, scalar2=1e9, op0=ALU.mult, op1=ALU.add), reads=["masters"], writes=["masters"])
        mk.op("dve", lambda e: e.tensor_scalar(out=FV[:], in0=MV[:], scalar1=1e30, scalar2=-1e30, op0=ALU.mult, op1=ALU.add), reads=["masters"], writes=["masters"])
        NB = 2
        q_sb = [mk.sb("q_sb%d" % i, [128, QC]) for i in range(NB)]
        qr_sb = q_sb
        ocmp = mk.sb("ocmp", [128, 4, NH, 128])
        imp = mk.sb("imp", [128, 4, 128])
        imx = mk.sb("imx", [128, 4, 128])
        selm = mk.sb("selm", [128, 4, 128])
        vmk = imx
        mx8 = mk.sb("mx8", [128, 16])
        rs = mk.sb("rs", [128, 8])
        selT = mk.sb("selT", [128, QC], BF16)
        gt_sb = mk.sb("gt_sb", [128, 4, 12])
        o_sb = [mk.sb("o_sb%d" % i, [128, 4, 128]) for i in range(NB)]
        fin = mk.sb("fin", [128, 8])
        it = 0

        def acc_ap(bank, bk, jq, idx):
            if jq < 3:
                return bank[:, jq * 129:(jq + 1) * 129], bk
            return psZ[:, idx * 129:(idx + 1) * 129], "psZ"

        for qc in range(NQC):
            q0 = qc * QC
            mk.dma("poolq", gt_sb[:], gat[q0:q0 + QC, :].rearrange("(j p) g -> p j g", p=128), writes=["gt_sb"])
            mk.op("act", lambda e: e.activation(out=gt_sb[:], in_=gt_sb[:], func=AF.Sigmoid), reads=["gt_sb"], writes=["gt_sb"])
            nct = min(NCT, (32 * qc + 30) // 128 + 1)
            for hd in range(NH):
                qb = (qc * NH + hd) % NB
                if tm:
                    emit_loadT(mk, q_sb[qb][:], "q_sb%d" % qb, q_tok[q0:q0 + QC, hd * 128:(hd + 1) * 128], QC // 128, tmps, psT, "psT", ident[:])
                else:
                    mk.dma("sync", q_sb[qb][:], qT[hd, :, q0:q0 + QC], writes=["q_sb%d" % qb])
                for c_t in range(nct):
                    pS, pk = psS[it % 2], "psS%d" % (it % 2)
                    pt, ptk = pT[it % 3], "pT%d" % (it % 3)
                    it += 1
                    mk.op("pe", lambda e: e.matmul(pS[:, 0:QC], lhsT=kcmpT[:, c_t * 128:(c_t + 1) * 128], rhs=q_sb[qb][:, :], start=True, stop=True),
                          reads=["kcmpT", "q_sb%d" % qb], writes=[pk])
                    mk.op("act", lambda e: e.activation(out=pt[:, :], in_=pS[:, 0:QC], func=AF.Exp, scale=float(scale)), reads=[pk], writes=[ptk])
                    delta = q0 - 2048 * c_t
                    if delta < 2063:
                        mk.op("pool", lambda e: e.tensor_tensor(out=pt[:, :], in0=pt[:, :], in1=cmask[:, delta // 512, :], op=ALU.mult),
                              reads=[ptk, "cmask"], writes=[ptk])
                    for jq in range(4):
                        bank = psC[jq // 2]
                        mk.op("pe", lambda e: e.matmul(bank[:, (jq % 2) * 256:(jq % 2 + 1) * 256], lhsT=pt[:, jq * 128:(jq + 1) * 128], rhs=vcm[:, c_t, :],
                                                       start=(c_t == 0 and jq % 2 == 0), stop=(c_t == nct - 1), skip_group_check=True),
                              reads=[ptk, "vcm"], writes=["psC%d" % (jq // 2)])
                for jq in range(4):
                    bank = psC[jq // 2]
                    cb = (jq % 2) * 256
                    mk.op("dve", lambda e: e.tensor_reduce(out=rs[:, 0:1], in_=bank[:, cb + 128:cb + 256], axis=AX.X, op=ALU.add),
                          reads=["psC%d" % (jq // 2)], writes=["rs"])
                    mk.op("dve", lambda e: e.tensor_scalar(out=rs[:, 0:1], in0=rs[:, 0:1], scalar1=1e-30, scalar2=None, op0=ALU.max), reads=["rs"], writes=["rs"])
                    mk.op("dve", lambda e: e.reciprocal(out=rs[:, 1:2], in_=rs[:, 0:1]), reads=["rs"], writes=["rs"])
                    mk.op("dve", lambda e: e.tensor_scalar(out=ocmp[:, jq, hd, :], in0=bank[:, cb:cb + 128], scalar1=rs[:, 1:2], scalar2=None, op0=ALU.mult),
                          reads=["psC%d" % (jq // 2), "rs"], writes=["ocmp"])
                    if hd == 0:
                        mk.op("dve", lambda e: e.tensor_scalar(out=imp[:, jq, :], in0=bank[:, cb + 128:cb + 256], scalar1=rs[:, 1:2], scalar2=None, op0=ALU.mult),
                              reads=["psC%d" % (jq // 2), "rs"], writes=["imp"])
                    else:
                        mk.op("dve", lambda e: e.scalar_tensor_tensor(out=imp[:, jq, :], in0=bank[:, cb + 128:cb + 256], scalar=rs[:, 1:2], in1=imp[:, jq, :],
                                                                      op0=ALU.mult, op1=ALU.add),
                              reads=["psC%d" % (jq // 2), "rs", "imp"], writes=["imp"])
            for jq in range(4):
                tb = q0 + jq * 128
                x = imp[:, jq, :]
                tt2 = 2 * (tb // 128)
                msl = slice(128 - tt2, 256 - tt2)
                mk.op("pool", lambda e: e.tensor_tensor(out=x, in0=x, in1=MA[:, msl], op=ALU.mult), reads=["imp", "masters"], writes=["imp"])
                mk.op("pool", lambda e: e.tensor_tensor(out=x, in0=x, in1=FA[:, msl], op=ALU.add), reads=["imp", "masters"], writes=["imp"])
                mk.op("dve", lambda e: e.memset(imp[:, jq, 0:1], 1e9), reads=["imp"], writes=["imp"])
                mk.op("pool", lambda e: e.tensor_tensor(out=x, in0=x, in1=MV[:, msl], op=ALU.mult), reads=["imp", "masters"], writes=["imp"])
                mk.op("pool", lambda e: e.tensor_tensor(out=x, in0=x, in1=FV[:, msl], op=ALU.add), reads=["imp", "masters"], writes=["imp"])
                mk.op("dve", lambda e: e.max(out=mx8[:, 0:8], in_=x), reads=["imp"], writes=["mx8"])
                mk.op("dve", lambda e: e.match_replace(out=imx[:, jq, :], in_to_replace=mx8[:, 0:8], in_values=x, imm_value=-3e38),
                      reads=["imp", "mx8"], writes=["imx"])
                mk.op("dve", lambda e: e.max(out=mx8[:, 8:16], in_=imx[:, jq, :]), reads=["imx"], writes=["mx8"])
                mk.op("dve", lambda e: e.tensor_scalar(out=selm[:, jq, :], in0=x, scalar1=mx8[:, 15:16], scalar2=None, op0=ALU.is_ge),
                      reads=["imp", "mx8"], writes=["selm"])
                mk.op("dve", lambda e: e.tensor_scalar(out=vmk[:, jq, :], in0=x, scalar1=-5e29, scalar2=None, op0=ALU.is_gt), reads=["imp"], writes=["imx"])
                mk.op("dve", lambda e: e.tensor_tensor(out=selm[:, jq, :], in0=selm[:, jq, :], in1=vmk[:, jq, :], op=ALU.mult), reads=["selm", "imx"], writes=["selm"])
                mk.op("dve", lambda e: e.tensor_scalar(out=selm[:, jq, :], in0=selm[:, jq, :], scalar1=-1.0, scalar2=None, op0=ALU.add), reads=["selm"], writes=["selm"])
                mk.op("pe", lambda e: e.matmul(psT[:, jq * 128:(jq + 1) * 128], lhsT=selm[:, jq, :], rhs=ident[:], start=True, stop=True),
                      reads=["selm", "ident"], writes=["psT"])
            mk.op("act", lambda e: e.copy(out=selT[:], in_=psT[:, 0:QC]), reads=["psT"], writes=["selT"])
            for hd in range(NH):
                qb = (qc * NH + hd) % NB
                ob = (qc * NH + hd) % NB
                if tm:
                    emit_loadT(mk, qr_sb[qb][:], "q_sb%d" % qb, qr_tok[q0:q0 + QC, hd * 128:(hd + 1) * 128], QC // 128, tmps, psT, "psT", ident[:])
                else:
                    mk.dma("sync", qr_sb[qb][:], qrT[hd, :, q0:q0 + QC], writes=["q_sb%d" % qb])
                nkt = 4 * qc + 4
                stA, stB = [], []
                for kt in range(nkt):
                    def mk_item(kt=kt, it=it):
                        r = max(0, kt - 4 * qc)
                        c0 = 128 * r
                        pS, pk = psS[it % 2], "psS%d" % (it % 2)
                        pt, ptk = pT[it % 3], "pT%d" % (it % 3)

                        def A():
                            mk.op("pe", lambda e: e.matmul(pS[:, c0:QC], lhsT=bufA[:, kt * 128:(kt + 1) * 128], rhs=qr_sb[qb][:, c0:QC], start=True, stop=False),
                                  reads=["bufA", "q_sb%d" % qb], writes=[pk])
                            mk.op("pe", lambda e: e.matmul(pS[:, c0:QC], lhsT=Eb[:, kt, :], rhs=selT[:, c0:QC], start=False, stop=True),
                                  reads=["Eb", "selT"], writes=[pk])
                            mk.op("act", lambda e: e.activation(out=pt[:, c0:QC], in_=pS[:, c0:QC], func=AF.Exp, scale=float(scale)), reads=[pk], writes=[ptk])
                            if kt >= 4 * qc:
                                mk.op("pool", lambda e: e.tensor_tensor(out=pt[:, c0:c0 + 128], in0=pt[:, c0:c0 + 128], in1=tri_incl, op=ALU.mult),
                                      reads=[ptk, "cst"], writes=[ptk])

                        def B():
                            for jq in range(r, 4):
                                ap, ak = acc_ap(psX, "psX", jq, 0)
                                mk.op("pe", lambda e: e.matmul(ap, lhsT=pt[:, jq * 128:(jq + 1) * 128], rhs=vs_sb[:, kt, :],
                                                               start=(kt == 0 and jq in (0, 3)), stop=(kt == 4 * qc + jq), skip_group_check=True),
                                      reads=[ptk, "vs_sb"], writes=[ak])
                        return A, B
                    a_, b_ = mk_item()
                    it += 1
                    stA.append(a_)
                    stB.append(b_)
                emit_skewed([stA, stB])
                first_w = {}
                stA, stB = [], []
                for kt in range(max(0, 4 * qc - 4), nkt):
                    far = kt < 4 * qc
                    r = kt - (4 * qc - 4) if far else kt - 4 * qc
                    if far:
                        cA, cB = 0, 128 * (r + 1)
                        slices = list(range(0, r + 1))
                    else:
                        cA, cB = 128 * r, QC
                        slices = list(range(r, 4))
                    plan = []
                    for jq in slices:
                        isfirst = jq not in first_w
                        first_w[jq] = True
                        bank_first = isfirst and (jq == 3 or len(first_w) == 1 or (jq < 3 and all(x == 3 for x in first_w if x != jq)))
                        plan.append((jq, bank_first))

                    def mk_item(kt=kt, far=far, r=r, cA=cA, cB=cB, plan=plan, it=it):
                        pS, pk = psS[it % 2], "psS%d" % (it % 2)
                        pt, ptk = pT[it % 3], "pT%d" % (it % 3)

                        def A():
                            mk.op("pe", lambda e: e.matmul(pS[:, cA:cB], lhsT=bufB[:, kt * 128:(kt + 1) * 128], rhs=qr_sb[qb][:, cA:cB], start=True, stop=True),
                                  reads=["bufB", "q_sb%d" % qb], writes=[pk])
                            mk.op("act", lambda e: e.activation(out=pt[:, cA:cB], in_=pS[:, cA:cB], func=AF.Exp, scale=float(scale)), reads=[pk], writes=[ptk])
                            if far:
                                mk.op("pool", lambda e: e.tensor_tensor(out=pt[:, 128 * r:128 * r + 128], in0=pt[:, 128 * r:128 * r + 128], in1=U, op=ALU.mult),
                                      reads=[ptk, "cst"], writes=[ptk])
                            else:
                                mk.op("pool", lambda e: e.tensor_tensor(out=pt[:, cA:cA + 128], in0=pt[:, cA:cA + 128], in1=tri_incl, op=ALU.mult),
                                      reads=[ptk, "cst"], writes=[ptk])

                        def B():
                            for (jq, bank_first) in plan:
                                ap, ak = acc_ap(psY, "psY", jq, 1)
                                mk.op("pe", lambda e: e.matmul(ap, lhsT=pt[:, jq * 128:(jq + 1) * 128], rhs=vw_sb[:, kt, :],
                                                               start=bank_first, stop=(kt == 4 * qc + jq), skip_group_check=True),
                                      reads=[ptk, "vw_sb"], writes=[ak])
                        return A, B
                    a_, b_ = mk_item()
                    it += 1
                    stA.append(a_)
                    stB.append(b_)
                emit_skewed([stA, stB])
                for jq in range(4):
                    aS, aSk = acc_ap(psX, "psX", jq, 0)
                    aW, aWk = acc_ap(psY, "psY", jq, 1)
                    mk.op("dve", lambda e: e.reciprocal(out=fin[:, 0:1], in_=aS[:, 128:129]), reads=[aSk], writes=["fin"])
                    mk.op("dve", lambda e: e.reciprocal(out=fin[:, 1:2], in_=aW[:, 128:129]), reads=[aWk], writes=["fin"])
                    mk.op("dve", lambda e: e.tensor_tensor(out=fin[:, 0:1], in0=fin[:, 0:1], in1=gt_sb[:, jq, hd * 3 + 1:hd * 3 + 2], op=ALU.mult),
                          reads=["fin", "gt_sb"], writes=["fin"])
                    mk.op("dve", lambda e: e.tensor_tensor(out=fin[:, 1:2], in0=fin[:, 1:2], in1=gt_sb[:, jq, hd * 3 + 2:hd * 3 + 3], op=ALU.mult),
                          reads=["fin", "gt_sb"], writes=["fin"])
                    mk.op("dve", lambda e: e.tensor_scalar(out=o_sb[ob][:, jq, :], in0=ocmp[:, jq, hd, :], scalar1=gt_sb[:, jq, hd * 3:hd * 3 + 1], scalar2=None, op0=ALU.mult),
                          reads=["ocmp", "gt_sb"], writes=["o_sb%d" % ob])
                    mk.op("dve", lambda e: e.scalar_tensor_tensor(out=o_sb[ob][:, jq, :], in0=aS[:, 0:128], scalar=fin[:, 0:1], in1=o_sb[ob][:, jq, :],
                                                                  op0=ALU.mult, op1=ALU.add),
                          reads=[aSk, "fin", "o_sb%d" % ob], writes=["o_sb%d" % ob])
                    mk.op("dve", lambda e: e.scalar_tensor_tensor(out=o_sb[ob][:, jq, :], in0=aW[:, 0:128], scalar=fin[:, 1:2], in1=o_sb[ob][:, jq, :],
                                                                  op0=ALU.mult, op1=ALU.add),
                          reads=[aWk, "fin", "o_sb%d" % ob], writes=["o_sb%d" % ob])
                mk.dma("sync", o[q0:q0 + QC, hd * 128:(hd + 1) * 128].rearrange("(j p) d -> p j d", p=128), o_sb[ob][:],
                       reads=["o_sb%d" % ob], is_output=True)


def mixer_nsa(h, w_in, cmp_pe, cmp_w1, cmp_w2):
    D = D_MODEL
    proj, projr = run_proj(h, w_in, rope_units=[(0, 8), (1536, 2), (2048, 2)], rope_half=64)
    nc = _prog(("nsa",), lambda: build_nsa(SEQ))
    Mc, Ec = nsa_consts(SEQ)
    cst = attn_consts()
    q, qr = proj[:, 0:D], projr[:, 0:D]
    kv = proj[:, D:D + 1536].reshape(BATCH, SEQ, 6, 2, 128)
    kvr = projr[:, D:D + 1536].reshape(BATCH, SEQ, 6, 2, 128)
    gates = proj[:, D + 1536:D + 1536 + 24].reshape(BATCH, SEQ, 8, 3)
    w1 = _c(cmp_w1.reshape(2, 32, 128, 128).transpose(2, 0, 1, 3).reshape(128, 2 * 32 * 128))
    peT = _c(cmp_pe.transpose(2, 0, 1).reshape(128, 64))
    w2 = _c(cmp_w2.transpose(1, 0, 2).reshape(128, 256))
    ident = np.eye(128, dtype=np.float32)
    ims = []
    for c in range(NCORES):
        b, g = c // 2, c % 2
        ims.append({"qT": _heads_T(q, b, g), "qrT": _heads_T(qr, b, g),
                    "kcv": _c(np.stack([kv[b, :, 0, g, :].T, kv[b, :, 1, g, :].T])),
                    "ksT": _c(kvr[b, :, 2, g, :].T), "kwT": _c(kvr[b, :, 4, g, :].T),
                    "vs": _c(kv[b, :, 3, g, :]), "vw": _c(kv[b, :, 5, g, :]),
                    "gat": _c(gates[b, :, 4 * g:4 * g + 4, :].reshape(SEQ, 12)),
                    "w1": w1, "peT": peT, "w2": w2, "Mc": Mc, "Ec": Ec, "cst": cst, "ident": ident})
    return _gather_o(run(nc, ims))


def build_fused(layers=(0, 1, 2, 3), S=SEQ, debug_outs=False):
    from contextlib import ExitStack
    nc = bass.Bass("TRN2", target_bir_lowering=False)
    D = D_MODEL

    def tin(name, shape):
        return nc.dram_tensor(name, list(shape), F32, kind="ExternalInput").ap()

    def scratch(name, shape):
        return nc.dram_tensor(name, list(shape), F32, kind="Internal").ap()

    x = tin("x", [S, D])
    SH = S // 2
    y_out = nc.dram_tensor("y", [SH, D], F32, kind="ExternalOutput").ap()
    tokidx = nc.dram_tensor("tokidx", [128, SH // 128], mybir.dt.int32, kind="ExternalInput").ap()
    ident = tin("ident", [128, 128])
    cst = tin("cst", [128, 4, 128])
    lng = {(l, j): tin("lng%d%d" % (l, j), [128, D]) for l in layers for j in range(2)}
    lnb = {(l, j): tin("lnb%d%d" % (l, j), [128, D]) for l in layers for j in range(2)}
    proj = scratch("proj", [S, 4096])
    projr = scratch("projr", [S, 4096])
    o_d = scratch("o_d", [S, D])
    hA = scratch("hA", [S, D])
    hB = scratch("hB", [S, D])
    FTd, FTe = D_FF // 128, D_FF_EXPERT // 128
    with ExitStack() as st:
        mk = MK(nc, st)

        def stage(emit, given, *a, **kw):
            mk.begin_stage()
            emit(mk, IO(nc, given), *a, **kw)
            mk.end_stage()

        h_in = x
        for li, layer in enumerate(layers):
            m = layer % 4
            last = (li == len(layers) - 1)
            if m == 0:
                N = 4096
                w_in = tin("hg_w_in", [D, N])
                w_out = tin("hg_w_out", [D, D])
                stage(emit_gemm, {"a": h_in, "ident": ident, "w": w_in, "y": proj[:, 0:N]}, D, N, ntok=S, tm=True)
                W = 2048
                given = {"q_tok": proj[:, 0:D], "f_tok": proj[:, D:2 * D], "v_tok": proj[:, 2 * D:3 * D], "g_tok": proj[:, 3 * D:4 * D],
                         "lbT": tin("hg_lbT", [8, 128, 5]), "gn": tin("hg_gn", [128, 128]), "msk": tin("hg_msk", [128, W]),
                         "cst": tin("hg_cst", [128, 2, 128]), "o": o_d}
                stage(emit_hgrn, given, NH=8, S=S, layer=layer, W=W, tm=True)
            elif m == 1:
                N = 3072
                w_in = tin("da_w_in", [D, N])
                w_out = tin("da_w_out", [D, D])
                stage(emit_gemm, {"a": h_in, "ident": ident, "w": w_in, "y": proj[:, 0:N], "yr": projr[:, 0:N],
                                  "cos": tin("cos32", [S, 32]), "sin": tin("sin32", [S, 32])},
                      D, N, rope_units=[(0, 32)], rope_half=32, ntok=S, tm=True)
                lam_init = 0.8 - 0.6 * math.exp(-0.3 * layer)
                given = {"q_tok": projr[:, 0:D], "k_tok": projr[:, D:2 * D], "v_tok": proj[:, 2 * D:3 * D], "ident": ident, "cst": cst,
                         "lam": tin("da_lam", [128, 256]), "gn": tin("da_gn", [128, 128]), "o": o_d}
                stage(emit_attn, given, "da", NH=8, S=S, lam_init=lam_init, tm=True)
            elif m == 2:
                N = 2584
                w_in = tin("nsa_w_in", [D, N])
                w_out = tin("nsa_w_out", [D, D])
                stage(emit_gemm, {"a": h_in, "ident": ident, "w": w_in, "y": proj[:, 0:N], "yr": projr[:, 0:N],
                                  "cos": tin("cos64", [S, 64]), "sin": tin("sin64", [S, 64])},
                      D, N, rope_units=[(0, 8), (1536, 2), (2048, 2)], rope_half=64, ntok=S, tm=True)
                NKT = S // 128
                NCT = ((S - 32) // 16 + 1 + 127) // 128
                nsa_c = {"w1": tin("nsa_w1", [128, 2 * 32 * 128]), "peT": tin("nsa_peT", [128, 64]), "w2": tin("nsa_w2", [128, 256]),
                         "Mc": tin("nsa_Mc", [128, NCT, 128]), "Ec": tin("nsa_Ec", [128, NKT, 128])}
                for g in range(2):
                    def kvc(t, j):
                        c0 = D + j * 256 + g * 128
                        return t[:, c0:c0 + 128]
                    given = dict(nsa_c)
                    given.update({"q_tok": proj[:, g * 512:(g + 1) * 512], "qr_tok": projr[:, g * 512:(g + 1) * 512],
                                  "kc_tok": kvc(proj, 0), "vc_tok": kvc(proj, 1), "ks_tok": kvc(projr, 2), "vs": kvc(proj, 3),
                                  "kw_tok": kvc(projr, 4), "vw": kvc(proj, 5), "gat": proj[:, 2560 + 12 * g:2560 + 12 * g + 12],
                                  "cst": cst, "ident": ident, "o": o_d[:, g * 512:(g + 1) * 512]})
                    stage(emit_nsa, given, S=S, tm=True)
            else:
                N = 3072
                w_in = tin("sb_w_in", [D, N])
                w_out = tin("sb_w_out", [D, D])
                stage(emit_gemm, {"a": h_in, "ident": ident, "w": w_in, "y": proj[:, 0:N]}, D, N, ntok=S, tm=True)
                given = {"q_tok": proj[:, 0:D], "k_tok": proj[:, D:2 * D], "v_tok": proj[:, 2 * D:3 * D], "ident": ident, "cst": cst, "o": o_d}
                stage(emit_attn, given, "sb", NH=8, S=S, tm=True)
            nt = SH if last else S
            given = {"a": o_d, "ident": ident, "w": w_out, "hres": h_in, "lng": lng[(layer, 0)], "lnb": lnb[(layer, 0)], "y": hA[0:nt, :]}
            if last:
                given["tokidx"] = tokidx
            stage(emit_gemm, given, D, D, ln=True, ntok=nt, tm=True, gather=last)
            h_out = y_out if last else hB
            j = layer // 2
            if layer % 2 == 0:
                given = {"h": hA[0:nt, :], "ident": ident, "wg": tin("ffn_wg%d" % j, [1, FTd, 128, 8, 128]), "wu": tin("ffn_wu%d" % j, [1, FTd, 128, 8, 128]),
                         "wd": tin("ffn_wd%d" % j, [1, FTd, 128, D]), "lng": lng[(layer, 1)], "lnb": lnb[(layer, 1)], "y": h_out}
                stage(emit_ffn, given, D_FF, 1, ntok=nt, tm=True)
            else:
                given = {"h": hA[0:nt, :], "ident": ident, "wg": tin("moe_wg%d" % j, [8, FTe, 128, 8, 128]), "wu": tin("moe_wu%d" % j, [8, FTe, 128, 8, 128]),
                         "wd": tin("moe_wd%d" % j, [8, FTe, 128, D]), "wr": tin("moe_wr%d" % j, [D, 8]),
                         "lng": lng[(layer, 1)], "lnb": lnb[(layer, 1)], "y": h_out}
                stage(emit_ffn, given, D_FF_EXPERT, N_EXPERTS, ntok=nt, tm=True)
            h_in = hB
        mk.finish()
    return nc


def fused_inputs(inp, layers=(0, 1, 2, 3), S=SEQ):
    f = lambda a: np.asarray(a, dtype=np.float32)
    sh = {"ident": np.eye(128, dtype=np.float32), "cst": attn_consts()}
    ln_g, ln_b = f(inp["ln_g"]), f(inp["ln_b"])
    for l in layers:
        for j in range(2):
            sh["lng%d%d" % (l, j)] = _rep(ln_g[l, j])
            sh["lnb%d%d" % (l, j)] = _rep(ln_b[l, j])
        m = l % 4
        jj = l // 2
        if m == 0:
            sh["hg_w_in"], sh["hg_w_out"] = _c(inp["hg_w_in"]), _c(inp["hg_w_out"])
            lb = f(inp["hg_lb"])
            sh["hg_lbT"] = _c(lb.T.reshape(N_HEADS, 128, lb.shape[0]))
            sh["hg_gn"] = _rep(f(inp["hg_norm_g"]))
            sh["hg_msk"], sh["hg_cst"] = hg_consts(2048)
        elif m == 1:
            sh["da_w_in"], sh["da_w_out"] = _c(inp["da_w_in"]), _c(inp["da_w_out"])
            sh["da_lam"] = _rep(f(inp["da_lam"]).reshape(-1))
            sh["da_gn"] = _rep(f(inp["da_norm_g"]))
            cos, sin = _rope_tables(32)
            sh["cos32"], sh["sin32"] = _c(cos[:S]), _c(sin[:S])
        elif m == 2:
            sh["nsa_w_in"], sh["nsa_w_out"] = _c(inp["nsa_w_in"]), _c(inp["nsa_w_out"])
            cos, sin = _rope_tables(64)
            sh["cos64"], sh["sin64"] = _c(cos[:S]), _c(sin[:S])
            sh["nsa_w1"] = _c(f(inp["nsa_cmp_w1"]).reshape(2, 32, 128, 128).transpose(2, 0, 1, 3).reshape(128, 2 * 32 * 128))
            sh["nsa_peT"] = _c(f(inp["nsa_cmp_pe"]).transpose(2, 0, 1).reshape(128, 64))
            sh["nsa_w2"] = _c(f(inp["nsa_cmp_w2"]).transpose(1, 0, 2).reshape(128, 256))
            sh["nsa_Mc"], sh["nsa_Ec"] = nsa_consts(S)
        else:
            sh["sb_w_in"], sh["sb_w_out"] = _c(inp["sb_w_in"]), _c(inp["sb_w_out"])
        if l % 2 == 0:
            F = D_FF
            sh["ffn_wg%d" % jj] = pretile_w_in(f(inp["ffn_w_gate"])[jj], F)[None]
            sh["ffn_wu%d" % jj] = pretile_w_in(f(inp["ffn_w_up"])[jj], F)[None]
            sh["ffn_wd%d" % jj] = _c(f(inp["ffn_w_down"])[jj].reshape(1, F // 128, 128, D_MODEL))
        else:
            F = D_FF_EXPERT
            sh["moe_wg%d" % jj] = np.stack([pretile_w_in(f(inp["moe_w_gate"])[jj, e], F) for e in range(N_EXPERTS)])
            sh["moe_wu%d" % jj] = np.stack([pretile_w_in(f(inp["moe_w_up"])[jj, e], F) for e in range(N_EXPERTS)])
            sh["moe_wd%d" % jj] = _c(f(inp["moe_w_down"])[jj].reshape(N_EXPERTS, F // 128, 128, D_MODEL))
            sh["moe_wr%d" % jj] = _c(f(inp["moe_w_router"])[jj])
    return sh


def kernel(**inp):
    _log("build start")
    nc = build_fused()
    _log("build done", nc.n_instructions())
    sh = fused_inputs(inp)
    x = np.asarray(inp["x"], dtype=np.float32)
    ims = []
    SH = SEQ // 2
    for c in range(NCORES):
        im = dict(sh)
        im["x"] = _c(x[c // 2])
        im["tokidx"] = fused_tokidx(c)
        ims.append(im)
    _log("inputs ready")
    res = run(nc, ims)
    _log("run done")
    return np.stack([np.concatenate([res[2 * b]["y"], res[2 * b + 1]["y"]], 0) for b in range(BATCH)], 0)


def fused_tokidx(c, S=SEQ):
    SH = S // 2
    base = (c % 2) * SH
    return np.ascontiguousarray((base + np.arange(SH, dtype=np.int32)).reshape(SH // 128, 128).T)
```
